# Optimizing a Trainium2 kernel written in Bass

```python
import math
import jax, jax.numpy as jnp
from jax import lax
import numpy as np

D_MODEL = 1024
BATCH = 8
SEQ = 4096
DEPTH = 2

GRID_W = 64
CTX_LEN = 256
N_EVEN = (DEPTH + 1) // 2
N_ODD = DEPTH // 2
DN_ALPHA = (2 * DEPTH) ** 0.25
DN_BETA = (8 * DEPTH) ** -0.25
LN_EPS = 1e-5
F32 = jnp.float32

GLA_HEADS = 4
GLA_DK = 64
GLA_DV = 128
GLA_QK = GLA_HEADS * GLA_DK
GLA_V = GLA_HEADS * GLA_DV
GLA_GATE_RANK = 16
GLA_GATE_NORM = 16.0
CHUNK = 64
HG_HEADS = 4
HG_EXPAND = 128
HG_W = HG_HEADS * HG_EXPAND
EVEN_COLS = (GLA_QK, GLA_QK, GLA_V, GLA_V, GLA_GATE_RANK, GLA_GATE_RANK, HG_W, HG_W, HG_W, HG_W, HG_W)
EVEN_WIDTH = sum(EVEN_COLS)
HY_CH = 512
HY_ORDER = 2
HY_WIDTH = (HY_ORDER + 1) * HY_CH
HY_SHORT = 3
HY_EMB = 33
HY_FILTER_HIDDEN = 64
HY_FAST_DECAY = 0.3
HY_SLOW_DECAY = 1.5
HY_TARGET = 1e-2
RW_HEADS = 8
RW_HEAD_DIM = 64
RW_W = RW_HEADS * RW_HEAD_DIM
RW_DECAY_LORA = 64
RW_AAA_LORA = 64
RW_GATE_LORA = 128
RW_GN_EPS = 64e-5
RW_COLS = (RW_W, RW_W, RW_W, RW_DECAY_LORA, RW_DECAY_LORA, RW_AAA_LORA, RW_GATE_LORA)
RW_WIDTH = sum(RW_COLS)
ODD_WIDTH = HY_WIDTH + RW_WIDTH
N_EXPERTS = 256
TOP_K = 8
N_GROUPS = 8
TOPK_GROUPS = 4
EXPERT_FF = 256
SHARED_FF = 256
ROUTED_SCALE = 2.5
MOE_BLOCK = 128

kernel_name = 'hybrid_gla_hgrn2_hyena_rwkv7_moe_dit'


def split_cols(t, sizes):
    return jnp.split(t, np.cumsum(sizes)[:-1].tolist(), axis=-1)


def layer_norm(x, g, b, eps=LN_EPS):
    xf = x.astype(F32)
    mu = xf.mean(-1, keepdims=True)
    var = jnp.square(xf - mu).mean(-1, keepdims=True)
    return ((xf - mu) * lax.rsqrt(var + eps)).astype(x.dtype) * g + b


def rms_norm(x, g, eps=1e-6):
    xf = x.astype(F32)
    return (xf * lax.rsqrt(jnp.mean(xf * xf, -1, keepdims=True) + eps)).astype(x.dtype) * g


def to_heads(t, n_heads):
    B, L, _ = t.shape
    return t.reshape(B, L, n_heads, -1).transpose(0, 2, 1, 3)


def from_heads(t):
    B, H, L, d = t.shape
    return t.transpose(0, 2, 1, 3).reshape(B, L, H * d)


def stack_dirs(t_fwd, t_bwd, axis):
    return jnp.stack([t_fwd, jnp.flip(t_bwd, axis)], 0)


def merge_dirs(o, axis):
    return o[0] + jnp.flip(o[1], axis)


def chunked_gated_recurrence(q, k, v, log_a, s0):
    L = q.shape[-2]
    nc = L // CHUNK

    def blocks(t):
        t = t.astype(F32)
        return jnp.moveaxis(t.reshape(t.shape[:-2] + (nc, CHUNK, t.shape[-1])), -3, 0)

    qc, kc, vc, ac = blocks(q), blocks(k), blocks(v), blocks(log_a)
    b = jnp.cumsum(ac, axis=-2)
    b_ref = b[..., CHUNK // 2:CHUNK // 2 + 1, :]
    b_last = b[..., -1:, :]
    scores = jnp.einsum('n...id,n...jd->n...ij', qc * jnp.exp(b - b_ref), kc * jnp.exp(b_ref - b))
    lower = jnp.tril(jnp.ones((CHUNK, CHUNK), bool))
    o_intra = jnp.einsum('n...ij,n...jv->n...iv', jnp.where(lower, scores, 0.0), vc)
    q_in = qc * jnp.exp(b)
    k_out = kc * jnp.exp(b_last - b)
    dec = jnp.exp(b_last[..., 0, :])

    def step(S, xs):
        q_t, k_t, v_t, d_t = xs
        o = jnp.einsum('...id,...dv->...iv', q_t, S)
        S = S * d_t[..., :, None] + jnp.einsum('...jd,...jv->...dv', k_t, v_t)
        return S, o

    S_fin, o_inter = lax.scan(step, s0.astype(F32), (q_in, k_out, vc, dec))
    o = jnp.moveaxis(o_intra + o_inter, 0, -3)
    return o.reshape(o.shape[:-3] + (L, o.shape[-1])), S_fin


def bidir_chunked(ctx_in, lat_in):
    def stacked(q, k_f, k_b, v, la_f, la_b):
        return (stack_dirs(q, q, -2), stack_dirs(k_f, k_b, -2), stack_dirs(v, v, -2), stack_dirs(la_f, la_b, -2))

    qc, kc, vc, lac = stacked(*ctx_in)
    s0 = jnp.zeros(kc.shape[:-2] + (kc.shape[-1], vc.shape[-1]), F32)
    o_c, s_c = chunked_gated_recurrence(qc, kc, vc, lac, s0)
    o_l, _ = chunked_gated_recurrence(*stacked(*lat_in), s_c)
    return merge_dirs(o_c, -2), merge_dirs(o_l, -2)


def gla_hgrn_mixer(h, hc, w_in, w_out, gate_w2, gate_b, gla_g, lb, hg_g):
    def prep(hh):
        q, k, v, r, a_f, a_b, hq, hf_f, hf_b, hi, hgate = split_cols(hh @ w_in, EVEN_COLS)
        la = [jax.nn.log_sigmoid((a @ gate_w2[d] + gate_b[d]).astype(F32)) / GLA_GATE_NORM
              for d, a in enumerate((a_f, a_b))]
        gla = (to_heads(q * GLA_DK ** -0.5, GLA_HEADS), to_heads(k, GLA_HEADS), to_heads(k, GLA_HEADS),
               to_heads(v, GLA_HEADS), to_heads(la[0], GLA_HEADS), to_heads(la[1], GLA_HEADS))
        f = [lb[d] + (1.0 - lb[d]) * jax.nn.sigmoid(z.astype(F32)) for d, z in enumerate((hf_f, hf_b))]
        hg = (to_heads(jax.nn.silu(hq), HG_HEADS), to_heads(1.0 - f[0], HG_HEADS), to_heads(1.0 - f[1], HG_HEADS),
              to_heads(hi, HG_HEADS), to_heads(jnp.log(f[0]), HG_HEADS), to_heads(jnp.log(f[1]), HG_HEADS))
        return gla, hg, r, hgate

    gla_c, hg_c, r_c, gt_c = prep(hc)
    gla_l, hg_l, r_l, gt_l = prep(h)
    o_gla_c, o_gla_l = bidir_chunked(gla_c, gla_l)
    o_hg_c, o_hg_l = bidir_chunked(hg_c, hg_l)

    def readout(o_gla, o_hg, r, gt, dtype):
        y = jnp.concatenate([from_heads(rms_norm(o_gla, gla_g)) * jax.nn.silu(r),
                             from_heads(rms_norm(o_hg, hg_g)) * jax.nn.silu(gt)], -1)
        return (y @ w_out).astype(dtype)

    return readout(o_gla_l, o_hg_l, r_l, gt_l, h.dtype), readout(o_gla_c, o_hg_c, r_c, gt_c, hc.dtype)


def hyena_filters(L, w1, b1, w2, b2, w3, sin_freq):
    t = jnp.linspace(0.0, 1.0, L, dtype=F32)[:, None]
    bands = (HY_EMB - 1) // 2
    wpos = 2.0 * math.pi * jnp.arange(L, dtype=F32)[:, None] / L
    fr = jnp.linspace(1e-4, bands - 1, bands, dtype=F32)[None, :]
    z = jnp.concatenate([t, jnp.cos(fr * wpos), -jnp.sin(fr * wpos)], -1)
    hdn = jnp.sin(sin_freq[0] * (z @ w1 + b1))
    hdn = jnp.sin(sin_freq[1] * (hdn @ w2 + b2))
    filt = (hdn @ w3).reshape(L, HY_ORDER, 2, HY_CH)
    max_decay = math.log(HY_TARGET) / HY_FAST_DECAY
    min_decay = math.log(HY_TARGET) / HY_SLOW_DECAY
    deltas = jnp.linspace(min_decay, max_decay, HY_CH, dtype=F32)
    window = jnp.exp(-t * jnp.abs(deltas))
    return filt * window[:, None, None, :]


def centred_long_conv(u, h_fwd, h_bwd):
    L = u.shape[1]
    filt_full = jnp.concatenate([h_fwd, jnp.zeros_like(h_fwd[:1]), jnp.flip(h_bwd[1:], 0)], 0)
    uf = jnp.fft.rfft(u.astype(F32), n=2 * L, axis=1)
    ff = jnp.fft.rfft(filt_full.astype(F32), n=2 * L, axis=0)
    return jnp.fft.irfft(uf * ff[None], n=2 * L, axis=1)[:, :L].astype(u.dtype)


def hyena(P, conv_w, conv_b, f_w1, f_b1, f_w2, f_b2, f_w3, sin_freq, hy_bias):
    L = P.shape[1]
    u = lax.conv_general_dilated(P, conv_w[:, None, :], window_strides=(1,),
                                 padding=[(HY_SHORT // 2, HY_SHORT // 2)],
                                 dimension_numbers=('NWC', 'WIO', 'NWC'),
                                 feature_group_count=HY_WIDTH) + conv_b
    v, x1, x2 = jnp.split(u, HY_ORDER + 1, axis=-1)
    filt = hyena_filters(L, f_w1, f_b1, f_w2, f_b2, f_w3, sin_freq)
    z = v
    for n, gate in enumerate((x1, x2)):
        z = gate * (centred_long_conv(z, filt[:, n, 0], filt[:, n, 1]) + z * hy_bias[n])
    return z


def grid_shift(t):
    B, L, C = t.shape
    rows = L // GRID_W
    g = jnp.pad(t.reshape(B, rows, GRID_W, C), ((0, 0), (1, 1), (1, 1), (0, 0)))
    g = g.reshape(B, rows + 2, GRID_W + 2, C // 4, 4)
    out = jnp.stack([g[:, 1:-1, :-2, :, 0], g[:, 1:-1, 2:, :, 1],
                     g[:, :-2, 1:-1, :, 2], g[:, 2:, 1:-1, :, 3]], -1)
    return out.reshape(B, L, C)


def seq_shift(t):
    B, L, C = t.shape
    p = jnp.pad(t, ((0, 0), (1, 1), (0, 0))).reshape(B, L + 2, C // 2, 2)
    return jnp.stack([p[:, :-2, :, 0], p[:, 2:, :, 1]], -1).reshape(B, L, C)


def rwkv_streams(P, shift_fn, mu, w0, w2, a0, a2, g2, k_k, k_a):
    P = P + mu * (shift_fn(P) - P)
    r, k, v, wl_f, wl_b, al, gl = split_cols(P, RW_COLS)

    def heads(t):
        return t.reshape(t.shape[:-1] + (RW_HEADS, RW_HEAD_DIM))

    def log_decay(wl, d):
        w = -jax.nn.softplus(-(w0[d] + jnp.tanh(wl) @ w2[d]).astype(F32)) - 0.5
        return heads(-jnp.exp(w))

    a = jax.nn.sigmoid(a0 + al @ a2)
    g = jax.nn.sigmoid(gl) @ g2
    kk = heads(k * k_k).astype(F32)
    kk = kk / jnp.maximum(jnp.linalg.norm(kk, axis=-1, keepdims=True), 1e-12)
    k = k * (1.0 + (a - 1.0) * k_a)
    return (heads(r), heads(k), heads(v), kk, heads(a), g, log_decay(wl_f, 0), log_decay(wl_b, 1))


def rwkv_bidir_scan(st, s0, emit):
    r, k, v, kk, a, g, ld_f, ld_b = st

    def tm(t_f, t_b=None):
        t_b = t_f if t_b is None else t_b
        return jnp.moveaxis(stack_dirs(t_f, t_b, 1), 2, 0).astype(F32)

    xs = (tm(jnp.exp(ld_f), jnp.exp(ld_b)), tm(k), tm(v), tm(kk), tm(kk * a)) + ((tm(r),) if emit else ())

    def step(S, xt):
        d_t, k_t, v_t, kk_t, b_t = xt[:5]
        S = (S * d_t[..., None, :]
             - jnp.einsum('...vk,...k->...v', S, kk_t)[..., None] * b_t[..., None, :]
             + v_t[..., :, None] * k_t[..., None, :])
        return S, (jnp.einsum('...vk,...k->...v', S, xt[5]) if emit else None)

    S, o = lax.scan(step, s0, xs)
    if emit:
        o = merge_dirs(jnp.moveaxis(o, 0, 2), 1)
    return S, o


def rwkv_readout(o, st, r_k, ln_g, ln_b):
    r, k, v, kk, a, g, _, _ = st
    B, L = o.shape[:2]
    on = layer_norm(o, ln_g, ln_b, RW_GN_EPS)
    bonus = jnp.sum(r * k * r_k, -1, keepdims=True) * v
    return (on + bonus).reshape(B, L, RW_W) * g


def hyena_rwkv_mixer(h, hc, need_ctx_out, w_in, w_out, conv_w, conv_b, f_w1, f_b1, f_w2, f_b2, f_w3,
                     sin_freq, hy_bias, mu, w0, w2, a0, a2, g2, k_k, k_a, r_k, ln_g, ln_b):
    w_hy, w_rw = w_in[:, :HY_WIDTH], w_in[:, HY_WIDTH:]
    rw_args = (mu, w0, w2, a0, a2, g2, k_k, k_a)
    st_c = rwkv_streams(hc @ w_rw, seq_shift, *rw_args)
    st_l = rwkv_streams(h @ w_rw, grid_shift, *rw_args)
    s0 = jnp.zeros((2, h.shape[0], RW_HEADS, RW_HEAD_DIM, RW_HEAD_DIM), F32)
    s_c, o_c = rwkv_bidir_scan(st_c, s0, need_ctx_out)
    _, o_l = rwkv_bidir_scan(st_l, s_c, True)
    hy_args = (conv_w, conv_b, f_w1, f_b1, f_w2, f_b2, f_w3, sin_freq, hy_bias)

    def readout(hh, st, o):
        y = jnp.concatenate([hyena(hh @ w_hy, *hy_args), rwkv_readout(o, st, r_k, ln_g, ln_b)], -1)
        return (y @ w_out).astype(hh.dtype)

    y_c = readout(hc, st_c, o_c) if need_ctx_out else None
    return readout(h, st_l, o_l), y_c


def routed_experts(u, eidx, gate, w13, w2):
    T, D = u.shape
    N = T * TOP_K
    flat_e = eidx.reshape(N)
    order = jnp.argsort(flat_e)
    sorted_e = flat_e[order]
    counts = jnp.bincount(flat_e, length=N_EXPERTS)
    padded = (counts + MOE_BLOCK - 1) // MOE_BLOCK * MOE_BLOCK
    pad_end = jnp.cumsum(padded)
    pad_start = pad_end - padded
    cnt_start = jnp.cumsum(counts) - counts
    dest = pad_start[sorted_e] + jnp.arange(N) - cnt_start[sorted_e]
    n_blocks = -(-N // MOE_BLOCK) + N_EXPERTS
    slot_tok = jnp.full((n_blocks * MOE_BLOCK,), T, jnp.int32).at[dest].set((order // TOP_K).astype(jnp.int32))
    slot_gate = jnp.zeros((n_blocks * MOE_BLOCK,), u.dtype).at[dest].set(gate.reshape(N)[order])
    blk_exp = jnp.minimum(jnp.searchsorted(pad_end, jnp.arange(n_blocks) * MOE_BLOCK, side='right'), N_EXPERTS - 1)
    u_pad = jnp.concatenate([u, jnp.zeros((1, D), u.dtype)], 0)

    def body(acc, xs):
        tok, gt, e = xs
        h13 = u_pad[tok] @ w13[e]
        yb = (jax.nn.silu(h13[:, :EXPERT_FF]) * h13[:, EXPERT_FF:]) @ w2[e]
        return acc.at[tok].add(yb * gt[:, None]), None

    acc, _ = lax.scan(body, jnp.zeros((T + 1, D), u.dtype),
                      (slot_tok.reshape(n_blocks, MOE_BLOCK), slot_gate.reshape(n_blocks, MOE_BLOCK), blk_exp))
    return acc[:T]


def moe(u, router_w, router_bias, exp_w13, exp_w2, sh_w13, sh_w2):
    T = u.shape[0]
    per_group = N_EXPERTS // N_GROUPS
    scores = jax.nn.sigmoid((u @ router_w).astype(F32))
    sel = scores + router_bias.astype(F32)
    grp_score = lax.top_k(sel.reshape(T, N_GROUPS, per_group), 2)[0].sum(-1)
    _, grp_idx = lax.top_k(grp_score, TOPK_GROUPS)
    grp_mask = jax.nn.one_hot(grp_idx, N_GROUPS, dtype=F32).sum(1) > 0
    sel = jnp.where(jnp.repeat(grp_mask, per_group, axis=1), sel, -jnp.inf)
    _, eidx = lax.top_k(sel, TOP_K)
    gate = jnp.take_along_axis(scores, eidx, 1)
    gate = gate / gate.sum(-1, keepdims=True) * ROUTED_SCALE
    routed = routed_experts(u, eidx, gate.astype(u.dtype), exp_w13, exp_w2)
    hs = u @ sh_w13
    shared = (jax.nn.silu(hs[:, :SHARED_FF]) * hs[:, SHARED_FF:]) @ sh_w2
    return routed + shared


def setup_inputs(seed: int = 0) -> dict:
    key = jax.random.key(seed)
    ks = iter(jax.random.split(key, 64))
    D = D_MODEL

    def nrm(shape, scale):
        return jax.random.normal(next(ks), shape, jnp.float32) * scale

    def gain(shape):
        return 1.0 + nrm(shape, 0.02)

    return {
        'x': nrm((BATCH, SEQ, D), 1.0),
        'c': nrm((BATCH, D), 1.0),
        'ctx': nrm((BATCH, CTX_LEN, D), 1.0),
        'c_ctx': nrm((D,), 1.0),
        'mod_w': nrm((DEPTH, D, 6 * D), 0.5 * D ** -0.5),
        'mod_b': nrm((DEPTH, 6 * D), 0.02),
        'ln1_g': gain((DEPTH, D)),
        'ln1_b': nrm((DEPTH, D), 0.02),
        'ln2_g': gain((DEPTH, D)),
        'ln2_b': nrm((DEPTH, D), 0.02),
        'ev_w_in': nrm((N_EVEN, D, EVEN_WIDTH), D ** -0.5),
        'ev_w_out': nrm((N_EVEN, GLA_V + HG_W, D), (GLA_V + HG_W) ** -0.5 * DN_BETA),
        'gla_gate_w2': nrm((N_EVEN, 2, GLA_GATE_RANK, GLA_QK), GLA_GATE_RANK ** -0.5),
        'gla_gate_b': nrm((N_EVEN, 2, GLA_QK), 0.1),
        'gla_norm_g': gain((N_EVEN, GLA_DV)),
        'hg_lb_logits': nrm((N_EVEN + 1, 2, HG_W), 0.1),
        'hg_norm_g': gain((N_EVEN, HG_EXPAND)),
        'od_w_in': nrm((N_ODD, D, ODD_WIDTH), D ** -0.5),
        'od_w_out': nrm((N_ODD, HY_CH + RW_W, D), (HY_CH + RW_W) ** -0.5 * DN_BETA),
        'hy_conv_w': nrm((N_ODD, HY_SHORT, HY_WIDTH), HY_SHORT ** -0.5),
        'hy_conv_b': nrm((N_ODD, HY_WIDTH), 0.02),
        'hy_ffn_w1': nrm((N_ODD, HY_EMB, HY_FILTER_HIDDEN), HY_EMB ** -0.5),
        'hy_ffn_b1': nrm((N_ODD, HY_FILTER_HIDDEN), 0.02),
        'hy_ffn_w2': nrm((N_ODD, HY_FILTER_HIDDEN, HY_FILTER_HIDDEN), HY_FILTER_HIDDEN ** -0.5),
        'hy_ffn_b2': nrm((N_ODD, HY_FILTER_HIDDEN), 0.02),
        'hy_ffn_w3': nrm((N_ODD, HY_FILTER_HIDDEN, HY_ORDER * 2 * HY_CH), 0.1 * HY_FILTER_HIDDEN ** -0.5),
        'hy_sin_freq': 1.0 + nrm((N_ODD, 2, HY_FILTER_HIDDEN), 0.1),
        'hy_bias': nrm((N_ODD, HY_ORDER, HY_CH), 0.5),
        'rw_mu': jax.random.uniform(next(ks), (N_ODD, RW_WIDTH), jnp.float32),
        'rw_w0': jax.random.uniform(next(ks), (N_ODD, 2, RW_W), jnp.float32, -6.5, -1.5),
        'rw_w2': nrm((N_ODD, 2, RW_DECAY_LORA, RW_W), 0.1 * RW_DECAY_LORA ** -0.5),
        'rw_a0': nrm((N_ODD, RW_W), 0.1),
        'rw_a2': nrm((N_ODD, RW_AAA_LORA, RW_W), 0.1 * RW_AAA_LORA ** -0.5),
        'rw_g2': nrm((N_ODD, RW_GATE_LORA, RW_W), RW_GATE_LORA ** -0.5),
        'rw_k_k': 0.85 + nrm((N_ODD, RW_W), 0.05),
        'rw_k_a': 1.0 + nrm((N_ODD, RW_W), 0.05),
        'rw_r_k': nrm((N_ODD, RW_HEADS, RW_HEAD_DIM), 0.1),
        'rw_ln_g': gain((N_ODD, RW_HEADS, RW_HEAD_DIM)),
        'rw_ln_b': nrm((N_ODD, RW_HEADS, RW_HEAD_DIM), 0.02),
        'router_w': nrm((DEPTH, D, N_EXPERTS), D ** -0.5),
        'router_bias': nrm((DEPTH, N_EXPERTS), 0.01),
        'exp_w13': nrm((DEPTH, N_EXPERTS, D, 2 * EXPERT_FF), D ** -0.5),
        'exp_w2': nrm((DEPTH, N_EXPERTS, EXPERT_FF, D), EXPERT_FF ** -0.5 * DN_BETA),
        'sh_w13': nrm((DEPTH, D, 2 * SHARED_FF), D ** -0.5),
        'sh_w2': nrm((DEPTH, SHARED_FF, D), SHARED_FF ** -0.5 * DN_BETA),
    }


def reference(x, c, ctx, c_ctx, mod_w, mod_b, ln1_g, ln1_b, ln2_g, ln2_b,
              ev_w_in, ev_w_out, gla_gate_w2, gla_gate_b, gla_norm_g, hg_lb_logits, hg_norm_g,
              od_w_in, od_w_out, hy_conv_w, hy_conv_b, hy_ffn_w1, hy_ffn_b1, hy_ffn_w2, hy_ffn_b2,
              hy_ffn_w3, hy_sin_freq, hy_bias, rw_mu, rw_w0, rw_w2, rw_a0, rw_a2, rw_g2, rw_k_k,
              rw_k_a, rw_r_k, rw_ln_g, rw_ln_b, router_w, router_bias, exp_w13, exp_w2, sh_w13, sh_w2):
    B, L, D = x.shape
    Lc = ctx.shape[1]
    hg_lb = jnp.cumsum(jax.nn.softmax(hg_lb_logits.astype(F32), axis=0), axis=0)
    xl, xc = x, ctx
    for l in range(DEPTH):
        last = l == DEPTH - 1
        j = l // 2
        m = jax.nn.silu(c) @ mod_w[l] + mod_b[l]
        mc = jax.nn.silu(c_ctx) @ mod_w[l] + mod_b[l]
        sh1, sc1, g1, sh2, sc2, g2 = jnp.split(m[:, None, :], 6, -1)
        sh1c, sc1c, g1c, sh2c, sc2c, g2c = jnp.split(mc, 6, -1)
        h = xl * (1.0 + sc1) + sh1
        hc = xc * (1.0 + sc1c) + sh1c
        if l % 2 == 0:
            y, yc = gla_hgrn_mixer(h, hc, ev_w_in[j], ev_w_out[j], gla_gate_w2[j], gla_gate_b[j],
                                   gla_norm_g[j], hg_lb[j], hg_norm_g[j])
        else:
            y, yc = hyena_rwkv_mixer(h, hc, not last, od_w_in[j], od_w_out[j], hy_conv_w[j], hy_conv_b[j],
                                     hy_ffn_w1[j], hy_ffn_b1[j], hy_ffn_w2[j], hy_ffn_b2[j], hy_ffn_w3[j],
                                     hy_sin_freq[j], hy_bias[j], rw_mu[j], rw_w0[j], rw_w2[j], rw_a0[j],
                                     rw_a2[j], rw_g2[j], rw_k_k[j], rw_k_a[j], rw_r_k[j], rw_ln_g[j], rw_ln_b[j])
        xl = layer_norm(DN_ALPHA * xl + g1 * y, ln1_g[l], ln1_b[l])
        moe_p = (router_w[l], router_bias[l], exp_w13[l], exp_w2[l], sh_w13[l], sh_w2[l])
        u_l = xl * (1.0 + sc2) + sh2
        if last:
            mo_l = moe(u_l.reshape(B * L, D), *moe_p).reshape(B, L, D)
        else:
            xc = layer_norm(DN_ALPHA * xc + g1c * yc, ln1_g[l], ln1_b[l])
            u_c = xc * (1.0 + sc2c) + sh2c
            mo = moe(jnp.concatenate([u_c.reshape(B * Lc, D), u_l.reshape(B * L, D)], 0), *moe_p)
            mo_l = mo[B * Lc:].reshape(B, L, D)
            xc = layer_norm(DN_ALPHA * xc + g2c * mo[:B * Lc].reshape(B, Lc, D), ln2_g[l], ln2_b[l])
        xl = layer_norm(DN_ALPHA * xl + g2 * mo_l, ln2_g[l], ln2_b[l])
    return xl
```

```python
import numpy as np
from contextlib import ExitStack
import concourse.bass as bass
import concourse.mybir as mybir
from concourse.bass_utils import run_bass_kernel_spmd

F32 = mybir.dt.float32
I32 = mybir.dt.int32
AF = mybir.ActivationFunctionType
ALU = mybir.AluOpType
AX = mybir.AxisListType

ENGS = ["tensor", "vector", "scalar", "gpsimd", "sync"]
NDS = 40


class Dep:
    __slots__ = ("w", "r")

    def __init__(self):
        self.w = None
        self.r = {}


class V:
    __slots__ = ("ap", "dep")

    def __init__(self, ap, dep):
        self.ap = ap
        self.dep = dep

    def __getitem__(self, idx):
        return V(self.ap[idx], self.dep)


class T:
    def __init__(self, h, tracked=True):
        self.h = h
        self.dep = Dep() if tracked else None

    def __getitem__(self, idx):
        return V(self.h[idx], self.dep)

    def v(self, ap):
        return V(ap, self.dep)


class Prog:
    def __init__(self):
        self.nc = bass.Bass("TRN2", target_bir_lowering=False)
        self.es = ExitStack()
        self.streams = {e: [] for e in ENGS}
        self.cnt = {e: 0 for e in ENGS}
        self.sem = {e: self.es.enter_context(self.nc.semaphore("s_" + e)) for e in ENGS}
        self.known = {e: {} for e in ENGS}
        self.dsem = [self.es.enter_context(self.nc.semaphore("d%d" % i)) for i in range(NDS)]
        self.dcnt = [0] * NDS
        self.dnext = 0
        self.nalloc = 0
        self.out_events = []
        self.arena = None
        self.banks = None
        self.aoff = 0
        self.nps = 0

    ARENA = 53200

    def use_arena(self):
        self.arena = self.es.enter_context(self.nc.sbuf_tensor("arena", [128, self.ARENA], F32))
        self.banks = [self.es.enter_context(self.nc.psum_tensor("bank%d" % i, [128, 512], F32)) for i in range(8)]

    def begin_phase(self):
        self.aoff = 0
        self.nps = 0

    def barrier(self):
        evs = [(e, self.cnt[e]) for e in ENGS if self.cnt[e] > 0]
        evs += [(sl, 16 * self.dcnt[sl]) for sl in range(NDS) if self.dcnt[sl] > 0]
        for e in ENGS:
            waits = []
            for k, v in evs:
                if self.known[e].get(k, 0) < v:
                    self.known[e][k] = v
                    waits.append((self._semof(k), v))
            self.streams[e].append((waits, None, None, 0))

    def end_phase(self):
        self.barrier()

    def _nm(self, name):
        self.nalloc += 1
        return "%s_%d" % (name, self.nalloc)

    def sb(self, name, shape, dt=F32):
        if self.arena is None:
            return T(self.es.enter_context(self.nc.sbuf_tensor(self._nm(name), list(shape), dt)))
        n = 1
        for d_ in shape[1:]:
            n *= d_
        n8 = (n + 7) // 8 * 8
        assert self.aoff + n8 <= self.ARENA, "arena overflow at %s: %d + %d" % (name, self.aoff, n8)
        ap = self.arena[0:shape[0], self.aoff:self.aoff + n]
        self.aoff += n8
        if dt != F32:
            ap = ap.bitcast(dt)
        if len(shape) == 3:
            ap = ap.rearrange("p (a b) -> p a b", b=shape[2])
        return T(ap)

    def ps(self, name, shape, dt=F32):
        if self.banks is None:
            return T(self.es.enter_context(self.nc.psum_tensor(self._nm(name), list(shape), dt)))
        b = self.banks[self.nps]
        self.nps += 1
        return T(b[:, :])

    def dram(self, name, shape, dt=F32, kind="Internal"):
        t = self.nc.dram_tensor(name, list(shape), dt, kind=kind)
        return T(t.ap(), tracked=(kind != "ExternalInput"))

    def _semof(self, key):
        return self.sem[key] if isinstance(key, str) else self.dsem[key]

    def _waits(self, eng, reads, writes):
        need = {}
        for d in reads:
            if d is not None and d.w is not None:
                k, v = d.w
                need[k] = max(need.get(k, 0), v)
        for d in writes:
            if d is None:
                continue
            if d.w is not None:
                k, v = d.w
                need[k] = max(need.get(k, 0), v)
            for k, v in d.r.items():
                need[k] = max(need.get(k, 0), v)
        out = []
        kn = self.known[eng]
        for k, v in need.items():
            if k == eng and eng == "tensor":
                continue
            if kn.get(k, 0) >= v:
                continue
            kn[k] = v
            out.append((self._semof(k), v))
        return out

    def _record(self, ev, reads, writes):
        k, v = ev
        for d in reads:
            if d is not None:
                d.r[k] = max(d.r.get(k, 0), v)
        for d in writes:
            if d is not None:
                d.w = ev
                d.r = {}

    def op(self, eng, fn, outs, ins):
        reads = [x.dep for x in ins if isinstance(x, V)]
        writes = [x.dep for x in outs]
        waits = self._waits(eng, reads, writes)
        self.cnt[eng] += 1
        ev = (eng, self.cnt[eng])
        self.streams[eng].append((waits, fn, self.sem[eng], 1))
        self._record(ev, reads, writes)
        return ev

    def dma(self, out, in_, eng="sync", fn=None, extra_reads=()):
        slot = self.dnext
        self.dnext = (self.dnext + 1) % NDS
        reads = [in_.dep] + [x.dep for x in extra_reads]
        writes = [out.dep]
        waits = self._waits(eng, reads, writes)
        if self.dcnt[slot] > 0:
            pv = 16 * self.dcnt[slot]
            if self.known[eng].get(slot, 0) < pv:
                self.known[eng][slot] = pv
                waits.append((self.dsem[slot], pv))
        self.dcnt[slot] += 1
        ev = (slot, 16 * self.dcnt[slot])
        if fn is None:
            o, i = out.ap, in_.ap
            fn = lambda e, o=o, i=i: e.dma_start(out=o, in_=i)
        self.streams[eng].append((waits, fn, self.dsem[slot], 16))
        self._record(ev, reads, writes)
        return ev

    def finish(self, final_deps):
        waits = self._waits("sync", [d for d in final_deps], [])
        self.streams["sync"].append((waits, None, None, 0))
        with self.nc.Block() as block:
            for e in ENGS:
                stream = self.streams[e]

                def body(engh, stream=stream):
                    for waits, fn, sem, inc in stream:
                        for (s, v) in waits:
                            engh.wait_ge(s, v)
                        if fn is not None:
                            ins = fn(engh)
                            ins.then_inc(sem, inc)

                getattr(block, e)(body)
        self.es.close()
        return self.nc

    def mm(self, out, lhsT, rhs, start=True, stop=True):
        o, a, b = out.ap, lhsT.ap, rhs.ap
        return self.op("tensor", lambda e: e.matmul(o, a, b, start=start, stop=stop), [out], [lhsT, rhs])

    def tr(self, out, in_, ident):
        o, a, b = out.ap, in_.ap, ident.ap
        return self.op("tensor", lambda e: e.transpose(o, a, b), [out], [in_, ident])

    def act(self, out, in_, func, bias=0.0, scale=1.0, eng="scalar", accum_out=None):
        o, a = out.ap, in_.ap
        bb = bias.ap if isinstance(bias, V) else bias
        ss = scale.ap if isinstance(scale, V) else scale
        outs = [out]
        kw = {}
        if accum_out is not None:
            kw["accum_out"] = accum_out.ap
            outs.append(accum_out)
        return self.op("scalar", lambda e: e.activation(o, a, func, bias=bb, scale=ss, **kw), outs, [in_, bias, scale])

    def tt(self, out, a, b, op, eng="vector"):
        o, x, y = out.ap, a.ap, b.ap
        return self.op(eng, lambda e: e.tensor_tensor(o, x, y, op), [out], [a, b])

    def ts(self, out, a, s1, op0, s2=None, op1=None, eng="vector", accum_out=None):
        o, x = out.ap, a.ap
        c1 = s1.ap if isinstance(s1, V) else s1
        c2 = s2.ap if isinstance(s2, V) else s2
        outs = [out]
        kw = {}
        if op1 is not None:
            kw["op1"] = op1
        if accum_out is not None:
            kw["accum_out"] = accum_out.ap
            outs.append(accum_out)
        return self.op(eng, lambda e: e.tensor_scalar(o, x, c1, c2, op0, **kw), outs, [a, s1, s2])

    def stt(self, out, a, s, b, op0, op1, eng="vector"):
        o, x, y = out.ap, a.ap, b.ap
        c = s.ap if isinstance(s, V) else s
        return self.op(eng, lambda e: e.scalar_tensor_tensor(o, x, c, y, op0, op1), [out], [a, s, b])

    def copy(self, out, in_, eng="vector"):
        o, a = out.ap, in_.ap
        if eng == "scalar":
            return self.op(eng, lambda e: e.copy(o, a), [out], [in_])
        return self.op(eng, lambda e: e.tensor_copy(o, a), [out], [in_])

    def memset(self, out, val, eng="vector"):
        o = out.ap
        return self.op(eng, lambda e: e.memset(o, val), [out], [])

    def reduce(self, out, in_, op, axis=None, eng="vector"):
        o, a = out.ap, in_.ap
        ax = AX.X if axis is None else axis
        return self.op(eng, lambda e: e.tensor_reduce(o, a, ax, op), [out], [in_])

    def recip(self, out, in_):
        o, a = out.ap, in_.ap
        return self.op("vector", lambda e: e.reciprocal(o, a), [out], [in_])


D = 1024
ALPHA = 4.0 ** 0.25
NT = 34


def pbc(t, n=128):
    return V(t.h[0].partition_broadcast(n), t.dep)


def modulation(p, cT, mod_w, mod_b_bc, c0, ncols, out, wk, ps, scr):
    sc = scr["sc"]
    lh = scr["lh"]
    p.dma(sc[:], cT[:])
    p.act(sc[:], sc[:], AF.Silu)
    for k in range(8):
        p.copy(lh[:, k, :], V(sc.h[:, k:k + 1].to_broadcast([128, 128]), sc.dep))
    p.dma(out[:, 0:ncols], V(mod_b_bc.ap[:, c0:c0 + ncols], None))
    for n in range(ncols // 512):
        w = wk[n % 2]
        p.dma(w[:], V(mod_w.h[:, c0 + n * 512:c0 + (n + 1) * 512].rearrange("(k q) n -> q k n", q=128), None))
        pp = ps[n % 2]
        for k in range(8):
            p.mm(pp[:], lh[:, k, :], w[:, k, :], start=(k == 0), stop=(k == 7))
        p.tt(out[:, n * 512:(n + 1) * 512], pp[:], out[:, n * 512:(n + 1) * 512], ALU.add)


def layer_norm(p, out, in_, g_bc, b_bc, st, tmp, eps=1e-5, n=1024):
    p.reduce(st[:, 0:1], in_, ALU.add)
    p.ts(st[:, 1:2], st[:, 0:1], -1.0 / n, ALU.mult)
    p.act(tmp, in_, AF.Square, bias=st[:, 1:2], accum_out=st[:, 2:3])
    p.ts(st[:, 3:4], st[:, 2:3], 1.0 / n, ALU.mult, eps, ALU.add)
    p.act(st[:, 3:4], st[:, 3:4], AF.Sqrt)
    p.recip(st[:, 3:4], st[:, 3:4])
    p.ts(tmp, in_, st[:, 1:2], ALU.add, st[:, 3:4], ALU.mult)
    p.tt(tmp, tmp, g_bc, ALU.mult)
    p.tt(out, tmp, b_bc, ALU.add)


def transpose_tile(p, dstT, src, ncol, ident, pst, eng="vector"):
    nb = ncol // 128
    for g0 in range(0, nb, 4):
        g1 = min(nb, g0 + 4)
        for g in range(g0, g1):
            p.tr(pst[:, (g - g0) * 128:(g - g0 + 1) * 128], V(src.ap[:, g * 128:(g + 1) * 128], src.dep), ident[:])
        p.copy(V(dstT.h[:, g0:g1, :].rearrange("p a b -> p (a b)"), dstT.dep), pst[:, 0:(g1 - g0) * 128], eng=eng)


def consts_l0():
    i = np.arange(128)
    ch = i // 64
    same = (ch[:, None] == ch[None, :])
    triF = (same & (i[:, None] <= i[None, :])).astype(np.float32)
    triB = (same & (i[:, None] >= i[None, :])).astype(np.float32)
    refF = triF[:, ch * 64 + 32]
    lastF = same.astype(np.float32)
    refB = triB[:, ch * 64 + 31]
    cmF = np.concatenate([triF - refF, lastF - triF, triF], 1)
    cmB = np.concatenate([triB - refB, lastF - triB, triB], 1)
    ci = np.stack([(ch == 0), (ch == 1)], 1).astype(np.float32)
    return dict(ident=np.eye(128, dtype=np.float32), cmF=cmF.astype(np.float32), cmB=cmB.astype(np.float32), ci=ci)


def build_l0_mixer(p=None, io=None, pfx=""):
    standalone = p is None
    if standalone:
        p = Prog()
    io = io or {}
    p.begin_phase()
    IN = lambda n, s, dt=F32: io[n] if n in io else p.dram(pfx + n, s, dt, kind="ExternalInput")
    xin = IN("xin", [NT * 128, D])
    cT = IN("cT", [128, 8]); ccT = IN("ccT", [128, 8])
    mod_w = IN("mod_w", [D, 6 * D]); mod_b = IN("mod_b", [1, 6 * D])
    ln_g = IN("ln_g", [1, D]); ln_b = IN("ln_b", [1, D])
    w_in = IN("w_in", [D, 4128]); w_out = IN("w_out", [D, D])
    gw2 = IN("gw2", [32, 512])
    gb = IN("gb", [1, 512])
    gvec = IN("gvec", [1, D])
    lbl = IN("lbl", [1, 2048])
    ident_d = IN("ident", [128, 128]); cmF_d = IN("cmF", [128, 384]); cmB_d = IN("cmB", [128, 384]); ci_d = IN("ci", [128, 2])
    xout = io["xout"] if "xout" in io else p.dram("xout", [NT * 128, D], kind="ExternalOutput")
    SC = p.dram(pfx + "SC", [NT, 128, 4352])
    OF = p.dram(pfx + "OF", [NT, 128, D])
    sc_deps = [Dep() for _ in range(NT)]
    of_deps = [Dep() for _ in range(NT)]
    out_deps = [Dep() for _ in range(NT)]

    S = lambda n, s: p.sb(n, s)
    ident = S("ident", [128, 128]); cm = [S("cmF", [128, 384]), S("cmB", [128, 384])]; ci = S("ci", [128, 2])
    p.dma(ident[:], ident_d[:]); p.dma(cm[0][:], cmF_d[:]); p.dma(cm[1][:], cmB_d[:]); p.dma(ci[:], ci_d[:])
    modl = S("modl", [128, 3 * D]); modc = S("modc", [128, 3 * D])
    wk = [S("wk0", [128, 8, 512]), S("wk1", [128, 8, 512])]
    pA = [p.ps("pA0", [128, 512]), p.ps("pA1", [128, 512])]
    pT = p.ps("pT", [128, 512]); pS = p.ps("pS", [128, 512]); pO = [p.ps("pO0", [128, 512]), p.ps("pO1", [128, 512])]
    pKV = p.ps("pKV", [128, 512]); pD = p.ps("pD", [128, 512])
    scr = dict(sc=S("sc", [128, 8]), lh=S("lh", [128, 8, 128]))
    mbb = pbc(mod_b)
    modulation(p, cT, mod_w, mbb, 0, 3 * D, modl, wk, pA, scr)
    modulation(p, ccT, mod_w, mbb, 0, 3 * D, modc, wk, pA, scr)
    for m in (modl, modc):
        p.ts(m[:, D:2 * D], m[:, D:2 * D], 1.0, ALU.add)
    gbt = S("gbt", [128, 512]); p.dma(gbt[:], pbc(gb))
    gw2t = S("gw2t", [32, 512]); p.dma(gw2t[:], gw2[:])
    gv = S("gv", [128, D]); p.dma(gv[:], pbc(gvec))
    lng = S("lng", [128, D]); p.dma(lng[:], pbc(ln_g)); lnb = S("lnb", [128, D]); p.dma(lnb[:], pbc(ln_b))
    Z = S("Z", [128, 4128])
    p.dma(Z[:, 0:2048], pbc(lbl))
    oml = S("oml", [128, 1024])
    p.tt(oml[:], Z[:, 1024:2048], Z[:, 0:1024], ALU.subtract)
    p.act(oml[:], oml[:], AF.Sigmoid)

    xt = S("xt", [128, D]); hT = S("hT", [128, 8, 128])
    aT = S("aT", [32, 128])
    Q = S("Q", [128, 768]); Kd = [S("K0", [128, 768]), S("K1", [128, 768])]; LAd = [S("LA0", [128, 768]), S("LA1", [128, 768])]
    Vv = S("Vv", [128, D]); G = S("G", [128, D])
    E = [S("E%d" % i, [128, 768]) for i in range(3)]
    qp = S("qp", [128, 768]); kp = S("kp", [128, 768]); qi = S("qi", [128, 768]); ko = S("ko", [128, 768])
    qpT = S("qpT", [128, 6, 128]); kpT = S("kpT", [128, 6, 128]); qiT = S("qiT", [128, 6, 128])
    dec = S("dec", [128, 12]); PT = S("PT", [128, 128]); ot = S("ot", [128, D]); of = S("of", [128, D])
    Sst = [[S("S%d_%d" % (d, g), [128, 128]) for g in range(6)] for d in range(2)]
    st = S("st", [128, 16]); tmp = S("tmp", [128, D]); ht = tmp; yn = S("yn", [128, D]); ynT = S("ynT", [128, 8, 128])
    for d in range(2):
        for g in range(6):
            p.memset(Sst[d][g][:], 0.0)

    def gates(i):
        p.tr(pT[0:32, 0:128], Z[:, 1536:1568], ident[:])
        p.copy(aT[:], pT[0:32, 0:128])
        p.mm(pA[0][:], aT[:], gw2t[:])
        p.tt(E[0][:, 0:512], pA[0][:], gbt[:], ALU.add)
        p.act(E[0][:, 0:512], E[0][:, 0:512], AF.Exp, scale=-1.0)
        p.act(E[0][:, 0:512], E[0][:, 0:512], AF.Ln, bias=1.0)
        for d in range(2):
            p.ts(LAd[d][:, 0:256], E[0][:, d * 256:(d + 1) * 256], -1.0 / 16.0, ALU.mult)
            p.copy(Kd[d][:, 0:256], Z[:, 256:512], eng="gpsimd")
            p.act(Kd[d][:, 256:768], Z[:, 2080 + 512 * d:2592 + 512 * d], AF.Sigmoid, scale=-1.0)
            p.tt(Kd[d][:, 256:768], Kd[d][:, 256:768], oml[:, 512 * d:512 * (d + 1)], ALU.mult)
            p.act(LAd[d][:, 256:768], Kd[d][:, 256:768], AF.Ln, scale=-1.0, bias=1.0)
        p.ts(Q[:, 0:256], Z[:, 0:256], 0.125, ALU.mult)
        p.act(Q[:, 256:768], Z[:, 1568:2080], AF.Silu)
        p.copy(Vv[:, 0:512], Z[:, 512:1024], eng="gpsimd")
        p.copy(Vv[:, 512:1024], Z[:, 3104:3616], eng="gpsimd")
        p.act(G[:, 0:512], Z[:, 1024:1536], AF.Silu)
        p.act(G[:, 512:1024], Z[:, 3616:4128], AF.Silu)

    def recur(i, d):
        la, kk = LAd[d], Kd[d]
        for e in range(3):
            for (c0, c1) in ((0, 512), (512, 768)):
                pp = pA[(e + (c0 > 0)) % 2]
                p.mm(pp[:, 0:c1 - c0], cm[d][:, e * 128:(e + 1) * 128], la[:, c0:c1])
                p.copy(E[e][:, c0:c1], pp[:, 0:c1 - c0], eng="scalar")
        for g in range(6):
            p.mm(pD[:, g * 2:g * 2 + 2], la[:, g * 128:(g + 1) * 128], ci[:], start=True, stop=True)
        p.act(dec[:], pD[:, 0:12], AF.Exp)
        p.act(qp[:], E[0][:], AF.Exp); p.tt(qp[:], qp[:], Q[:], ALU.mult)
        p.act(kp[:], E[0][:], AF.Exp, scale=-1.0); p.tt(kp[:], kp[:], kk[:], ALU.mult)
        p.act(ko[:], E[1][:], AF.Exp); p.tt(ko[:], ko[:], kk[:], ALU.mult)
        p.act(qi[:], E[2][:], AF.Exp); p.tt(qi[:], qi[:], Q[:], ALU.mult)
        transpose_tile(p, qpT, qp[:], 768, ident, pT)
        transpose_tile(p, kpT, kp[:], 768, ident, pT)
        transpose_tile(p, qiT, qi[:], 768, ident, pT)
        chunks = (0, 1) if d == 0 else (1, 0)
        for hh in range(8):
            if hh < 4:
                g, base, dk = hh // 2, (hh % 2) * 64, 64
            else:
                g, base, dk = hh - 2, 0, 128
            vc = slice(hh * 128, (hh + 1) * 128)
            ob = pO[hh // 4]
            oc = slice((hh % 4) * 128, (hh % 4 + 1) * 128)
            rows = slice(base, base + dk)
            p.mm(pS[:, 0:128], kpT[rows, g, :], qpT[rows, g, :])
            p.tt(PT[:], pS[:, 0:128], cm[d][:, 256:384], ALU.mult)
            p.mm(ob[:, oc], PT[:], Vv[:, vc], start=True, stop=False)
            Sg = Sst[d][g]
            for n, c in enumerate(chunks):
                tk = slice(c * 64, (c + 1) * 64)
                p.mm(ob[tk, oc], qiT[rows, g, tk], Sg[rows, :], start=False, stop=(n == 1))
                kvs = slice((hh % 2) * 256 + n * 128, (hh % 2) * 256 + (n + 1) * 128)
                p.mm(pKV[rows, kvs], ko[tk, g * 128 + base:g * 128 + base + dk], Vv[tk, vc])
                p.stt(Sg[rows, :], Sg[rows, :], dec[rows, g * 2 + c:g * 2 + c + 1], pKV[rows, kvs], ALU.mult, ALU.add)

    order_f = list(range(NT))
    order_b = [1, 0] + list(range(NT - 1, 1, -1))
    for i in order_f:
        mod = modc if i < 2 else modl
        p.dma(xt[:], xin[i * 128:(i + 1) * 128, :])
        p.tt(ht[:], xt[:], mod[:, D:2 * D], ALU.mult)
        p.tt(ht[:], ht[:], mod[:, 0:D], ALU.add)
        transpose_tile(p, hT, ht[:], D, ident, pT)
        for n in range(9):
            c0 = n * 512; c1 = min(4128, c0 + 512); w = wk[n % 2]
            p.dma(V(w.h[:, :, 0:c1 - c0], w.dep), V(w_in.h[:, c0:c1].rearrange("(k q) n -> q k n", q=128), None))
            pp = pA[n % 2]
            for k in range(8):
                p.mm(pp[:, 0:c1 - c0], hT[:, k, :], V(w.h[:, k, 0:c1 - c0], w.dep), start=(k == 0), stop=(k == 7))
            p.copy(Z[:, c0:c1], pp[:, 0:c1 - c0], eng="scalar")
        gates(i)
        scd = sc_deps[i]
        p.dma(V(SC.h[i, :, 0:768], scd), Q[:]); p.dma(V(SC.h[i, :, 768:1536], scd), Kd[1][:])
        p.dma(V(SC.h[i, :, 1536:2304], scd), LAd[1][:]); p.dma(V(SC.h[i, :, 2304:3328], scd), Vv[:])
        p.dma(V(SC.h[i, :, 3328:4352], scd), G[:])
        recur(i, 0)
        p.copy(of[:, 0:512], pO[0][:], eng="scalar"); p.copy(of[:, 512:1024], pO[1][:], eng="scalar")
        p.dma(V(OF.h[i], of_deps[i]), of[:])
    for i in order_b:
        mod = modc if i < 2 else modl
        scd = sc_deps[i]
        p.dma(Q[:], V(SC.h[i, :, 0:768], scd)); p.dma(Kd[1][:], V(SC.h[i, :, 768:1536], scd))
        p.dma(LAd[1][:], V(SC.h[i, :, 1536:2304], scd)); p.dma(Vv[:], V(SC.h[i, :, 2304:3328], scd))
        p.dma(G[:], V(SC.h[i, :, 3328:4352], scd))
        p.dma(of[:], V(OF.h[i], of_deps[i]))
        p.dma(xt[:], xin[i * 128:(i + 1) * 128, :])
        recur(i, 1)
        p.tt(ot[:, 0:512], pO[0][:], of[:, 0:512], ALU.add); p.tt(ot[:, 512:1024], pO[1][:], of[:, 512:1024], ALU.add)
        p.tt(tmp[:], ot[:], ot[:], ALU.mult)
        p.reduce(st[:, 0:8], V(tmp.h[:].rearrange("p (a b) -> p a b", b=128), tmp.dep), ALU.add)
        p.ts(st[:, 0:8], st[:, 0:8], 1.0 / 128, ALU.mult, 1e-6, ALU.add)
        p.act(st[:, 0:8], st[:, 0:8], AF.Sqrt)
        p.recip(st[:, 0:8], st[:, 0:8])
        p.tt(V(yn.h[:].rearrange("p (a b) -> p a b", b=128), yn.dep), V(ot.h[:].rearrange("p (a b) -> p a b", b=128), ot.dep),
             V(st.h[:, 0:8].to_broadcast([128, 8, 128]), st.dep), ALU.mult)
        p.tt(yn[:], yn[:], gv[:], ALU.mult)
        p.tt(yn[:], yn[:], G[:], ALU.mult)
        transpose_tile(p, ynT, yn[:], D, ident, pT)
        for n in range(2):
            p.dma(wk[n][:], V(w_out.h[:, n * 512:(n + 1) * 512].rearrange("(k q) n -> q k n", q=128), None))
            for k in range(8):
                p.mm(pA[n][:], ynT[:, k, :], wk[n][:, k, :], start=(k == 0), stop=(k == 7))
            p.tt(tmp[:, n * 512:(n + 1) * 512], pA[n][:], mod[:, 2 * D + n * 512:2 * D + (n + 1) * 512], ALU.mult)
        p.stt(ot[:], xt[:], ALPHA, tmp[:], ALU.mult, ALU.add)
        layer_norm(p, yn[:], ot[:], lng[:], lnb[:], V(st.h[:, 8:12], st.dep), tmp[:])
        p.dma(V(xout.h[i * 128:(i + 1) * 128, :], out_deps[i]), yn[:])
    if standalone:
        return p.finish(out_deps)
    p.end_phase()


NB = NT * 8 + 256


def consts_moe():
    i = np.arange(128)
    triU = (i[:, None] <= i[None, :]).astype(np.float32)
    io13 = (np.arange(8)[None, :] * 128 + i[:, None]).astype(np.float32)
    io2 = (np.arange(2)[None, :] * 128 + i[:, None]).astype(np.float32)
    return dict(ident=np.eye(128, dtype=np.float32), triU=triU, ones=np.ones((128, 128), np.float32),
                bvals=(np.arange(NB, dtype=np.float32) * 128)[None], io13=io13, io2=io2)


def build_moe(dbg=99, p=None, io=None, pfx=""):
    standalone = p is None
    if standalone:
        p = Prog()
    io = io or {}
    p.begin_phase()
    IN = lambda n, s, dt=F32: io[n] if n in io else p.dram(pfx + n, s, dt, kind="ExternalInput")
    xin = IN("xin", [NT * 128, D])
    cT = IN("cT", [128, 8]); ccT = IN("ccT", [128, 8])
    mod_w = IN("mod_w", [D, 6 * D]); mod_b = IN("mod_b", [1, 6 * D])
    ln_g = IN("ln_g", [1, D]); ln_b = IN("ln_b", [1, D])
    rw = IN("rw", [D, 256]); rbias = IN("rbias", [1, 256])
    W13 = IN("w13", [256 * 1024, 512]); W2 = IN("w2", [256 * 256, 1024])
    sw13 = IN("sw13", [D, 512]); sw2 = IN("sw2", [256, D])
    ident_d = IN("ident", [128, 128]); triU_d = IN("triU", [128, 128]); ones_d = IN("ones", [128, 128])
    bvals_d = IN("bvals", [1, NB]); io13_d = IN("io13", [128, 8]); io2_d = IN("io2", [128, 2])
    xout = io["xout"] if "xout" in io else p.dram("xout", [NT * 128, D], kind="ExternalOutput")
    U = io["U"] if "U" in io else p.dram("U", [NT, 128, D]); RG = io["RG"] if "RG" in io else p.dram("RG", [NT, 128, 512])
    HB = NB // 2; HR = HB * 128
    XS = io["XS"] if "XS" in io else [p.dram("XSa", [HR, D]), p.dram("XSb", [HR, D])]; YS = io["YS"] if "YS" in io else [p.dram("YSa", [HR, D]), p.dram("YSb", [HR, D])]
    u_deps = [Dep() for _ in range(NT)]; rg_deps = [Dep() for _ in range(NT)]; out_deps = [Dep() for _ in range(NT)]

    if dbg == 0:
        return p.finish([])
    S = lambda n, s, dt=F32: p.sb(n, s, dt)
    ident = S("ident", [128, 128]); triU = S("triU", [128, 128]); ones = S("ones", [128, 128])
    p.dma(ident[:], ident_d[:]); p.dma(triU[:], triU_d[:]); p.dma(ones[:], ones_d[:])
    io13 = S("io13", [128, 8]); io2 = S("io2", [128, 2]); p.dma(io13[:], io13_d[:]); p.dma(io2[:], io2_d[:])
    bv = S("bv", [128, NB]); p.dma(bv[:], pbc(bvals_d))
    modl = S("modl", [128, 3 * D]); modc = S("modc", [128, 3 * D])
    w13t = [S("w13t0", [128, 8, 512]), S("w13t1", [128, 8, 512])]
    w2t = [S("w2t0", [128, 2, 1024]), S("w2t1", [128, 2, 1024])]
    pA = [p.ps("pA0", [128, 512]), p.ps("pA1", [128, 512])]
    pT = p.ps("pT", [128, 512]); pH = p.ps("pH", [128, 512]); pY = [p.ps("pY0", [128, 512]), p.ps("pY1", [128, 512])]
    scr = dict(sc=S("sc", [128, 8]), lh=S("lh", [128, 8, 128]))
    mbb = pbc(mod_b)
    modulation(p, cT, mod_w, mbb, 3 * D, 3 * D, modl, w13t, pA, scr)
    modulation(p, ccT, mod_w, mbb, 3 * D, 3 * D, modc, w13t, pA, scr)
    for m in (modl, modc):
        p.ts(m[:, D:2 * D], m[:, D:2 * D], 1.0, ALU.add)
    lng = S("lng", [128, D]); p.dma(lng[:], pbc(ln_g)); lnb = S("lnb", [128, D]); p.dma(lnb[:], pbc(ln_b))
    rwt = S("rwt", [128, 8, 256]); p.dma(rwt[:], V(rw.h.rearrange("(k q) n -> q k n", q=128), None))
    rbt = S("rbt", [128, 256]); p.dma(rbt[:], pbc(rbias))

    xt = S("xt", [128, D]); ut = S("ut", [128, D]); uT = S("uT", [128, 8, 128])
    sc = S("scs", [128, 256]); sel = S("sel", [128, 256]); selm = S("selm", [128, 256]); M = S("M", [128, 256])
    Macc = S("Macc", [128, 256]); RGt = S("RGt", [128, 512]); t256 = S("t256", [128, 256])
    m8g = S("m8g", [128, 8, 8]); m8 = S("m8", [128, 8]); grp = S("grp", [128, 8]); gm = S("gm", [128, 8]); pen = S("pen", [128, 8])
    st = S("st", [128, 16])
    gk = S("gk", [128, NT, 8]); d8 = S("d8", [128, 8]); dsti = [S("dstia", [128, NT, 8], I32), S("dstib", [128, NT, 8], I32)]
    d8b = S("d8b", [128, 8]); ge8 = S("ge8", [128, 8])
    p.memset(Macc[:], 0.0)

    def vmax(out, in_):
        o, a = out.ap, in_.ap
        p.op("vector", lambda e: e.max(o, a), [out], [in_])

    _rc = {}

    def bcreg(e):
        if "r" not in _rc:
            r = e.alloc_register(pfx + "bcreg")
            e.reg_mov(r, HR - 1)
            _rc["r"] = r
        return _rc["r"]

    def bc3(v, shape):
        return V(v.ap.to_broadcast(shape), v.dep)

    zt = V(w13t[1].h[:].rearrange("p a b -> p (a b)"), w13t[1].dep)
    p.memset(zt, 0.0)
    for h in range(2):
        XSv = XS[h].h.rearrange("(n q f) d -> n q (f d)", q=128, f=4)
        for n in range(HR // 512):
            p.dma(V(XSv[n], XS[h].dep), zt)

    for i in range(NT):
        mod = modc if i < 2 else modl
        p.dma(xt[:], xin[i * 128:(i + 1) * 128, :])
        p.tt(ut[:], xt[:], mod[:, D:2 * D], ALU.mult)
        p.tt(ut[:], ut[:], mod[:, 0:D], ALU.add)
        p.dma(V(U.h[i], u_deps[i]), ut[:])
        transpose_tile(p, uT, ut[:], D, ident, pT)
        for k in range(8):
            p.mm(pA[0][:, 0:256], uT[:, k, :], rwt[:, k, :], start=(k == 0), stop=(k == 7))
        p.act(sc[:], pA[0][:, 0:256], AF.Sigmoid)
        p.tt(sel[:], sc[:], rbt[:], ALU.add)
        for g in range(8):
            vmax(m8g[:, g, :], sel[:, g * 32:(g + 1) * 32])
        p.tt(grp[:], m8g[:, :, 0], m8g[:, :, 1], ALU.add)
        vmax(m8[:], grp[:])
        p.ts(gm[:], grp[:], m8[:, 3:4], ALU.is_ge)
        p.ts(pen[:], gm[:], -1.0, ALU.add, 1e30, ALU.mult)
        s3 = V(sel.h[:].rearrange("p (a b) -> p a b", b=32), sel.dep)
        sm3 = V(selm.h[:].rearrange("p (a b) -> p a b", b=32), selm.dep)
        p.tt(sm3, s3, V(gm.h[:].to_broadcast([128, 8, 32]), gm.dep), ALU.mult)
        p.tt(sm3, sm3, V(pen.h[:].to_broadcast([128, 8, 32]), pen.dep), ALU.add)
        vmax(m8[:], selm[:])
        p.ts(M[:], selm[:], m8[:, 7:8], ALU.is_ge)
        p.tt(t256[:], M[:], sc[:], ALU.mult)
        p.reduce(st[:, 0:1], t256[:], ALU.add)
        p.recip(st[:, 0:1], st[:, 0:1])
        p.ts(RGt[:, 256:512], t256[:], st[:, 0:1], ALU.mult, 2.5, ALU.mult)
        p.mm(pA[1][:, 0:256], ones[:], Macc[:], start=True, stop=False)
        p.mm(pA[1][:, 0:256], triU[:], M[:], start=False, stop=True)
        p.tt(RGt[:, 0:256], M[:], pA[1][:, 0:256], ALU.mult)
        p.tt(Macc[:], Macc[:], M[:], ALU.add)
        p.dma(V(RG.h[i], rg_deps[i]), RGt[:])

    if dbg == 1:
        return p.finish(rg_deps)
    cnt = S("cnt", [128, 256]); cnti = S("cnti", [128, 256], I32); pe = [S("pe0", [128, 256]), S("pe1", [128, 256])]
    pstart = S("pstart", [128, 256])
    p.mm(pA[0][:, 0:256], ones[:], Macc[:])
    p.ts(cnt[:], pA[0][:, 0:256], 127.0, ALU.add)
    p.copy(cnti[:], cnt[:])
    p.ts(cnti[:], cnti[:], 7, ALU.arith_shift_right, 7, ALU.logical_shift_left)
    p.copy(cnt[:], cnti[:])
    p.copy(pe[0][:], cnt[:])
    cur = 0
    for sft in (1, 2, 4, 8, 16, 32, 64, 128):
        nxt = 1 - cur
        p.copy(pe[nxt][:, 0:sft], pe[cur][:, 0:sft])
        p.tt(pe[nxt][:, sft:256], pe[cur][:, sft:256], pe[cur][:, 0:256 - sft], ALU.add)
        cur = nxt
    pend = pe[cur]
    p.tt(pstart[:], pend[:], cnt[:], ALU.subtract)
    be = S("be", [128, NB])
    cmp_ = V(w13t[0].h[:].rearrange("p a b -> p (a b)"), w13t[0].dep)
    for c0 in range(0, NB, 16):
        cv = V(cmp_.ap.rearrange("p (a b) -> p a b", b=256), cmp_.dep)
        p.tt(cv, V(pend.h[:, None, :].to_broadcast([128, 16, 256]), pend.dep),
             V(bv.h[:, c0:c0 + 16, None].to_broadcast([128, 16, 256]), bv.dep), ALU.is_le)
        p.reduce(be[:, c0:c0 + 16], cv, ALU.add)
    p.ts(be[:], be[:], 255.0, ALU.min)
    idx13 = S("idx13", [128, NB, 8], I32); idx2 = S("idx2", [128, NB, 2], I32)
    for h0 in range(0, NB, 264):
        f13 = V(cmp_.ap[:, 0:264 * 8].rearrange("p (a b) -> p a b", b=8), cmp_.dep)
        p.ts(f13, V(be.h[:, h0:h0 + 264, None].to_broadcast([128, 264, 8]), be.dep), 1024.0, ALU.mult)
        p.tt(f13, f13, V(io13.h[:, None, :].to_broadcast([128, 264, 8]), io13.dep), ALU.add)
        p.copy(idx13[:, h0:h0 + 264, :], f13)
        f2 = V(cmp_.ap[:, 0:264 * 2].rearrange("p (a b) -> p a b", b=2), cmp_.dep)
        p.ts(f2, V(be.h[:, h0:h0 + 264, None].to_broadcast([128, 264, 2]), be.dep), 256.0, ALU.mult)
        p.tt(f2, f2, V(io2.h[:, None, :].to_broadcast([128, 264, 2]), io2.dep), ALU.add)
        p.copy(idx2[:, h0:h0 + 264, :], f2)

    if dbg == 2:
        return p.finish([idx13.dep, idx2.dep])
    for i in range(NT):
        p.dma(RGt[:], V(RG.h[i], rg_deps[i]))
        p.dma(ut[:], V(U.h[i], u_deps[i]))
        p.stt(t256[:], RGt[:, 0:256], 0.0, pstart[:], ALU.is_gt, ALU.mult)
        p.tt(sel[:], t256[:], RGt[:, 0:256], ALU.add)
        vmax(d8[:], sel[:])
        for k in range(8):
            p.stt(t256[:], sel[:], d8[:, k:k + 1], RGt[:, 256:512], ALU.is_equal, ALU.mult)
            p.reduce(gk[:, i, k:k + 1], t256[:], ALU.add)
        p.ts(d8[:], d8[:], -1.0, ALU.add)
        p.copy(dsti[0][:, i, :], d8[:])
        p.ts(ge8[:], d8[:], float(HR), ALU.is_ge)
        p.ts(d8b[:], d8[:], -float(HR) - 1e6, ALU.add)
        p.tt(d8b[:], d8b[:], ge8[:], ALU.mult)
        p.ts(d8b[:], d8b[:], 1e6, ALU.add)
        p.copy(dsti[1][:, i, :], d8b[:])
        for k in range(8):
            for h in range(2):
                oap, iap, src = XS[h].h[:, :], dsti[h].h[:, i, k:k + 1], ut.h[:, :]
                p.dma(XS[h][:], ut[:], eng="gpsimd", extra_reads=[dsti[h][:]],
                      fn=lambda e, oap=oap, iap=iap, src=src: e.indirect_dma_start(
                          out=oap, out_offset=bass.IndirectOffsetOnAxis(ap=iap, axis=0), in_=src, in_offset=None,
                          bounds_check=bcreg(e), oob_is_err=False))

    if dbg == 3:
        return p.finish([XS[0].dep, XS[1].dep])
    xs = [S("xs0", [128, D]), S("xs1", [128, D])]; xT = S("xT", [128, 8, 128])
    a1 = S("a1", [128, 256]); actT = S("actT", [128, 2, 128]); yb = [S("yb0", [128, D]), S("yb1", [128, D])]
    for b in range(NB):
        w13b, w2b, xsb, ybb = w13t[b % 2], w2t[b % 2], xs[b % 2], yb[b % 2]
        bh, bl = b // HB, b % HB
        p.dma(xsb[:], XS[bh][bl * 128:(bl + 1) * 128, :])
        for k in range(8):
            oap, iap, src = w13b.h[:, k, :], idx13.h[:, b, k:k + 1], W13.h[:, :]
            p.dma(w13b[:], W13[:], eng="gpsimd", extra_reads=[idx13[:]],
                  fn=lambda e, oap=oap, iap=iap, src=src: e.indirect_dma_start(
                      out=oap, out_offset=None, in_=src, in_offset=bass.IndirectOffsetOnAxis(ap=iap, axis=0)))
        for k in range(2):
            oap, iap, src = w2b.h[:, k, :], idx2.h[:, b, k:k + 1], W2.h[:, :]
            p.dma(w2b[:], W2[:], eng="gpsimd", extra_reads=[idx2[:]],
                  fn=lambda e, oap=oap, iap=iap, src=src: e.indirect_dma_start(
                      out=oap, out_offset=None, in_=src, in_offset=bass.IndirectOffsetOnAxis(ap=iap, axis=0)))
        transpose_tile(p, xT, xsb[:], D, ident, pT, eng="scalar")
        for m in range(4):
            for k in range(8):
                p.mm(pH[:, m * 128:(m + 1) * 128], w13b[:, k, m * 128:(m + 1) * 128], xT[:, k, :], start=(k == 0), stop=(k == 7))
        p.act(a1[:], pH[:, 0:256], AF.Silu)
        p.tt(V(actT.h[:].rearrange("p a b -> p (a b)"), actT.dep), a1[:], pH[:, 256:512], ALU.mult)
        for n in range(2):
            for k in range(2):
                p.mm(pY[n][:], actT[:, k, :], w2b[:, k, n * 512:(n + 1) * 512], start=(k == 0), stop=(k == 1))
        p.copy(ybb[:, 0:512], pY[0][:], eng="scalar")
        p.copy(ybb[:, 512:1024], pY[1][:], eng="vector")
        p.dma(YS[bh][bl * 128:(bl + 1) * 128, :], ybb[:])

    if dbg == 4:
        return p.finish([YS[0].dep, YS[1].dep])
    acc = yb[1]; yg = xs; tmp = yb[0]
    sw13t = w13t[0]; p.dma(sw13t[:], V(sw13.h.rearrange("(k q) n -> q k n", q=128), None))
    sw2t = w2t[0]; p.dma(sw2t[:], V(sw2.h.rearrange("(k q) n -> q k n", q=128), None))
    for i in range(NT):
        mod = modc if i < 2 else modl
        p.dma(ut[:], V(U.h[i], u_deps[i]))
        p.dma(xt[:], xin[i * 128:(i + 1) * 128, :])
        transpose_tile(p, uT, ut[:], D, ident, pT)
        for m in range(4):
            for k in range(8):
                p.mm(pH[:, m * 128:(m + 1) * 128], sw13t[:, k, m * 128:(m + 1) * 128], uT[:, k, :], start=(k == 0), stop=(k == 7))
        p.act(a1[:], pH[:, 0:256], AF.Silu)
        p.tt(V(actT.h[:].rearrange("p a b -> p (a b)"), actT.dep), a1[:], pH[:, 256:512], ALU.mult)
        for n in range(2):
            for k in range(2):
                p.mm(pY[n][:], actT[:, k, :], sw2t[:, k, n * 512:(n + 1) * 512], start=(k == 0), stop=(k == 1))
        p.copy(acc[:, 0:512], pY[0][:], eng="scalar")
        p.copy(acc[:, 512:1024], pY[1][:], eng="scalar")
        for k in range(8):
            ygk = yg[k % 2]
            for h in range(2):
                oap, iap, src = ygk.h[:, :], dsti[h].h[:, i, k:k + 1], YS[h].h[:, :]
                p.dma(ygk[:], YS[h][:], eng="gpsimd", extra_reads=[dsti[h][:]],
                      fn=lambda e, oap=oap, iap=iap, src=src: e.indirect_dma_start(
                          out=oap, out_offset=None, in_=src, in_offset=bass.IndirectOffsetOnAxis(ap=iap, axis=0),
                          bounds_check=bcreg(e), oob_is_err=False))
            p.stt(acc[:], ygk[:], gk[:, i, k:k + 1], acc[:], ALU.mult, ALU.add)
        p.tt(acc[:], acc[:], mod[:, 2 * D:3 * D], ALU.mult)
        p.stt(acc[:], xt[:], ALPHA, acc[:], ALU.mult, ALU.add)
        layer_norm(p, ut[:], acc[:], lng[:], lnb[:], V(st.h[:, 8:12], st.dep), tmp[:])
        p.dma(V(xout.h[i * 128:(i + 1) * 128, :], out_deps[i]), ut[:])
    if standalone:
        return p.finish(out_deps)
    p.end_phase()


def moe_inputs(inp, l, b, xl, xc):
    d = dict(xin=np.concatenate([xc, xl], 0), cT=np.ascontiguousarray(inp["c"][b].reshape(8, 128).T),
             ccT=np.ascontiguousarray(inp["c_ctx"].reshape(8, 128).T), mod_w=inp["mod_w"][l], mod_b=inp["mod_b"][l][None],
             ln_g=inp["ln2_g"][l][None], ln_b=inp["ln2_b"][l][None], rw=inp["router_w"][l], rbias=inp["router_bias"][l][None],
             w13=inp["exp_w13"][l].reshape(256 * 1024, 512), w2=inp["exp_w2"][l].reshape(256 * 256, 1024),
             sw13=inp["sh_w13"][l], sw2=inp["sh_w2"][l])
    d.update(consts_moe())
    return d


RWW = 1856
DBGT = 0


def consts_rw():
    i = np.arange(128)
    ch = i // 64
    same = (ch[:, None] == ch[None, :])
    triF = (same & (i[:, None] <= i[None, :])).astype(np.float32)
    triB = (same & (i[:, None] >= i[None, :])).astype(np.float32)
    eye = np.eye(128, dtype=np.float32)
    last = same.astype(np.float32)
    out = dict(ident=eye, ci=np.stack([(ch == 0), (ch == 1)], 1).astype(np.float32))
    for nm, tri in (("F", triF), ("B", triB)):
        ms = tri - eye
        out["cm" + nm] = np.concatenate([tri, last - tri], 1)
        out["mk1" + nm] = np.concatenate([ms, tri], 1)
        out["mk2" + nm] = np.concatenate([-ms, -tri], 1)
        out["mk3" + nm] = np.ascontiguousarray(-ms.T)
    out["mlr"] = np.stack([(i % 64 != 0), (i % 64 != 63)], 1).astype(np.float32)
    return out


def build_l1_rw(dbg=99, p=None, io=None, pfx=""):
    standalone = p is None
    if standalone:
        p = Prog()
    io = io or {}
    p.begin_phase()
    IN = lambda n, s, dt=F32: io[n] if n in io else p.dram(pfx + n, s, dt, kind="ExternalInput")
    xin = IN("xin", [NT * 128, D])
    cT = IN("cT", [128, 8]); ccT = IN("ccT", [128, 8])
    mod_w = IN("mod_w", [D, 6 * D]); mod_b = IN("mod_b", [1, 6 * D])
    w_in = IN("w_in", [D, 3392])
    cw = IN("cw", [3, 1536]); cb = IN("cb", [1, 1536]); mu_d = IN("mu", [1, RWW])
    w0_d = IN("w0", [1, 1024]); w2_d = IN("w2", [128, 512]); a0_d = IN("a0", [1, 512]); a2_d = IN("a2", [64, 512]); g2_d = IN("g2", [128, 512])
    vec_d = IN("vecs", [5, 512])
    cst = {k: IN(k, list(v.shape)) for k, v in consts_rw().items()}
    yrw = io["yrw"] if "yrw" in io else p.dram("yrw", [4096, 512], kind="ExternalOutput")
    hv = io["hv"] if "hv" in io else p.dram("hv", [4096, 1536], kind="ExternalOutput")
    PH = p.dram(pfx + "PH", [4096 + 2, 1536]); PRc = p.dram(pfx + "PRc", [256 + 2, RWW]); PRl = p.dram(pfx + "PRl", [4096 + 128, RWW])
    SC = p.dram(pfx + "SC2", [NT, 128, 3584]); OF = p.dram(pfx + "OF2", [NT, 128, 512])
    sc_deps = [Dep() for _ in range(NT)]; of_deps = [Dep() for _ in range(NT)]
    out_deps = [Dep() for _ in range(64)]

    S = lambda n, s, dt=F32: p.sb(n, s, dt)
    C = {}
    for k in ("ident", "ci", "cmF", "cmB", "mk1F", "mk1B", "mk2F", "mk2B", "mk3F", "mk3B", "mlr"):
        C[k] = S(k, list(cst[k].h.shape)); p.dma(C[k][:], cst[k][:])
    ident = C["ident"]; ci = C["ci"]; cm = [C["cmF"], C["cmB"]]; mk1 = [C["mk1F"], C["mk1B"]]; mk2 = [C["mk2F"], C["mk2B"]]; mk3 = [C["mk3F"], C["mk3B"]]
    mlr = C["mlr"]
    modl = S("modl", [128, 2 * D]); modc = S("modc", [128, 2 * D])
    wk = [S("wk0", [128, 8, 512]), S("wk1", [128, 8, 512])]
    pA = [p.ps("pA0", [128, 512]), p.ps("pA1", [128, 512])]
    pT = p.ps("pT", [128, 512]); pG = p.ps("pG", [128, 512]); pP = p.ps("pP", [128, 512]); pY = p.ps("pY", [128, 512])
    pO = p.ps("pO", [128, 512]); pKV = p.ps("pKV", [128, 512])
    xt = S("xt", [128, D]); hT = S("hT", [128, 8, 128]); Z = S("Z", [128, 3392])

    def al(tt_, ap):
        t = T(ap); t.dep = tt_.dep; return t
    scr = dict(sc=S("sc", [128, 8]), lh=al(Z, Z.h[:, 0:1024].rearrange("p (a b) -> p a b", b=128)))
    mbb = pbc(mod_b)
    modulation(p, cT, mod_w, mbb, 0, 2 * D, modl, wk, pA, scr)
    modulation(p, ccT, mod_w, mbb, 0, 2 * D, modc, wk, pA, scr)
    for m in (modl, modc):
        p.ts(m[:, D:2 * D], m[:, D:2 * D], 1.0, ALU.add)

    def bct(name, src_v, n):
        t = S(name, [128, n]); p.dma(t[:], src_v); return t
    cwt = [bct("cw%d" % j, V(cw.h[j].partition_broadcast(128), None), 1536) for j in range(3)]
    cbt = bct("cbt", pbc(cb), 1536); mut = bct("mut", pbc(mu_d), RWW)
    w0t = bct("w0t", pbc(w0_d), 1024); a0t = bct("a0t", pbc(a0_d), 512)
    vecs = [bct("vec%d" % j, V(vec_d.h[j].partition_broadcast(128), None), 512) for j in range(5)]
    kkt, kat, rkt, lgt, lbt = vecs
    w2t = S("w2t", [128, 512]); p.dma(w2t[:], w2_d[:]); a2t = S("a2t", [64, 512]); p.dma(a2t[:], a2_d[:]); g2t = S("g2t", [128, 512]); p.dma(g2t[:], g2_d[:])

    zt = S("zt", [128, RWW]); p.memset(zt[:], 0.0)
    p.dma(PH[0:1, :], zt[0:1, 0:1536]); p.dma(PH[4097:4098, :], zt[0:1, 0:1536])
    p.dma(PRc[0:1, :], zt[0:1, :]); p.dma(PRc[257:258, :], zt[0:1, :])
    p.dma(PRl[0:64, :], zt[0:64, :]); p.dma(PRl[4096 + 64:4096 + 128, :], zt[0:64, :])

    for i in range(NT):
        mod = modc if i < 2 else modl
        p.dma(xt[:], xin[i * 128:(i + 1) * 128, :])
        p.tt(xt[:], xt[:], mod[:, D:2 * D], ALU.mult)
        p.tt(xt[:], xt[:], mod[:, 0:D], ALU.add)
        transpose_tile(p, hT, xt[:], D, ident, pT)
        for n in range(7):
            c0 = n * 512; c1 = min(3392, c0 + 512); w = wk[n % 2]
            p.dma(V(w.h[:, :, 0:c1 - c0], w.dep), V(w_in.h[:, c0:c1].rearrange("(k q) n -> q k n", q=128), None))
            pp = pA[n % 2]
            for k in range(8):
                p.mm(pp[:, 0:c1 - c0], hT[:, k, :], V(w.h[:, k, 0:c1 - c0], w.dep), start=(k == 0), stop=(k == 7))
            p.copy(Z[:, c0:c1], pp[:, 0:c1 - c0], eng="scalar")
        if i < 2:
            p.dma(PRc[1 + i * 128:1 + (i + 1) * 128, :], Z[:, 1536:3392])
        else:
            t0 = (i - 2) * 128
            p.dma(PRl[64 + t0:64 + t0 + 128, :], Z[:, 1536:3392])
            p.dma(PH[1 + t0:1 + t0 + 128, :], Z[:, 0:1536])
    if dbg == 1:
        return p.finish([PRl.dep, PH.dep, PRc.dep])

    Pc = S("Pc", [128, RWW]); Ps = [al(modl, modl.h[:, 0:RWW]), al(modc, modc.h[:, 0:RWW])]; sh = zt
    X1 = S("X1", [128, 128]); X3 = S("X3", [128, 128]); T1 = S("T1", [128, 128]); T2 = S("T2", [64, 128]); T3 = S("T3", [128, 128])
    NM = ["kk", "b", "km", "v", "r", "ldb", "g", "ldf", "a"]
    st_ = {n: S("s_" + n, [128, 512]) for n in NM}
    st8 = S("st8", [128, 16])
    E3 = S("E3", [128, 512]); E2 = S("E2", [128, 512]); ex = S("ex", [128, 512])
    At_ = S("Atl", [128, 512]); Rt_ = S("Rtl", [128, 512]); Kt_ = S("Ktl", [128, 512]); Bt_ = S("Btl", [128, 512])
    Kh = S("Kh", [128, 512]); nBh = S("nBh", [128, 512])
    ARt = S("ARt", [128, 4, 256]); KtT = S("KtT", [128, 4, 128]); BtT = S("BtT", [128, 4, 128])
    dec = S("dec", [128, 8])
    LM1 = S("LM1", [128, 256]); LM2 = S("LM2", [128, 256])
    Atp = [S("Atp%d" % j, [128, 128]) for j in range(6)]; Anp = [S("Anp%d" % j, [128, 128]) for j in range(5)]
    Y = S("Y", [128, 128]); ApT = S("ApT", [128, 128]); Usb = S("Usb", [128, 64])
    Tst = [[S("T%d_%d" % (d, g), [128, 64]) for g in range(4)] for d in range(2)]
    ot = al(xt, xt.h[:, 0:512]); of = S("of", [128, 512]); tmp5 = al(hT, hT.h[:].rearrange("p a b -> p (a b)")[:, 0:512])
    c3 = [al(Z, Z.h[:, 0:1536]), al(wk[0], wk[0].h[:].rearrange("p a b -> p (a b)")[:, 0:1536]), al(wk[1], wk[1].h[:].rearrange("p a b -> p (a b)")[:, 0:1536])]
    p.memset(Usb[:], 0.0)
    for d in range(2):
        for g in range(4):
            p.memset(Tst[d][g][:], 0.0)

    def v3(v, b):
        return V(v.ap.rearrange("p (a b) -> p a b", b=b), v.dep)

    def streams(i):
        if i < 2:
            r0 = 1 + i * 128
            p.dma(Pc[:], PRc[r0:r0 + 128, :])
            p.dma(Ps[0][:], PRc[r0 - 1:r0 + 127, :]); p.dma(Ps[1][:], PRc[r0 + 1:r0 + 129, :])
            for j in range(2):
                p.copy(v3(sh[:], 2)[:, :, j], v3(Ps[j][:], 2)[:, :, j], eng="gpsimd")
        else:
            r0 = 64 + (i - 2) * 128
            p.dma(Pc[:], PRl[r0:r0 + 128, :])
            for j, off in enumerate((-1, 1, -64, 64)):
                q = Ps[j % 2]
                p.dma(q[:], PRl[r0 + off:r0 + off + 128, :])
                if j < 2:
                    p.ts(v3(sh[:], 4)[:, :, j], v3(q[:], 4)[:, :, j], mlr[:, j:j + 1], ALU.mult)
                else:
                    p.copy(v3(sh[:], 4)[:, :, j], v3(q[:], 4)[:, :, j], eng="gpsimd")
        p.tt(sh[:], sh[:], Pc[:], ALU.subtract)
        p.tt(sh[:], sh[:], mut[:], ALU.mult)
        p.tt(Pc[:], Pc[:], sh[:], ALU.add)
        r, k, v = Pc[:, 0:512], Pc[:, 512:1024], Pc[:, 1024:1536]
        p.act(X1[:], Pc[:, 1536:1664], AF.Tanh)
        p.act(X3[:], Pc[:, 1728:1856], AF.Sigmoid)
        p.tr(pT[:, 0:128], X1[:], ident[:]); p.tr(pT[0:64, 128:256], Pc[:, 1664:1728], ident[:]); p.tr(pT[:, 256:384], X3[:], ident[:])
        p.copy(T1[:], pT[:, 0:128]); p.copy(T2[:], pT[0:64, 128:256]); p.copy(T3[:], pT[:, 256:384])
        for d, nm in ((0, "ldf"), (1, "ldb")):
            rows = slice(d * 64, d * 64 + 64)
            p.mm(pA[d][:], T1[rows, :], w2t[rows, :])
            p.tt(st_[nm][:], pA[d][:], w0t[:, d * 512:(d + 1) * 512], ALU.add)
            p.act(st_[nm][:], st_[nm][:], AF.Sigmoid)
            p.ts(st_[nm][:], st_[nm][:], -float(np.exp(-0.5)), ALU.mult)
        p.mm(pG[:], T2[:], a2t[:])
        p.tt(st_["a"][:], pG[:], a0t[:], ALU.add)
        p.act(st_["a"][:], st_["a"][:], AF.Sigmoid)
        p.mm(pP[:], T3[:], g2t[:])
        p.copy(st_["g"][:], pP[:], eng="scalar")
        p.copy(st_["v"][:], v, eng="gpsimd"); p.copy(st_["r"][:], r, eng="gpsimd")
        p.tt(st_["kk"][:], k, kkt[:], ALU.mult)
        p.tt(tmp5[:], st_["kk"][:], st_["kk"][:], ALU.mult)
        p.reduce(st8[:, 0:8], v3(tmp5[:], 64), ALU.add)
        p.act(st8[:, 0:8], st8[:, 0:8], AF.Sqrt)
        p.ts(st8[:, 0:8], st8[:, 0:8], 1e-12, ALU.max)
        p.recip(st8[:, 0:8], st8[:, 0:8])
        p.tt(v3(st_["kk"][:], 64), v3(st_["kk"][:], 64), V(st8.h[:, 0:8].to_broadcast([128, 8, 64]), st8.dep), ALU.mult)
        p.ts(tmp5[:], st_["a"][:], -1.0, ALU.add)
        p.tt(tmp5[:], tmp5[:], kat[:], ALU.mult)
        p.ts(tmp5[:], tmp5[:], 1.0, ALU.add)
        p.tt(st_["km"][:], k, tmp5[:], ALU.mult)
        p.tt(st_["b"][:], st_["kk"][:], st_["a"][:], ALU.mult)

    def recur(i, d, emit):
        ld = st_["ldf"] if d == 0 else st_["ldb"]
        for e, dst in ((0, E3), (1, E2)):
            p.mm(pA[e][:], cm[d][:, e * 128:(e + 1) * 128], ld[:])
            p.copy(dst[:], pA[e][:], eng="scalar")
        for g in range(4):
            p.mm(pKV[:, 384 + g * 2:384 + g * 2 + 2], ld[:, g * 128:(g + 1) * 128], ci[:])
        p.act(dec[:], pKV[:, 384:392], AF.Exp)
        p.act(ex[:], E3[:], AF.Exp); p.tt(Rt_[:], st_["r"][:], ex[:], ALU.mult)
        p.act(ex[:], E3[:], AF.Exp, scale=-1.0); p.tt(Kt_[:], st_["km"][:], ex[:], ALU.mult); p.tt(Bt_[:], st_["b"][:], ex[:], ALU.mult)
        p.tt(ex[:], E3[:], ld[:], ALU.subtract); p.act(ex[:], ex[:], AF.Exp); p.tt(At_[:], st_["kk"][:], ex[:], ALU.mult)
        p.act(ex[:], E2[:], AF.Exp); p.tt(Kh[:], st_["km"][:], ex[:], ALU.mult)
        p.stt(nBh[:], st_["b"][:], -1.0, ex[:], ALU.mult, ALU.mult)
        for src, dstv in ((At_, lambda g: ARt[:, g, 0:128]), (Rt_, lambda g: ARt[:, g, 128:256]), (Kt_, lambda g: KtT[:, g, :]), (Bt_, lambda g: BtT[:, g, :])):
            for g in range(4):
                p.tr(pT[:, g * 128:(g + 1) * 128], src[:, g * 128:(g + 1) * 128], ident[:])
            for g in range(4):
                p.copy(dstv(g), pT[:, g * 128:(g + 1) * 128], eng=("scalar" if g % 2 else "vector"))
        chunks = (0, 1) if d == 0 else (1, 0)
        for hh in range(8):
            g, base = hh // 2, (hh % 2) * 64
            rows = slice(base, base + 64)
            hc = slice(hh * 64, hh * 64 + 64)
            ya = slice(base, base + 64)
            yv = slice(64 - base, 128 - base)
            p.mm(pG[:, 0:256], KtT[rows, g, :], ARt[rows, g, :])
            p.tt(LM1[:], pG[:, 0:256], mk1[d][:], ALU.mult)
            p.mm(pG[:, 256:512], BtT[rows, g, :], ARt[rows, g, :])
            p.tt(LM2[:], pG[:, 256:512], mk2[d][:], ALU.mult)
            p.mm(pP[:, 0:128], ARt[rows, g, 0:128], BtT[rows, g, :])
            p.tt(Anp[0][:], pP[:, 0:128], mk3[d][:], ALU.mult)
            p.copy(Atp[0][:], LM2[:, 0:128], eng="gpsimd")
            p.mm(pY[:, 0:64], LM1[:, 0:128], st_["v"][:, hc])
            p.copy(Y[:, yv], pY[:, 0:64], eng="scalar")
            p.copy(Y[:, ya], At_[:, hc], eng="gpsimd")
            for j in range(1, 6):
                p.mm(pP[:, 128:256], Anp[j - 1][:], Atp[j - 1][:])
                p.copy(Atp[j][:], pP[:, 128:256], eng="scalar")
                if j < 5:
                    p.mm(pP[:, 256:384], Atp[j - 1][:], Anp[j - 1][:])
                    p.copy(Anp[j][:], pP[:, 256:384], eng="vector")
            for j in range(6):
                p.mm(pY[:, 128:256], Atp[j][:], Y[:])
                p.tt(Y[:], Y[:], pY[:, 128:256], ALU.add)
            p.tr(pY[:, 256:384], Y[:], ident[:])
            p.copy(ApT[:], pY[:, 256:384], eng="scalar")
            Tg = Tst[d][g]
            for n, c in enumerate(chunks):
                tk = slice(c * 64, (c + 1) * 64)
                us = slice(256 + n * 64, 256 + (n + 1) * 64)
                p.mm(pKV[tk, us], ApT[rows, tk], Tg[rows, :])
                p.tt(Usb[tk, :], pKV[tk, us], Y[tk, yv], ALU.add)
                if emit:
                    p.mm(pA[0][tk, hc], ARt[rows, g, 128 + c * 64:128 + (c + 1) * 64], Tg[rows, :], start=True, stop=True)
                    p.mm(pO[tk, hc], LM1[tk, 128 + c * 64:128 + (c + 1) * 64], st_["v"][tk, hc], start=True, stop=False)
                    p.mm(pO[tk, hc], LM2[tk, 128 + c * 64:128 + (c + 1) * 64], Usb[tk, :], start=False, stop=True)
                kvs = slice((hh % 2) * 128 + n * 64, (hh % 2) * 128 + (n + 1) * 64)
                p.mm(pKV[rows, kvs], Kh[tk, hc], st_["v"][tk, hc], start=True, stop=False)
                p.mm(pKV[rows, kvs], nBh[tk, hc], Usb[tk, :], start=False, stop=True)
                p.stt(Tg[rows, :], Tg[rows, :], dec[rows, g * 2 + c:g * 2 + c + 1], pKV[rows, kvs], ALU.mult, ALU.add)

    order_f = list(range(NT))
    order_b = [1, 0] + list(range(NT - 1, 1, -1))
    for i in order_f:
        streams(i)
        if dbg == 2 and i == DBGT:
            return p.finish([st_[n].dep for n in NM])
        scd = sc_deps[i]
        for j, nm in enumerate(("kk", "b", "km", "v", "r", "ldb", "g")):
            p.dma(V(SC.h[i, :, j * 512:(j + 1) * 512], scd), st_[nm][:])
        recur(i, 0, i >= 2)
        if dbg == 3 and i == DBGT:
            return p.finish([t.dep for t in Tst[0]] + [pO.dep])
        if i >= 2:
            p.copy(of[:], pO[:], eng="scalar")
            p.tt(of[:], of[:], pA[0][:], ALU.add)
            p.dma(V(OF.h[i], of_deps[i]), of[:])
            t0 = (i - 2) * 128
            for j in range(3):
                p.dma(c3[j][:], PH[t0 + j:t0 + j + 128, :])
            p.tt(c3[1][:], c3[1][:], cwt[1][:], ALU.mult)
            p.tt(c3[0][:], c3[0][:], cwt[0][:], ALU.mult)
            p.tt(c3[2][:], c3[2][:], cwt[2][:], ALU.mult)
            p.tt(c3[1][:], c3[1][:], c3[0][:], ALU.add)
            p.tt(c3[1][:], c3[1][:], c3[2][:], ALU.add)
            p.tt(c3[1][:], c3[1][:], cbt[:], ALU.add)
            p.dma(V(hv.h[t0:t0 + 128, :], out_deps[32 + i - 2]), c3[1][:])
        if (dbg == 4 and i == 2) or (dbg == 5 and i == NT - 1):
            return p.finish(out_deps + of_deps)
    for i in order_b:
        scd = sc_deps[i]
        for j, nm in enumerate(("kk", "b", "km", "v", "r", "ldb", "g")):
            p.dma(st_[nm][:], V(SC.h[i, :, j * 512:(j + 1) * 512], scd))
        recur(i, 1, i >= 2)
        if i < 2:
            continue
        p.dma(of[:], V(OF.h[i], of_deps[i]))
        p.tt(ot[:], pO[:], of[:], ALU.add)
        p.tt(ot[:], ot[:], pA[0][:], ALU.add)
        o3 = v3(ot[:], 64)
        p.reduce(st8[:, 0:8], o3, ALU.add)
        p.ts(st8[:, 0:8], st8[:, 0:8], -1.0 / 64, ALU.mult)
        p.tt(o3, o3, V(st8.h[:, 0:8].to_broadcast([128, 8, 64]), st8.dep), ALU.add)
        p.tt(tmp5[:], ot[:], ot[:], ALU.mult)
        p.reduce(st8[:, 8:16], v3(tmp5[:], 64), ALU.add)
        p.ts(st8[:, 8:16], st8[:, 8:16], 1.0 / 64, ALU.mult, 64e-5, ALU.add)
        p.act(st8[:, 8:16], st8[:, 8:16], AF.Sqrt)
        p.recip(st8[:, 8:16], st8[:, 8:16])
        p.tt(o3, o3, V(st8.h[:, 8:16].to_broadcast([128, 8, 64]), st8.dep), ALU.mult)
        p.tt(ot[:], ot[:], lgt[:], ALU.mult)
        p.tt(ot[:], ot[:], lbt[:], ALU.add)
        p.tt(tmp5[:], st_["r"][:], st_["km"][:], ALU.mult)
        p.tt(tmp5[:], tmp5[:], rkt[:], ALU.mult)
        p.reduce(st8[:, 0:8], v3(tmp5[:], 64), ALU.add)
        p.tt(v3(tmp5[:], 64), v3(st_["v"][:], 64), V(st8.h[:, 0:8].to_broadcast([128, 8, 64]), st8.dep), ALU.mult)
        p.tt(ot[:], ot[:], tmp5[:], ALU.add)
        p.tt(ot[:], ot[:], st_["g"][:], ALU.mult)
        t0 = (i - 2) * 128
        p.dma(V(yrw.h[t0:t0 + 128, :], out_deps[i - 2]), ot[:])
    if standalone:
        return p.finish(out_deps)
    p.end_phase()


def l1rw_inputs(inp, b, xl, xc):
    j = 0
    d = dict(xin=np.concatenate([xc, xl], 0), cT=np.ascontiguousarray(inp["c"][b].reshape(8, 128).T),
             ccT=np.ascontiguousarray(inp["c_ctx"].reshape(8, 128).T), mod_w=inp["mod_w"][1], mod_b=inp["mod_b"][1][None],
             w_in=inp["od_w_in"][j], cw=inp["hy_conv_w"][j], cb=inp["hy_conv_b"][j][None], mu=inp["rw_mu"][j][None],
             w0=inp["rw_w0"][j].reshape(1, 1024), w2=inp["rw_w2"][j].reshape(128, 512), a0=inp["rw_a0"][j][None], a2=inp["rw_a2"][j],
             g2=inp["rw_g2"][j],
             vecs=np.stack([inp["rw_k_k"][j], inp["rw_k_a"][j], inp["rw_r_k"][j].reshape(512), inp["rw_ln_g"][j].reshape(512), inp["rw_ln_b"][j].reshape(512)]))
    d.update(consts_rw())
    return d


NF = 33
_TAB = {}


def dft_tables():
    if not _TAB:
        n = np.arange(NF * 128, dtype=np.int64)
        prod = (n[:, None] * n[None, :]) % 8192
        ang = prod.astype(np.float64) * (2.0 * np.pi / 8192.0)
        valid = ((n[:, None] <= 4096) & (n[None, :] <= 4096))
        for nm, f in (("C", np.cos), ("S", np.sin)):
            t = (f(ang) * valid).astype(np.float32)
            t = t.reshape(NF, 128, NF, 128).transpose(2, 1, 0, 3)
            _TAB[nm] = np.ascontiguousarray(t)
        cf = np.full(NF * 128, 2.0 / 8192.0); cf[0] = 1.0 / 8192.0; cf[4096] = 1.0 / 8192.0; cf[4097:] = 0.0
        _TAB["cfn"] = np.ascontiguousarray(cf.reshape(NF, 128).T.astype(np.float32))
    return _TAB


def hyena_pos_consts():
    L = 4096
    t = np.linspace(0.0, 1.0, L, dtype=np.float32)[:, None]
    wpos = (2.0 * np.pi * np.arange(L, dtype=np.float32)[:, None] / L).astype(np.float32)
    fr = np.linspace(1e-4, 15, 16, dtype=np.float32)[None, :]
    z = np.concatenate([t, np.cos(fr * wpos), -np.sin(fr * wpos)], -1).astype(np.float32)
    deltas = np.linspace(np.log(1e-2) / 1.5, np.log(1e-2) / 0.3, 512, dtype=np.float32)
    tneg = np.ascontiguousarray((-t[:, 0]).reshape(32, 128).T)
    nz = np.ones((128, 32), np.float32); nz[0, 0] = 0.0
    return dict(zT=np.ascontiguousarray(z.T), absd=np.abs(deltas)[None].astype(np.float32), tneg=tneg, nz=nz)


def build_hy_filters():
    p = Prog()
    IN = lambda n, s, dt=F32: p.dram(n, s, dt, kind="ExternalInput")
    zT = IN("zT", [33, 4096]); w1 = IN("w1", [33, 64]); w2 = IN("w2", [64, 64]); w3s = IN("w3s", [64, 256])
    pv = IN("pv", [64, 4])
    absd = IN("absd", [1, 64]); tneg_d = IN("tneg", [128, 32]); nz_d = IN("nz", [128, 32]); cfn_d = IN("cfn", [128, NF])
    Cb = IN("Cb", [NF, 128, NF, 128]); Sb = IN("Sb", [NF, 128, NF, 128])
    FF = p.dram("FF", [2, 2, NF * 128, 64], kind="ExternalOutput")
    ff_deps = [Dep() for _ in range(NF)]
    S = lambda n, s, dt=F32: p.sb(n, s, dt)
    zTt = S("zTt", [33, 4096]); p.dma(zTt[:], zT[:])
    w1t = S("w1t", [33, 64]); p.dma(w1t[:], w1[:]); w2t = S("w2t", [64, 64]); p.dma(w2t[:], w2[:]); w3t = S("w3t", [64, 256]); p.dma(w3t[:], w3s[:])
    pvt = S("pvt", [64, 4]); p.dma(pvt[:], pv[:])
    adt = S("adt", [128, 64]); p.dma(adt[:], pbc(absd)); tneg = S("tneg", [128, 32]); p.dma(tneg[:], tneg_d[:])
    nz = S("nz", [128, 32]); p.dma(nz[:], nz_d[:]); cfn = S("cfn", [128, NF]); p.dma(cfn[:], cfn_d[:])
    H2 = S("H2", [64, 4096]); arg = S("arg", [64, 512]); nf = S("nf", [64, 512]); ni = S("ni", [64, 512], I32); h1s = S("h1s", [64, 512])
    pA = [p.ps("pA0", [128, 512]), p.ps("pA1", [128, 512])]; pB = [p.ps("pB0", [128, 512]), p.ps("pB1", [128, 512])]
    TWO_PI = float(2.0 * np.pi)

    def sin_of(dst, src_ps, bcol, fcol):
        p.ts(arg[:], src_ps, pvt[:, bcol:bcol + 1], ALU.add, pvt[:, fcol:fcol + 1], ALU.mult)
        p.ts(nf[:], arg[:], 1.0 / TWO_PI, ALU.mult)
        p.copy(ni[:], nf[:]); p.copy(nf[:], ni[:])
        p.stt(arg[:], nf[:], -TWO_PI, arg[:], ALU.mult, ALU.add)
        p.ts(arg[:], arg[:], 3.14159, ALU.min, -3.14159, ALU.max)
        p.act(dst, arg[:], AF.Sin)

    for c in range(8):
        cs = slice(c * 512, (c + 1) * 512)
        p.mm(pA[0][0:64, :], w1t[:], zTt[:, cs])
        sin_of(h1s[:], pA[0][0:64, :], 0, 1)
        p.mm(pA[1][0:64, :], w2t[:], h1s[:])
        sin_of(H2[:, cs], pA[1][0:64, :], 2, 3)
    HS = S("HS", [128, 32, 256]); wdw = S("wdw", [128, 64]); F = S("F", [128, 256]); fb = S("fb", [128, 128])
    for i in range(32):
        p.mm(pB[i % 2][:, 0:256], H2[:, i * 128:(i + 1) * 128], w3t[:])
        p.act(wdw[:], adt[:], AF.Exp, scale=tneg[:, i:i + 1])
        p.tt(V(F.h[:].rearrange("p (a c) -> p a c", c=64), F.dep), V(pB[i % 2].h[:, 0:256].rearrange("p (a c) -> p a c", c=64), pB[i % 2].dep),
             V(wdw.h[:, None, :].to_broadcast([128, 4, 64]), wdw.dep), ALU.mult)
        F4 = F.h[:].rearrange("p (o s c) -> p o s c", o=2, s=2)
        fb2 = V(fb.h[:].rearrange("p (o c) -> p o c", o=2), fb.dep)
        p.ts(fb2, V(F4[:, :, 1, :], F.dep), nz[:, i:i + 1], ALU.mult)
        p.tt(V(HS.h[:, i, 0:128].rearrange("p (o c) -> p o c", o=2), HS.dep), V(F4[:, :, 0, :], F.dep), fb2, ALU.add)
        p.tt(V(HS.h[:, i, 128:256].rearrange("p (o c) -> p o c", o=2), HS.dep), V(F4[:, :, 0, :], F.dep), fb2, ALU.subtract)
    tb = [[S("tb%d_%d" % (a, b), [128, 32, 128]) for b in range(2)] for a in range(2)]
    fo = [S("fo0", [128, 256]), S("fo1", [128, 256])]
    for m in range(NF):
        ct, stb = tb[0][m % 2], tb[1][m % 2]
        p.dma(ct[:], Cb[m, :, 0:32, :]); p.dma(stb[:], Sb[m, :, 0:32, :])
        pr, pi_ = pA[m % 2], pB[m % 2]
        for k in range(32):
            p.mm(pr[:, 0:128], ct[:, k, :], HS[:, k, 0:128], start=(k == 0), stop=(k == 31))
        for k in range(32):
            p.mm(pi_[:, 0:128], stb[:, k, :], HS[:, k, 128:256], start=(k == 0), stop=(k == 31))
        o = fo[m % 2]
        p.ts(o[:, 0:128], pr[:, 0:128], cfn[:, m:m + 1], ALU.mult)
        p.ts(o[:, 128:256], pi_[:, 0:128], cfn[:, m:m + 1], ALU.mult, -1.0, ALU.mult)
        for ri in range(2):
            for od in range(2):
                p.dma(V(FF.h[od, ri, m * 128:(m + 1) * 128, :], ff_deps[m]), o[:, ri * 128 + od * 64:ri * 128 + od * 64 + 64])
    return p.finish(ff_deps)


def hyf_inputs(inp, core):
    j = 0
    w3 = inp["hy_ffn_w3"][j].reshape(64, 2, 2, 512)[:, :, :, core * 64:(core + 1) * 64].reshape(64, 256)
    pc = hyena_pos_consts(); tb = dft_tables()
    return dict(zT=pc["zT"], w1=inp["hy_ffn_w1"][j], w2=inp["hy_ffn_w2"][j], w3s=np.ascontiguousarray(w3),
                pv=np.stack([inp["hy_ffn_b1"][j], inp["hy_sin_freq"][j][0], inp["hy_ffn_b2"][j], inp["hy_sin_freq"][j][1]], 1).astype(np.float32),
                absd=np.ascontiguousarray(pc["absd"][:, core * 64:(core + 1) * 64]), tneg=pc["tneg"], nz=pc["nz"], cfn=tb["cfn"],
                Cb=tb["C"], Sb=tb["S"])


def build_hy_conv(p=None, io=None, pfx=""):
    standalone = p is None
    if standalone:
        p = Prog()
    io = io or {}
    p.begin_phase()
    IN = lambda n, s, dt=F32: io[n] if n in io else p.dram(pfx + n, s, dt, kind="ExternalInput")
    hv = IN("hv", [4096, 1536]); FFd = IN("FFd", [2, 2, NF * 128, 512]); yrw = IN("yrw", [4096, 512]); hyb = IN("hyb", [2, 512])
    xin = IN("xin", [4096, D]); cT = IN("cT", [128, 8]); mod_w = IN("mod_w", [D, 6 * D]); mod_b = IN("mod_b", [1, 6 * D])
    w_out = IN("w_out", [D, D]); ln_g = IN("ln_g", [1, D]); ln_b = IN("ln_b", [1, D]); ident_d = IN("ident", [128, 128])
    Cb = IN("Cb", [NF, 128, NF, 128]); Sb = IN("Sb", [NF, 128, NF, 128])
    xout = io["xout"] if "xout" in io else p.dram("xout", [4096, D], kind="ExternalOutput")
    ZZ = p.dram(pfx + "ZZ", [2, NF * 128, 512]); Z1 = p.dram(pfx + "Z1", [4096, 512]); HY = p.dram(pfx + "HY", [4096, 512])
    zz_deps = [Dep() for _ in range(NF)]; z1_deps = [Dep() for _ in range(32)]; hy_deps = [Dep() for _ in range(32)]
    out_deps = [Dep() for _ in range(32)]
    S = lambda n, s, dt=F32: p.sb(n, s, dt)

    def al(tt_, ap):
        t = T(ap); t.dep = tt_.dep; return t
    ident = S("ident", [128, 128]); p.dma(ident[:], ident_d[:])
    BIG = S("BIG", [128, NF, 512])
    tb = [[S("tb%d_%d" % (a, b), [128, NF, 128]) for b in range(2)] for a in range(2)]
    pA = [p.ps("pA0", [128, 512]), p.ps("pA1", [128, 512])]; pB = [p.ps("pB0", [128, 512]), p.ps("pB1", [128, 512])]
    pT = p.ps("pT", [128, 512])
    wk = [al(tb[0][b], tb[0][b].h[:].rearrange("p a b -> p (a b)")[:, 0:4096].rearrange("p (k n) -> p k n", n=512)) for b in range(2)]
    scr = dict(sc=S("sc", [128, 8]), lh=al(BIG, BIG.h[:, 0:2, :].rearrange("p a b -> p (a b)").rearrange("p (k n) -> p k n", n=128)))
    modg = S("modg", [128, D])
    modulation(p, cT, mod_w, pbc(mod_b), 2 * D, D, modg, wk, pA, scr)
    lng = S("lng", [128, D]); p.dma(lng[:], pbc(ln_g)); lnb = S("lnb", [128, D]); p.dma(lnb[:], pbc(ln_b))
    hbt = [S("hbt%d" % n, [128, 512]) for n in range(2)]
    for n in range(2):
        p.dma(hbt[n][:], V(hyb.h[n].partition_broadcast(128), None))
    ffr = [S("ffr%d" % b, [128, 512]) for b in range(2)]; ffi = [S("ffi%d" % b, [128, 512]) for b in range(2)]
    za = [S("za%d" % b, [128, 512]) for b in range(2)]; zb = [S("zb%d" % b, [128, 512]) for b in range(2)]
    t1 = S("t1", [128, 512])
    yt = S("yt", [128, 256]); zc = S("zc", [128, 256]); gt = S("gt", [128, 256])
    for n in range(2):
        for k in range(32):
            if n == 0:
                p.dma(BIG[:, k, :], hv[k * 128:(k + 1) * 128, 0:512])
            else:
                p.dma(BIG[:, k, :], V(Z1.h[k * 128:(k + 1) * 128, :], z1_deps[k]))
        for m in range(NF):
            ct, stb = tb[0][m % 2], tb[1][m % 2]
            p.dma(ct[:, 0:32, :], Cb[m, :, 0:32, :]); p.dma(stb[:, 0:32, :], Sb[m, :, 0:32, :])
            fr, fi = ffr[m % 2], ffi[m % 2]
            p.dma(fr[:], FFd[n, 0, m * 128:(m + 1) * 128, :]); p.dma(fi[:], FFd[n, 1, m * 128:(m + 1) * 128, :])
            pr, pi_ = pA[m % 2], pB[m % 2]
            for k in range(32):
                p.mm(pr[:], ct[:, k, :], BIG[:, k, :], start=(k == 0), stop=(k == 31))
            for k in range(32):
                p.mm(pi_[:], stb[:, k, :], BIG[:, k, :], start=(k == 0), stop=(k == 31))
            a_, b_ = za[m % 2], zb[m % 2]
            p.tt(a_[:], pr[:], fr[:], ALU.mult); p.tt(t1[:], pi_[:], fi[:], ALU.mult); p.tt(a_[:], a_[:], t1[:], ALU.add)
            p.tt(b_[:], pi_[:], fr[:], ALU.mult); p.tt(t1[:], pr[:], fi[:], ALU.mult); p.tt(b_[:], b_[:], t1[:], ALU.subtract)
            p.dma(V(ZZ.h[0, m * 128:(m + 1) * 128, :], zz_deps[m]), a_[:])
            p.dma(V(ZZ.h[1, m * 128:(m + 1) * 128, :], zz_deps[m]), b_[:])
        for h in range(2):
            hs_ = slice(h * 256, (h + 1) * 256)
            for k in range(NF):
                p.dma(BIG[:, k, 0:256], V(ZZ.h[0, k * 128:(k + 1) * 128, hs_], zz_deps[k]))
                p.dma(BIG[:, k, 256:512], V(ZZ.h[1, k * 128:(k + 1) * 128, hs_], zz_deps[k]))
            for m in range(32):
                ct, stb = tb[0][m % 2], tb[1][m % 2]
                p.dma(ct[:], Cb[m]); p.dma(stb[:], Sb[m])
                py = pA[m % 2]
                for k in range(NF):
                    p.mm(py[:, 0:256], ct[:, k, :], BIG[:, k, 0:256], start=(k == 0), stop=False)
                for k in range(NF):
                    p.mm(py[:, 0:256], stb[:, k, :], BIG[:, k, 256:512], start=False, stop=(k == NF - 1))
                rs = slice(m * 128, (m + 1) * 128)
                if n == 0:
                    p.dma(zc[:], hv[rs, h * 256:(h + 1) * 256])
                else:
                    p.dma(zc[:], V(Z1.h[rs, hs_], z1_deps[m]))
                p.dma(gt[:], hv[rs, 512 * (n + 1) + h * 256:512 * (n + 1) + (h + 1) * 256])
                p.tt(zc[:], zc[:], hbt[n][:, hs_], ALU.mult)
                p.tt(yt[:], py[:, 0:256], zc[:], ALU.add)
                p.tt(yt[:], yt[:], gt[:], ALU.mult)
                if n == 0:
                    p.dma(V(Z1.h[rs, hs_], z1_deps[m]), yt[:])
                else:
                    p.dma(V(HY.h[rs, hs_], hy_deps[m]), yt[:])
    flat = BIG.h[:].rearrange("p a b -> p (a b)")
    wo = [al(BIG, flat[:, b * 4096:(b + 1) * 4096].rearrange("p (k n) -> p k n", n=512)) for b in range(2)]
    xt = al(BIG, flat[:, 8192:9216]); yc = al(BIG, flat[:, 9216:10240]); tmp = al(BIG, flat[:, 10240:11264]); pre = al(BIG, flat[:, 11264:12288])
    ycT = al(BIG, flat[:, 12288:13312].rearrange("p (k n) -> p k n", n=128))
    st = S("st", [128, 8])
    for nn in range(2):
        p.dma(wo[nn][:], V(w_out.h[:, nn * 512:(nn + 1) * 512].rearrange("(k q) n -> q k n", q=128), None))
    for i in range(32):
        rs = slice(i * 128, (i + 1) * 128)
        p.dma(yc[:, 0:512], V(HY.h[rs, :], hy_deps[i])); p.dma(yc[:, 512:1024], yrw[rs, :]); p.dma(xt[:], xin[rs, :])
        transpose_tile(p, ycT, yc[:], D, ident, pT)
        for nn in range(2):
            for k in range(8):
                p.mm(pB[nn][:], ycT[:, k, :], wo[nn][:, k, :], start=(k == 0), stop=(k == 7))
            p.tt(tmp[:, nn * 512:(nn + 1) * 512], pB[nn][:], modg[:, nn * 512:(nn + 1) * 512], ALU.mult)
        p.stt(pre[:], xt[:], ALPHA, tmp[:], ALU.mult, ALU.add)
        layer_norm(p, yc[:], pre[:], lng[:], lnb[:], st, tmp[:])
        p.dma(V(xout.h[rs, :], out_deps[i]), yc[:])
    if standalone:
        return p.finish(out_deps)
    p.end_phase()


def hyc_inputs(inp, b, xl, hv, yrw, FF):
    tb = dft_tables(); j = 0
    return dict(hv=hv, FFd=FF, yrw=yrw, hyb=inp["hy_bias"][j], xin=xl, cT=np.ascontiguousarray(inp["c"][b].reshape(8, 128).T),
                mod_w=inp["mod_w"][1], mod_b=inp["mod_b"][1][None], w_out=inp["od_w_out"][j], ln_g=inp["ln1_g"][1][None], ln_b=inp["ln1_b"][1][None],
                ident=np.eye(128, dtype=np.float32), Cb=tb["C"], Sb=tb["S"])


def emit_hy_filters_full(p, io, pfx):
    p.begin_phase()
    IN = lambda n, s, dt=F32: io[n] if n in io else p.dram(pfx + n, s, dt, kind="ExternalInput")
    zT = IN("zT", [33, 4096]); w1 = IN("w1", [33, 64]); w2 = IN("w2", [64, 64]); w3 = IN("w3", [64, 2048])
    pv = IN("pv", [64, 4]); absd = IN("absd", [1, 512]); tneg_d = IN("tneg", [128, 32]); nz_d = IN("nz", [128, 32]); cfn_d = IN("cfn", [128, NF])
    Cb = io["Cb"]; Sb = io["Sb"]; FF = io["FF"]
    ff_deps = [Dep() for _ in range(NF)]
    S = lambda n, s, dt=F32: p.sb(n, s, dt)
    zTt = S("zTt", [33, 4096]); p.dma(zTt[:], zT[:])
    w1t = S("w1t", [33, 64]); p.dma(w1t[:], w1[:]); w2t = S("w2t", [64, 64]); p.dma(w2t[:], w2[:])
    pvt = S("pvt", [64, 4]); p.dma(pvt[:], pv[:])
    adt = S("adt", [128, 512]); p.dma(adt[:], pbc(absd)); tneg = S("tneg", [128, 32]); p.dma(tneg[:], tneg_d[:])
    nz = S("nz", [128, 32]); p.dma(nz[:], nz_d[:]); cfn = S("cfn", [128, NF]); p.dma(cfn[:], cfn_d[:])
    H2 = S("H2", [64, 4096]); arg = S("arg", [64, 512]); nf = S("nf", [64, 512]); ni = S("ni", [64, 512], I32); h1s = S("h1s", [64, 512])
    pA = [p.ps("pA0", [128, 512]), p.ps("pA1", [128, 512])]; pB = [p.ps("pB0", [128, 512]), p.ps("pB1", [128, 512])]
    TWO_PI = float(2.0 * np.pi)

    def sin_of(dst, src_ps, bcol, fcol):
        p.ts(arg[:], src_ps, pvt[:, bcol:bcol + 1], ALU.add, pvt[:, fcol:fcol + 1], ALU.mult)
        p.ts(nf[:], arg[:], 1.0 / TWO_PI, ALU.mult)
        p.copy(ni[:], nf[:]); p.copy(nf[:], ni[:])
        p.stt(arg[:], nf[:], -TWO_PI, arg[:], ALU.mult, ALU.add)
        p.ts(arg[:], arg[:], 3.14159, ALU.min, -3.14159, ALU.max)
        p.act(dst, arg[:], AF.Sin)

    for c in range(8):
        cs = slice(c * 512, (c + 1) * 512)
        p.mm(pA[0][0:64, :], w1t[:], zTt[:, cs])
        sin_of(h1s[:], pA[0][0:64, :], 0, 1)
        p.mm(pA[1][0:64, :], w2t[:], h1s[:])
        sin_of(H2[:, cs], pA[1][0:64, :], 2, 3)
    HS = S("HS", [128, 32, 512]); wdw = S("wdw", [128, 256]); F = S("F", [128, 512]); fb = S("fb", [128, 256]); w3p = S("w3p", [64, 512])
    tb = [[S("tb%d_%d" % (a, b), [128, 32, 128]) for b in range(2)] for a in range(2)]
    fo = [S("fo0", [128, 512]), S("fo1", [128, 512])]
    for od in range(2):
        for h in range(2):
            hs_ = slice(h * 256, (h + 1) * 256)
            for sd in range(2):
                c0 = (od * 2 + sd) * 512 + h * 256
                p.dma(w3p[:, sd * 256:(sd + 1) * 256], w3[:, c0:c0 + 256])
            for i in range(32):
                p.mm(pB[i % 2][:], H2[:, i * 128:(i + 1) * 128], w3p[:])
                p.act(wdw[:], adt[:, hs_], AF.Exp, scale=tneg[:, i:i + 1])
                p.tt(V(F.h[:].rearrange("p (a c) -> p a c", c=256), F.dep), V(pB[i % 2].h[:].rearrange("p (a c) -> p a c", c=256), pB[i % 2].dep),
                     V(wdw.h[:, None, :].to_broadcast([128, 2, 256]), wdw.dep), ALU.mult)
                p.ts(fb[:], F[:, 256:512], nz[:, i:i + 1], ALU.mult)
                p.tt(HS[:, i, 0:256], F[:, 0:256], fb[:], ALU.add)
                p.tt(HS[:, i, 256:512], F[:, 0:256], fb[:], ALU.subtract)
            for m in range(NF):
                ct, stb = tb[0][m % 2], tb[1][m % 2]
                p.dma(ct[:], Cb[m, :, 0:32, :]); p.dma(stb[:], Sb[m, :, 0:32, :])
                pr, pi_ = pA[m % 2], pB[m % 2]
                for k in range(32):
                    p.mm(pr[:, 0:256], ct[:, k, :], HS[:, k, 0:256], start=(k == 0), stop=(k == 31))
                for k in range(32):
                    p.mm(pi_[:, 0:256], stb[:, k, :], HS[:, k, 256:512], start=(k == 0), stop=(k == 31))
                o = fo[m % 2]
                p.ts(o[:, 0:256], pr[:, 0:256], cfn[:, m:m + 1], ALU.mult)
                p.ts(o[:, 256:512], pi_[:, 0:256], cfn[:, m:m + 1], ALU.mult, -1.0, ALU.mult)
                for ri in range(2):
                    p.dma(V(FF.h[od, ri, m * 128:(m + 1) * 128, hs_], ff_deps[m]), o[:, ri * 256:(ri + 1) * 256])
    p.end_phase()


def build_fused():
    p = Prog()
    p.use_arena()
    X0 = p.dram("xin0", [NT * 128, D], kind="ExternalInput")
    Cb = p.dram("Cb", [NF, 128, NF, 128], kind="ExternalInput"); Sb = p.dram("Sb", [NF, 128, NF, 128], kind="ExternalInput")
    OUT = p.dram("out", [NT * 128, D], kind="ExternalOutput")
    un = lambda n, s: p.dram(n, s)
    XA = un("XA", [NT * 128, D]); XB = un("XB", [NT * 128, D]); XD = un("XD", [NT * 128, D])
    U = un("U", [NT, 128, D]); RG = un("RG", [NT, 128, 512])
    HR = (NB // 2) * 128
    XS = [un("XSa", [HR, D]), un("XSb", [HR, D])]; YS = [un("YSa", [HR, D]), un("YSb", [HR, D])]
    FF = un("FF", [2, 2, NF * 128, 512]); HV = un("HV", [4096, 1536]); YRW = un("YRW", [4096, 512])
    for t in (XA, XB, XD, FF, HV, YRW):
        t.dep = None
    sub = lambda t, r0, r1: T(t.h[r0:r1, :], tracked=False)
    moe_io = dict(U=U, RG=RG, XS=XS, YS=YS)
    build_l0_mixer(p, dict(xin=X0, xout=XA), "a_")
    build_moe(99, p, dict(xin=XA, xout=XB, **moe_io), "b_")
    emit_hy_filters_full(p, dict(Cb=Cb, Sb=Sb, FF=FF), "f_")
    build_l1_rw(99, p, dict(xin=XB, yrw=YRW, hv=HV), "r_")
    build_hy_conv(p, dict(hv=HV, FFd=FF, yrw=YRW, xin=sub(XB, 256, NT * 128), xout=sub(XD, 256, NT * 128), Cb=Cb, Sb=Sb), "h_")
    p.begin_phase()
    p.dma(V(XD.h[0:256, :], None), V(XB.h[0:256, :], None))
    p.end_phase()
    for t in (U, RG, XS[0], XS[1], YS[0], YS[1]):
        t.dep = Dep()
    p.begin_phase()
    build_moe(99, p, dict(xin=XD, xout=OUT, **moe_io), "e_")
    return p.finish([OUT.dep])


def _pref(d, pfx, drop=()):
    return {pfx + k: v for k, v in d.items() if k not in drop}


def fused_inputs(inp, b):
    j = 0
    tb = dft_tables(); pc = hyena_pos_consts()
    xl, xc = inp["x"][b], inp["ctx"][b]
    z = np.zeros((1, 1), np.float32)
    d = {"xin0": np.concatenate([xc, xl], 0), "Cb": tb["C"], "Sb": tb["S"]}
    d.update(_pref(l0_inputs(inp, b, xl, xc), "a_", ("xin",)))
    d.update(_pref(moe_inputs(inp, 0, b, z, z), "b_", ("xin",)))
    d.update(_pref(dict(zT=pc["zT"], w1=inp["hy_ffn_w1"][j], w2=inp["hy_ffn_w2"][j], w3=inp["hy_ffn_w3"][j],
                        pv=np.stack([inp["hy_ffn_b1"][j], inp["hy_sin_freq"][j][0], inp["hy_ffn_b2"][j], inp["hy_sin_freq"][j][1]], 1).astype(np.float32),
                        absd=pc["absd"], tneg=pc["tneg"], nz=pc["nz"], cfn=tb["cfn"]), "f_"))
    d.update(_pref(l1rw_inputs(inp, b, z, z), "r_", ("xin",)))
    d.update(_pref(hyc_inputs(inp, b, z, z, z, z), "h_", ("xin", "hv", "yrw", "FFd", "Cb", "Sb")))
    d.update(_pref(moe_inputs(inp, 1, b, z, z), "e_", ("xin",)))
    return d


def l0_inputs(inp, b, xl, xc):
    gw2 = np.zeros((32, 512), np.float32)
    gw2[0:16, 0:256] = inp["gla_gate_w2"][0, 0]
    gw2[16:32, 256:512] = inp["gla_gate_w2"][0, 1]
    d = dict(xin=np.concatenate([xc, xl], 0), cT=np.ascontiguousarray(inp["c"][b].reshape(8, 128).T),
             ccT=np.ascontiguousarray(inp["c_ctx"].reshape(8, 128).T),
             mod_w=inp["mod_w"][0], mod_b=inp["mod_b"][0][None], ln_g=inp["ln1_g"][0][None], ln_b=inp["ln1_b"][0][None],
             w_in=inp["ev_w_in"][0], w_out=inp["ev_w_out"][0], gw2=gw2, gb=inp["gla_gate_b"][0].reshape(1, 512),
             gvec=np.concatenate([np.tile(inp["gla_norm_g"][0], 4), np.tile(inp["hg_norm_g"][0], 4)])[None],
             lbl=inp["hg_lb_logits"].reshape(1, 2048))
    d.update(consts_l0())
    return d


def _run(nc, maps):
    res = run_bass_kernel_spmd(nc, maps, core_ids=list(range(len(maps))))
    return res.results


def kernel_unfused(**inputs):
    inp = {k: np.ascontiguousarray(np.asarray(v, dtype=np.float32)) for k, v in inputs.items()}
    B = 8
    xl = [inp["x"][b] for b in range(B)]
    xc = [inp["ctx"][b] for b in range(B)]
    r = _run(build_l0_mixer(), [l0_inputs(inp, b, xl[b], xc[b]) for b in range(B)])
    xc = [r[b]["xout"][:256] for b in range(B)]
    xl = [r[b]["xout"][256:] for b in range(B)]
    moe_nc = build_moe()
    r = _run(moe_nc, [moe_inputs(inp, 0, b, xl[b], xc[b]) for b in range(B)])
    xc = [r[b]["xout"][:256] for b in range(B)]
    xl = [r[b]["xout"][256:] for b in range(B)]
    r = _run(build_hy_filters(), [hyf_inputs(inp, c) for c in range(B)])
    FF = np.ascontiguousarray(np.concatenate([r[c]["FF"] for c in range(B)], -1))
    r = _run(build_l1_rw(), [l1rw_inputs(inp, b, xl[b], xc[b]) for b in range(B)])
    yrw = [r[b]["yrw"] for b in range(B)]
    hv = [r[b]["hv"] for b in range(B)]
    r = _run(build_hy_conv(), [hyc_inputs(inp, b, xl[b], hv[b], yrw[b], FF) for b in range(B)])
    xl = [r[b]["xout"] for b in range(B)]
    r = _run(build_moe(), [moe_inputs(inp, 1, b, xl[b], xc[b]) for b in range(B)])
    out = np.stack([r[b]["xout"][256:] for b in range(B)], 0)
    return out.astype(np.float32)


def kernel(**inputs):
    inp = {k: np.ascontiguousarray(np.asarray(v, dtype=np.float32)) for k, v in inputs.items()}
    B = 8
    nc = build_fused()
    r = _run(nc, [fused_inputs(inp, b) for b in range(B)])
    out = np.stack([r[b]["out"][256:] for b in range(B)], 0)
    return out.astype(np.float32)
```

```python
import numpy as np
from contextlib import ExitStack
import concourse.bass as bass
import concourse.mybir as mybir
from concourse.bass_utils import run_bass_kernel_spmd

F32 = mybir.dt.float32
I32 = mybir.dt.int32
AF = mybir.ActivationFunctionType
ALU = mybir.AluOpType
AX = mybir.AxisListType

BF16 = mybir.dt.bfloat16
ENGS = ["tensor", "vector", "scalar", "gpsimd", "sync"]


def f32r(v):
    return v
NDS = 40


class Dep:
    __slots__ = ("w", "r")

    def __init__(self):
        self.w = None
        self.r = {}


class V:
    __slots__ = ("ap", "dep")

    def __init__(self, ap, dep):
        self.ap = ap
        self.dep = dep

    def __getitem__(self, idx):
        return V(self.ap[idx], self.dep)


class T:
    def __init__(self, h, tracked=True):
        self.h = h
        self.dep = Dep() if tracked else None

    def __getitem__(self, idx):
        return V(self.h[idx], self.dep)

    def v(self, ap):
        return V(ap, self.dep)


class Prog:
    def __init__(self):
        self.nc = bass.Bass("TRN2", target_bir_lowering=False)
        self.es = ExitStack()
        self.streams = {e: [] for e in ENGS}
        self.cnt = {e: 0 for e in ENGS}
        self.sem = {e: self.es.enter_context(self.nc.semaphore("s_" + e)) for e in ENGS}
        self.known = {e: {} for e in ENGS}
        self.dsem = [self.es.enter_context(self.nc.semaphore("d%d" % i)) for i in range(NDS)]
        self.dcnt = [0] * NDS
        self.dnext = 0
        self.nalloc = 0
        self.out_events = []
        self.arena = None
        self.banks = None
        self.aoff = 0
        self.nps = 0

    ARENA = 53200

    def use_arena(self):
        self.arena = self.es.enter_context(self.nc.sbuf_tensor("arena", [128, self.ARENA], F32))
        self.banks = [self.es.enter_context(self.nc.psum_tensor("bank%d" % i, [128, 512], F32)) for i in range(8)]

    def begin_phase(self):
        self.aoff = 0
        self.nps = 0

    def barrier(self):
        evs = [(e, self.cnt[e]) for e in ENGS if self.cnt[e] > 0]
        evs += [(sl, 16 * self.dcnt[sl]) for sl in range(NDS) if self.dcnt[sl] > 0]
        for e in ENGS:
            waits = []
            for k, v in evs:
                if self.known[e].get(k, 0) < v:
                    self.known[e][k] = v
                    waits.append((self._semof(k), v))
            self.streams[e].append((waits, None, None, 0))

    def end_phase(self):
        self.barrier()

    def _nm(self, name):
        self.nalloc += 1
        return "%s_%d" % (name, self.nalloc)

    def sb(self, name, shape, dt=F32):
        if self.arena is None:
            return T(self.es.enter_context(self.nc.sbuf_tensor(self._nm(name), list(shape), dt)))
        n = 1
        for d_ in shape[1:]:
            n *= d_
        words = (n + 1) // 2 if dt == BF16 else n
        n8 = (words + 7) // 8 * 8
        assert self.aoff + n8 <= self.ARENA, "arena overflow at %s: %d + %d" % (name, self.aoff, n8)
        ap = self.arena[0:shape[0], self.aoff:self.aoff + words]
        self.aoff += n8
        if dt != F32:
            ap = ap.bitcast(dt)
            if dt == BF16:
                ap = ap[:, 0:n]
        if len(shape) == 3:
            ap = ap.rearrange("p (a b) -> p a b", b=shape[2])
        return T(ap)

    def ps(self, name, shape, dt=F32):
        if self.banks is None:
            return T(self.es.enter_context(self.nc.psum_tensor(self._nm(name), list(shape), dt)))
        b = self.banks[self.nps]
        self.nps += 1
        return T(b[:, :])

    def dram(self, name, shape, dt=F32, kind="Internal"):
        t = self.nc.dram_tensor(name, list(shape), dt, kind=kind)
        return T(t.ap(), tracked=(kind != "ExternalInput"))

    def _semof(self, key):
        return self.sem[key] if isinstance(key, str) else self.dsem[key]

    def _waits(self, eng, reads, writes):
        need = {}
        for d in reads:
            if d is not None and d.w is not None:
                k, v = d.w
                need[k] = max(need.get(k, 0), v)
        for d in writes:
            if d is None:
                continue
            if d.w is not None:
                k, v = d.w
                need[k] = max(need.get(k, 0), v)
            for k, v in d.r.items():
                need[k] = max(need.get(k, 0), v)
        out = []
        kn = self.known[eng]
        for k, v in need.items():
            if k == eng and eng == "tensor":
                continue
            if kn.get(k, 0) >= v:
                continue
            kn[k] = v
            out.append((self._semof(k), v))
        return out

    def _record(self, ev, reads, writes):
        k, v = ev
        for d in reads:
            if d is not None:
                d.r[k] = max(d.r.get(k, 0), v)
        for d in writes:
            if d is not None:
                d.w = ev
                d.r = {}

    def op(self, eng, fn, outs, ins):
        reads = [x.dep for x in ins if isinstance(x, V)]
        writes = [x.dep for x in outs]
        waits = self._waits(eng, reads, writes)
        self.cnt[eng] += 1
        ev = (eng, self.cnt[eng])
        self.streams[eng].append((waits, fn, self.sem[eng], 1))
        self._record(ev, reads, writes)
        return ev

    def dma(self, out, in_, eng="sync", fn=None, extra_reads=()):
        slot = self.dnext
        self.dnext = (self.dnext + 1) % NDS
        reads = [in_.dep] + [x.dep for x in extra_reads]
        writes = [out.dep]
        waits = self._waits(eng, reads, writes)
        if self.dcnt[slot] > 0:
            pv = 16 * self.dcnt[slot]
            if self.known[eng].get(slot, 0) < pv:
                self.known[eng][slot] = pv
                waits.append((self.dsem[slot], pv))
        self.dcnt[slot] += 1
        ev = (slot, 16 * self.dcnt[slot])
        if fn is None:
            o, i = out.ap, in_.ap
            fn = lambda e, o=o, i=i: e.dma_start(out=o, in_=i)
        self.streams[eng].append((waits, fn, self.dsem[slot], 16))
        self._record(ev, reads, writes)
        return ev

    def finish(self, final_deps):
        waits = self._waits("sync", [d for d in final_deps], [])
        self.streams["sync"].append((waits, None, None, 0))
        with self.nc.Block() as block:
            for e in ENGS:
                stream = self.streams[e]

                def body(engh, stream=stream):
                    for waits, fn, sem, inc in stream:
                        for (s, v) in waits:
                            engh.wait_ge(s, v)
                        if fn is not None:
                            ins = fn(engh)
                            ins.then_inc(sem, inc)

                getattr(block, e)(body)
        self.es.close()
        return self.nc

    def mm(self, out, lhsT, rhs, start=True, stop=True):
        o, a, b = out.ap, lhsT.ap, rhs.ap
        return self.op("tensor", lambda e: e.matmul(o, a, b, start=start, stop=stop), [out], [lhsT, rhs])

    def tr(self, out, in_, ident):
        o, a, b = out.ap, in_.ap, ident.ap
        return self.op("tensor", lambda e: e.transpose(o, a, b), [out], [in_, ident])

    def act(self, out, in_, func, bias=0.0, scale=1.0, eng="scalar", accum_out=None):
        o, a = out.ap, in_.ap
        bb = bias.ap if isinstance(bias, V) else bias
        ss = scale.ap if isinstance(scale, V) else scale
        outs = [out]
        kw = {}
        if accum_out is not None:
            kw["accum_out"] = accum_out.ap
            outs.append(accum_out)
        return self.op("scalar", lambda e: e.activation(o, a, func, bias=bb, scale=ss, **kw), outs, [in_, bias, scale])

    def tt(self, out, a, b, op, eng="vector"):
        o, x, y = out.ap, a.ap, b.ap
        return self.op(eng, lambda e: e.tensor_tensor(o, x, y, op), [out], [a, b])

    def ts(self, out, a, s1, op0, s2=None, op1=None, eng="vector", accum_out=None):
        o, x = out.ap, a.ap
        c1 = s1.ap if isinstance(s1, V) else s1
        c2 = s2.ap if isinstance(s2, V) else s2
        outs = [out]
        kw = {}
        if op1 is not None:
            kw["op1"] = op1
        if accum_out is not None:
            kw["accum_out"] = accum_out.ap
            outs.append(accum_out)
        return self.op(eng, lambda e: e.tensor_scalar(o, x, c1, c2, op0, **kw), outs, [a, s1, s2])

    def stt(self, out, a, s, b, op0, op1, eng="vector"):
        o, x, y = out.ap, a.ap, b.ap
        c = s.ap if isinstance(s, V) else s
        return self.op(eng, lambda e: e.scalar_tensor_tensor(o, x, c, y, op0, op1), [out], [a, s, b])

    def copy(self, out, in_, eng="vector"):
        o, a = out.ap, in_.ap
        if eng == "scalar":
            return self.op(eng, lambda e: e.copy(o, a), [out], [in_])
        return self.op(eng, lambda e: e.tensor_copy(o, a), [out], [in_])

    def memset(self, out, val, eng="vector"):
        o = out.ap
        return self.op(eng, lambda e: e.memset(o, val), [out], [])

    def reduce(self, out, in_, op, axis=None, eng="vector"):
        o, a = out.ap, in_.ap
        ax = AX.X if axis is None else axis
        return self.op(eng, lambda e: e.tensor_reduce(o, a, ax, op), [out], [in_])

    def recip(self, out, in_):
        o, a = out.ap, in_.ap
        return self.op("vector", lambda e: e.reciprocal(o, a), [out], [in_])


D = 1024
ALPHA = 4.0 ** 0.25
NT = 34


def pbc(t, n=128):
    return V(t.h[0].partition_broadcast(n), t.dep)


def modulation(p, cT, mod_w, mod_b_bc, c0, ncols, out, wk, ps, scr):
    sc = scr["sc"]
    lh = scr["lh"]
    p.dma(sc[:], cT[:])
    p.act(sc[:], sc[:], AF.Silu)
    for k in range(8):
        p.copy(lh[:, k, :], V(sc.h[:, k:k + 1].to_broadcast([128, 128]), sc.dep))
    p.dma(out[:, 0:ncols], V(mod_b_bc.ap[:, c0:c0 + ncols], None))
    for n in range(ncols // 512):
        w = wk[n % 2]
        p.dma(w[:], V(mod_w.h[:, c0 + n * 512:c0 + (n + 1) * 512].rearrange("(k q) n -> q k n", q=128), None))
        pp = ps[n % 2]
        for k in range(8):
            p.mm(pp[:], lh[:, k, :], w[:, k, :], start=(k == 0), stop=(k == 7))
        p.tt(out[:, n * 512:(n + 1) * 512], pp[:], out[:, n * 512:(n + 1) * 512], ALU.add)


def layer_norm(p, out, in_, g_bc, b_bc, st, tmp, eps=1e-5, n=1024):
    p.reduce(st[:, 0:1], in_, ALU.add)
    p.ts(st[:, 1:2], st[:, 0:1], -1.0 / n, ALU.mult)
    p.act(tmp, in_, AF.Square, bias=st[:, 1:2], accum_out=st[:, 2:3])
    p.ts(st[:, 3:4], st[:, 2:3], 1.0 / n, ALU.mult, eps, ALU.add)
    p.act(st[:, 3:4], st[:, 3:4], AF.Sqrt)
    p.recip(st[:, 3:4], st[:, 3:4])
    p.ts(tmp, in_, st[:, 1:2], ALU.add, st[:, 3:4], ALU.mult)
    p.tt(tmp, tmp, g_bc, ALU.mult)
    p.tt(out, tmp, b_bc, ALU.add)


def transpose_tile(p, dstT, src, ncol, ident, pst, eng="vector", rnd=False):
    nb = ncol // 128
    for g0 in range(0, nb, 4):
        g1 = min(nb, g0 + 4)
        for g in range(g0, g1):
            p.tr(pst[:, (g - g0) * 128:(g - g0 + 1) * 128], V(src.ap[:, g * 128:(g + 1) * 128], src.dep), ident[:])
        dv = V(dstT.h[:, g0:g1, :].rearrange("p a b -> p (a b)"), dstT.dep)
        p.copy(f32r(dv) if rnd else dv, pst[:, 0:(g1 - g0) * 128], eng=eng)


def consts_l0():
    i = np.arange(128)
    ch = i // 64
    same = (ch[:, None] == ch[None, :])
    triF = (same & (i[:, None] <= i[None, :])).astype(np.float32)
    triB = (same & (i[:, None] >= i[None, :])).astype(np.float32)
    refF = triF[:, ch * 64 + 32]
    lastF = same.astype(np.float32)
    refB = triB[:, ch * 64 + 31]
    cmF = np.concatenate([triF - refF, lastF - triF, triF], 1)
    cmB = np.concatenate([triB - refB, lastF - triB, triB], 1)
    ci = np.stack([(ch == 0), (ch == 1)], 1).astype(np.float32)
    return dict(ident=np.eye(128, dtype=np.float32), cmF=cmF.astype(np.float32), cmB=cmB.astype(np.float32), ci=ci)


def build_l0_mixer(p=None, io=None, pfx=""):
    standalone = p is None
    if standalone:
        p = Prog()
    io = io or {}
    p.begin_phase()
    IN = lambda n, s, dt=F32: io[n] if n in io else p.dram(pfx + n, s, dt, kind="ExternalInput")
    xin = IN("xin", [NT * 128, D])
    cT = IN("cT", [128, 8]); ccT = IN("ccT", [128, 8])
    mod_w = IN("mod_w", [D, 6 * D]); mod_b = IN("mod_b", [1, 6 * D])
    ln_g = IN("ln_g", [1, D]); ln_b = IN("ln_b", [1, D])
    w_in = IN("w_in", [D, 4128]); w_out = IN("w_out", [D, D])
    gw2 = IN("gw2", [32, 512])
    gb = IN("gb", [1, 512])
    gvec = IN("gvec", [1, D])
    lbl = IN("lbl", [1, 2048])
    ident_d = IN("ident", [128, 128]); cmF_d = IN("cmF", [128, 384]); cmB_d = IN("cmB", [128, 384]); ci_d = IN("ci", [128, 2])
    xout = io["xout"] if "xout" in io else p.dram("xout", [NT * 128, D], kind="ExternalOutput")
    SC = p.dram(pfx + "SC", [NT, 128, 4352])
    OF = p.dram(pfx + "OF", [NT, 128, D])
    sc_deps = [Dep() for _ in range(NT)]
    of_deps = [Dep() for _ in range(NT)]
    out_deps = [Dep() for _ in range(NT)]

    S = lambda n, s: p.sb(n, s)
    ident = S("ident", [128, 128]); cm = [S("cmF", [128, 384]), S("cmB", [128, 384])]; ci = S("ci", [128, 2])
    p.dma(ident[:], ident_d[:]); p.dma(cm[0][:], cmF_d[:]); p.dma(cm[1][:], cmB_d[:]); p.dma(ci[:], ci_d[:])
    modl = S("modl", [128, 3 * D]); modc = S("modc", [128, 3 * D])
    wk = [S("wk0", [128, 8, 512]), S("wk1", [128, 8, 512])]
    pA = [p.ps("pA0", [128, 512]), p.ps("pA1", [128, 512])]
    pT = p.ps("pT", [128, 512]); pS = p.ps("pS", [128, 512]); pO = [p.ps("pO0", [128, 512]), p.ps("pO1", [128, 512])]
    pKV = p.ps("pKV", [128, 512]); pD = p.ps("pD", [128, 512])
    scr = dict(sc=S("sc", [128, 8]), lh=S("lh", [128, 8, 128]))
    mbb = pbc(mod_b)
    modulation(p, cT, mod_w, mbb, 0, 3 * D, modl, wk, pA, scr)
    modulation(p, ccT, mod_w, mbb, 0, 3 * D, modc, wk, pA, scr)
    for m in (modl, modc):
        p.ts(m[:, D:2 * D], m[:, D:2 * D], 1.0, ALU.add)
    gbt = S("gbt", [128, 512]); p.dma(gbt[:], pbc(gb))
    gw2t = S("gw2t", [32, 512]); p.dma(gw2t[:], gw2[:])
    gv = S("gv", [128, D]); p.dma(gv[:], pbc(gvec))
    lng = S("lng", [128, D]); p.dma(lng[:], pbc(ln_g)); lnb = S("lnb", [128, D]); p.dma(lnb[:], pbc(ln_b))
    Z = S("Z", [128, 4128])
    p.dma(Z[:, 0:2048], pbc(lbl))
    oml = S("oml", [128, 1024])
    p.tt(oml[:], Z[:, 1024:2048], Z[:, 0:1024], ALU.subtract)
    p.act(oml[:], oml[:], AF.Sigmoid)

    xt = S("xt", [128, D]); hT = S("hT", [128, 8, 128])
    aT = S("aT", [32, 128])
    Q = S("Q", [128, 768]); Kd = [S("K0", [128, 768]), S("K1", [128, 768])]; LAd = [S("LA0", [128, 768]), S("LA1", [128, 768])]
    Vv = S("Vv", [128, D]); G = S("G", [128, D])
    E = [S("E%d" % i, [128, 768]) for i in range(3)]
    qp = S("qp", [128, 768]); kp = S("kp", [128, 768]); qi = S("qi", [128, 768]); ko = S("ko", [128, 768])
    qpT = S("qpT", [128, 6, 128]); kpT = S("kpT", [128, 6, 128]); qiT = S("qiT", [128, 6, 128])
    dec = S("dec", [128, 12]); PT = S("PT", [128, 128]); ot = S("ot", [128, D]); of = S("of", [128, D])
    Sst = [[S("S%d_%d" % (d, g), [128, 128]) for g in range(6)] for d in range(2)]
    st = S("st", [128, 16]); tmp = S("tmp", [128, D]); ht = tmp; yn = S("yn", [128, D]); ynT = S("ynT", [128, 8, 128])
    for d in range(2):
        for g in range(6):
            p.memset(Sst[d][g][:], 0.0)

    def gates(i):
        p.tr(pT[0:32, 0:128], Z[:, 1536:1568], ident[:])
        p.copy(aT[:], pT[0:32, 0:128])
        p.mm(pA[0][:], aT[:], gw2t[:])
        p.tt(E[0][:, 0:512], pA[0][:], gbt[:], ALU.add)
        p.act(E[0][:, 0:512], E[0][:, 0:512], AF.Exp, scale=-1.0)
        p.act(E[0][:, 0:512], E[0][:, 0:512], AF.Ln, bias=1.0)
        for d in range(2):
            p.ts(LAd[d][:, 0:256], E[0][:, d * 256:(d + 1) * 256], -1.0 / 16.0, ALU.mult)
            p.copy(Kd[d][:, 0:256], Z[:, 256:512], eng="gpsimd")
            p.act(Kd[d][:, 256:768], Z[:, 2080 + 512 * d:2592 + 512 * d], AF.Sigmoid, scale=-1.0)
            p.tt(Kd[d][:, 256:768], Kd[d][:, 256:768], oml[:, 512 * d:512 * (d + 1)], ALU.mult)
            p.act(LAd[d][:, 256:768], Kd[d][:, 256:768], AF.Ln, scale=-1.0, bias=1.0)
        p.ts(Q[:, 0:256], Z[:, 0:256], 0.125, ALU.mult)
        p.act(Q[:, 256:768], Z[:, 1568:2080], AF.Silu)
        p.copy(Vv[:, 0:512], Z[:, 512:1024], eng="gpsimd")
        p.copy(Vv[:, 512:1024], Z[:, 3104:3616], eng="gpsimd")
        p.act(G[:, 0:512], Z[:, 1024:1536], AF.Silu)
        p.act(G[:, 512:1024], Z[:, 3616:4128], AF.Silu)

    def recur(i, d):
        la, kk = LAd[d], Kd[d]
        for e in range(3):
            for (c0, c1) in ((0, 512), (512, 768)):
                pp = pA[(e + (c0 > 0)) % 2]
                p.mm(pp[:, 0:c1 - c0], cm[d][:, e * 128:(e + 1) * 128], la[:, c0:c1])
                p.copy(E[e][:, c0:c1], pp[:, 0:c1 - c0], eng="scalar")
        for g in range(6):
            p.mm(pD[:, g * 2:g * 2 + 2], la[:, g * 128:(g + 1) * 128], ci[:], start=True, stop=True)
        p.act(dec[:], pD[:, 0:12], AF.Exp)
        p.act(qp[:], E[0][:], AF.Exp); p.tt(qp[:], qp[:], Q[:], ALU.mult)
        p.act(kp[:], E[0][:], AF.Exp, scale=-1.0); p.tt(kp[:], kp[:], kk[:], ALU.mult)
        p.act(ko[:], E[1][:], AF.Exp); p.tt(ko[:], ko[:], kk[:], ALU.mult)
        p.act(qi[:], E[2][:], AF.Exp); p.tt(qi[:], qi[:], Q[:], ALU.mult)
        transpose_tile(p, qpT, qp[:], 768, ident, pT)
        transpose_tile(p, kpT, kp[:], 768, ident, pT)
        transpose_tile(p, qiT, qi[:], 768, ident, pT)
        chunks = (0, 1) if d == 0 else (1, 0)
        for hh in range(8):
            if hh < 4:
                g, base, dk = hh // 2, (hh % 2) * 64, 64
            else:
                g, base, dk = hh - 2, 0, 128
            vc = slice(hh * 128, (hh + 1) * 128)
            ob = pO[hh // 4]
            oc = slice((hh % 4) * 128, (hh % 4 + 1) * 128)
            rows = slice(base, base + dk)
            p.mm(pS[:, 0:128], kpT[rows, g, :], qpT[rows, g, :])
            p.tt(PT[:], pS[:, 0:128], cm[d][:, 256:384], ALU.mult)
            p.mm(ob[:, oc], PT[:], Vv[:, vc], start=True, stop=False)
            Sg = Sst[d][g]
            for n, c in enumerate(chunks):
                tk = slice(c * 64, (c + 1) * 64)
                p.mm(ob[tk, oc], qiT[rows, g, tk], Sg[rows, :], start=False, stop=(n == 1))
                kvs = slice((hh % 2) * 256 + n * 128, (hh % 2) * 256 + (n + 1) * 128)
                p.mm(pKV[rows, kvs], ko[tk, g * 128 + base:g * 128 + base + dk], Vv[tk, vc])
                p.stt(Sg[rows, :], Sg[rows, :], dec[rows, g * 2 + c:g * 2 + c + 1], pKV[rows, kvs], ALU.mult, ALU.add)

    order_f = list(range(NT))
    order_b = [1, 0] + list(range(NT - 1, 1, -1))
    for i in order_f:
        mod = modc if i < 2 else modl
        p.dma(xt[:], xin[i * 128:(i + 1) * 128, :])
        p.tt(ht[:], xt[:], mod[:, D:2 * D], ALU.mult)
        p.tt(ht[:], ht[:], mod[:, 0:D], ALU.add)
        transpose_tile(p, hT, ht[:], D, ident, pT)
        for n in range(9):
            c0 = n * 512; c1 = min(4128, c0 + 512); w = wk[n % 2]
            p.dma(V(w.h[:, :, 0:c1 - c0], w.dep), V(w_in.h[:, c0:c1].rearrange("(k q) n -> q k n", q=128), None))
            pp = pA[n % 2]
            for k in range(8):
                p.mm(pp[:, 0:c1 - c0], hT[:, k, :], V(w.h[:, k, 0:c1 - c0], w.dep), start=(k == 0), stop=(k == 7))
            p.copy(Z[:, c0:c1], pp[:, 0:c1 - c0], eng="scalar")
        gates(i)
        scd = sc_deps[i]
        p.dma(V(SC.h[i, :, 0:768], scd), Q[:]); p.dma(V(SC.h[i, :, 768:1536], scd), Kd[1][:])
        p.dma(V(SC.h[i, :, 1536:2304], scd), LAd[1][:]); p.dma(V(SC.h[i, :, 2304:3328], scd), Vv[:])
        p.dma(V(SC.h[i, :, 3328:4352], scd), G[:])
        recur(i, 0)
        p.copy(of[:, 0:512], pO[0][:], eng="scalar"); p.copy(of[:, 512:1024], pO[1][:], eng="scalar")
        p.dma(V(OF.h[i], of_deps[i]), of[:])
    for i in order_b:
        mod = modc if i < 2 else modl
        scd = sc_deps[i]
        p.dma(Q[:], V(SC.h[i, :, 0:768], scd)); p.dma(Kd[1][:], V(SC.h[i, :, 768:1536], scd))
        p.dma(LAd[1][:], V(SC.h[i, :, 1536:2304], scd)); p.dma(Vv[:], V(SC.h[i, :, 2304:3328], scd))
        p.dma(G[:], V(SC.h[i, :, 3328:4352], scd))
        p.dma(of[:], V(OF.h[i], of_deps[i]))
        p.dma(xt[:], xin[i * 128:(i + 1) * 128, :])
        recur(i, 1)
        p.tt(ot[:, 0:512], pO[0][:], of[:, 0:512], ALU.add); p.tt(ot[:, 512:1024], pO[1][:], of[:, 512:1024], ALU.add)
        p.tt(tmp[:], ot[:], ot[:], ALU.mult)
        p.reduce(st[:, 0:8], V(tmp.h[:].rearrange("p (a b) -> p a b", b=128), tmp.dep), ALU.add)
        p.ts(st[:, 0:8], st[:, 0:8], 1.0 / 128, ALU.mult, 1e-6, ALU.add)
        p.act(st[:, 0:8], st[:, 0:8], AF.Sqrt)
        p.recip(st[:, 0:8], st[:, 0:8])
        p.tt(V(yn.h[:].rearrange("p (a b) -> p a b", b=128), yn.dep), V(ot.h[:].rearrange("p (a b) -> p a b", b=128), ot.dep),
             V(st.h[:, 0:8].to_broadcast([128, 8, 128]), st.dep), ALU.mult)
        p.tt(yn[:], yn[:], gv[:], ALU.mult)
        p.tt(yn[:], yn[:], G[:], ALU.mult)
        transpose_tile(p, ynT, yn[:], D, ident, pT)
        for n in range(2):
            p.dma(wk[n][:], V(w_out.h[:, n * 512:(n + 1) * 512].rearrange("(k q) n -> q k n", q=128), None))
            for k in range(8):
                p.mm(pA[n][:], ynT[:, k, :], wk[n][:, k, :], start=(k == 0), stop=(k == 7))
            p.tt(tmp[:, n * 512:(n + 1) * 512], pA[n][:], mod[:, 2 * D + n * 512:2 * D + (n + 1) * 512], ALU.mult)
        p.stt(ot[:], xt[:], ALPHA, tmp[:], ALU.mult, ALU.add)
        layer_norm(p, yn[:], ot[:], lng[:], lnb[:], V(st.h[:, 8:12], st.dep), tmp[:])
        p.dma(V(xout.h[i * 128:(i + 1) * 128, :], out_deps[i]), yn[:])
    if standalone:
        return p.finish(out_deps)
    p.end_phase()


SKIPW = False
NB = NT * 8 + 256


def consts_moe():
    i = np.arange(128)
    triU = (i[:, None] <= i[None, :]).astype(np.float32)
    io13 = (np.arange(8)[None, :] * 128 + i[:, None]).astype(np.float32)
    io2 = (np.arange(2)[None, :] * 128 + i[:, None]).astype(np.float32)
    return dict(ident=np.eye(128, dtype=np.float32), triU=triU, ones=np.ones((128, 128), np.float32),
                bvals=(np.arange(NB, dtype=np.float32) * 128)[None], io13=io13, io2=io2)


def build_moe(dbg=99, p=None, io=None, pfx=""):
    standalone = p is None
    if standalone:
        p = Prog()
    io = io or {}
    p.begin_phase()
    IN = lambda n, s, dt=F32: io[n] if n in io else p.dram(pfx + n, s, dt, kind="ExternalInput")
    xin = IN("xin", [NT * 128, D])
    cT = IN("cT", [128, 8]); ccT = IN("ccT", [128, 8])
    mod_w = IN("mod_w", [D, 6 * D]); mod_b = IN("mod_b", [1, 6 * D])
    ln_g = IN("ln_g", [1, D]); ln_b = IN("ln_b", [1, D])
    rw = IN("rw", [D, 256]); rbias = IN("rbias", [1, 256])
    W13 = IN("w13", [256 * 1024, 512]); W2 = IN("w2", [256 * 256, 1024])
    sw13 = IN("sw13", [D, 512]); sw2 = IN("sw2", [256, D])
    ident_d = IN("ident", [128, 128]); triU_d = IN("triU", [128, 128]); ones_d = IN("ones", [128, 128])
    bvals_d = IN("bvals", [1, NB]); io13_d = IN("io13", [128, 8]); io2_d = IN("io2", [128, 2])
    xout = io["xout"] if "xout" in io else p.dram("xout", [NT * 128, D], kind="ExternalOutput")
    U = io["U"] if "U" in io else p.dram("U", [NT, 128, D]); RG = io["RG"] if "RG" in io else p.dram("RG", [NT, 128, 512])
    HB = NB // 2; HR = HB * 128
    XS = io["XS"] if "XS" in io else [p.dram("XSa", [HR, D]), p.dram("XSb", [HR, D])]; YS = io["YS"] if "YS" in io else [p.dram("YSa", [HR, D]), p.dram("YSb", [HR, D])]
    u_deps = [Dep() for _ in range(NT)]; rg_deps = [Dep() for _ in range(NT)]; out_deps = [Dep() for _ in range(NT)]

    if dbg == 0:
        return p.finish([])
    S = lambda n, s, dt=F32: p.sb(n, s, dt)
    ident = S("ident", [128, 128]); triU = S("triU", [128, 128]); ones = S("ones", [128, 128])
    p.dma(ident[:], ident_d[:]); p.dma(triU[:], triU_d[:]); p.dma(ones[:], ones_d[:])
    io13 = S("io13", [128, 8]); io2 = S("io2", [128, 2]); p.dma(io13[:], io13_d[:]); p.dma(io2[:], io2_d[:])
    bv = S("bv", [128, NB]); p.dma(bv[:], pbc(bvals_d))
    modl = S("modl", [128, 3 * D]); modc = S("modc", [128, 3 * D])
    w13t = [S("w13t0", [128, 8, 512]), S("w13r", [128, 8, 512], BF16), S("w13t1", [128, 8, 512])]
    w2t = [S("w2t0", [128, 2, 1024]), S("w2r", [128, 2, 1024], BF16), S("w2t1", [128, 2, 1024])]
    pA = [p.ps("pA0", [128, 512]), p.ps("pA1", [128, 512])]
    pT = p.ps("pT", [128, 512]); pH = p.ps("pH", [128, 512]); pY = [p.ps("pY0", [128, 512]), p.ps("pY1", [128, 512])]
    scr = dict(sc=S("sc", [128, 8]), lh=S("lh", [128, 8, 128]))
    mbb = pbc(mod_b)
    modulation(p, cT, mod_w, mbb, 3 * D, 3 * D, modl, [w13t[0], w13t[0]], pA, scr)
    modulation(p, ccT, mod_w, mbb, 3 * D, 3 * D, modc, [w13t[0], w13t[0]], pA, scr)
    for m in (modl, modc):
        p.ts(m[:, D:2 * D], m[:, D:2 * D], 1.0, ALU.add)
    lng = S("lng", [128, D]); p.dma(lng[:], pbc(ln_g)); lnb = S("lnb", [128, D]); p.dma(lnb[:], pbc(ln_b))
    rwt = S("rwt", [128, 8, 256]); p.dma(rwt[:], V(rw.h.rearrange("(k q) n -> q k n", q=128), None))
    rbt = S("rbt", [128, 256]); p.dma(rbt[:], pbc(rbias))

    xt = S("xt", [128, D]); ut = S("ut", [128, D]); uT = S("uT", [128, 8, 128])
    sc = S("scs", [128, 256]); sel = S("sel", [128, 256]); selm = S("selm", [128, 256]); M = S("M", [128, 256])
    Macc = S("Macc", [128, 256]); RGt = S("RGt", [128, 512]); t256 = S("t256", [128, 256])
    m8g = S("m8g", [128, 8, 8]); m8 = S("m8", [128, 8]); grp = S("grp", [128, 8]); gm = S("gm", [128, 8]); pen = S("pen", [128, 8])
    st = S("st", [128, 16])
    gk = S("gk", [128, NT, 8]); d8 = S("d8", [128, 8]); dsti = [S("dstia", [128, NT, 8], I32), S("dstib", [128, NT, 8], I32)]
    d8b = S("d8b", [128, 8]); ge8 = S("ge8", [128, 8])
    p.memset(Macc[:], 0.0)

    def vmax(out, in_):
        o, a = out.ap, in_.ap
        p.op("vector", lambda e: e.max(o, a), [out], [in_])

    _rc = {}

    def ereg(e):
        if "e" not in _rc:
            r = e.alloc_register(pfx + "ereg")
            e.reg_mov(r, 256 * 128 - 1)
            _rc["e"] = r
        return _rc["e"]

    def bcreg(e):
        if "r" not in _rc:
            r = e.alloc_register(pfx + "bcreg")
            e.reg_mov(r, HR - 1)
            _rc["r"] = r
        return _rc["r"]

    def bc3(v, shape):
        return V(v.ap.to_broadcast(shape), v.dep)

    zt = V(w13t[0].h[:].rearrange("p a b -> p (a b)"), w13t[0].dep)
    p.memset(zt, 0.0)
    for h in range(2):
        XSv = XS[h].h.rearrange("(n q f) d -> n q (f d)", q=128, f=4)
        for n in range(HR // 512):
            p.dma(V(XSv[n], XS[h].dep), zt)

    for i in range(NT):
        mod = modc if i < 2 else modl
        p.dma(xt[:], xin[i * 128:(i + 1) * 128, :])
        p.tt(ut[:], xt[:], mod[:, D:2 * D], ALU.mult)
        p.tt(ut[:], ut[:], mod[:, 0:D], ALU.add)
        p.dma(V(U.h[i], u_deps[i]), ut[:])
        transpose_tile(p, uT, ut[:], D, ident, pT)
        for k in range(8):
            p.mm(pA[0][:, 0:256], uT[:, k, :], rwt[:, k, :], start=(k == 0), stop=(k == 7))
        p.act(sc[:], pA[0][:, 0:256], AF.Sigmoid)
        p.tt(sel[:], sc[:], rbt[:], ALU.add)
        for g in range(8):
            vmax(m8g[:, g, :], sel[:, g * 32:(g + 1) * 32])
        p.tt(grp[:], m8g[:, :, 0], m8g[:, :, 1], ALU.add)
        vmax(m8[:], grp[:])
        p.ts(gm[:], grp[:], m8[:, 3:4], ALU.is_ge)
        p.ts(pen[:], gm[:], -1.0, ALU.add, 1e30, ALU.mult)
        s3 = V(sel.h[:].rearrange("p (a b) -> p a b", b=32), sel.dep)
        sm3 = V(selm.h[:].rearrange("p (a b) -> p a b", b=32), selm.dep)
        p.tt(sm3, s3, V(gm.h[:].to_broadcast([128, 8, 32]), gm.dep), ALU.mult)
        p.tt(sm3, sm3, V(pen.h[:].to_broadcast([128, 8, 32]), pen.dep), ALU.add)
        vmax(m8[:], selm[:])
        p.ts(M[:], selm[:], m8[:, 7:8], ALU.is_ge)
        p.tt(t256[:], M[:], sc[:], ALU.mult)
        p.reduce(st[:, 0:1], t256[:], ALU.add)
        p.recip(st[:, 0:1], st[:, 0:1])
        p.ts(RGt[:, 256:512], t256[:], st[:, 0:1], ALU.mult, 2.5, ALU.mult)
        p.mm(pA[1][:, 0:256], ones[:], Macc[:], start=True, stop=False)
        p.mm(pA[1][:, 0:256], triU[:], M[:], start=False, stop=True)
        p.tt(RGt[:, 0:256], M[:], pA[1][:, 0:256], ALU.mult)
        p.tt(Macc[:], Macc[:], M[:], ALU.add)
        p.dma(V(RG.h[i], rg_deps[i]), RGt[:])

    if dbg == 1:
        return p.finish(rg_deps)
    cnt = S("cnt", [128, 256]); cnti = S("cnti", [128, 256], I32); pe = [S("pe0", [128, 256]), S("pe1", [128, 256])]
    pstart = S("pstart", [128, 256])
    p.mm(pA[0][:, 0:256], ones[:], Macc[:])
    p.ts(cnt[:], pA[0][:, 0:256], 127.0, ALU.add)
    p.copy(cnti[:], cnt[:])
    p.ts(cnti[:], cnti[:], 7, ALU.arith_shift_right, 7, ALU.logical_shift_left)
    p.copy(cnt[:], cnti[:])
    p.copy(pe[0][:], cnt[:])
    cur = 0
    for sft in (1, 2, 4, 8, 16, 32, 64, 128):
        nxt = 1 - cur
        p.copy(pe[nxt][:, 0:sft], pe[cur][:, 0:sft])
        p.tt(pe[nxt][:, sft:256], pe[cur][:, sft:256], pe[cur][:, 0:256 - sft], ALU.add)
        cur = nxt
    pend = pe[cur]
    p.tt(pstart[:], pend[:], cnt[:], ALU.subtract)
    be = S("be", [128, NB])
    cmp_ = V(w13t[0].h[:].rearrange("p a b -> p (a b)"), w13t[0].dep)
    for c0 in range(0, NB, 16):
        cv = V(cmp_.ap.rearrange("p (a b) -> p a b", b=256), cmp_.dep)
        p.tt(cv, V(pend.h[:, None, :].to_broadcast([128, 16, 256]), pend.dep),
             V(bv.h[:, c0:c0 + 16, None].to_broadcast([128, 16, 256]), bv.dep), ALU.is_le)
        p.reduce(be[:, c0:c0 + 16], cv, ALU.add)
    idxE = S("idxE", [128, NB], I32)
    p.ts(be[:], be[:], 128.0, ALU.mult, io13[:, 0:1], ALU.add)
    p.copy(idxE[:], be[:])

    if dbg == 2:
        return p.finish([idxE.dep])
    for i in range(NT):
        p.dma(RGt[:], V(RG.h[i], rg_deps[i]))
        p.dma(ut[:], V(U.h[i], u_deps[i]))
        p.stt(t256[:], RGt[:, 0:256], 0.0, pstart[:], ALU.is_gt, ALU.mult)
        p.tt(sel[:], t256[:], RGt[:, 0:256], ALU.add)
        vmax(d8[:], sel[:])
        for k in range(8):
            p.stt(t256[:], sel[:], d8[:, k:k + 1], RGt[:, 256:512], ALU.is_equal, ALU.mult)
            p.reduce(gk[:, i, k:k + 1], t256[:], ALU.add)
        p.ts(d8[:], d8[:], -1.0, ALU.add)
        p.copy(dsti[0][:, i, :], d8[:])
        p.ts(ge8[:], d8[:], float(HR), ALU.is_ge)
        p.ts(d8b[:], d8[:], -float(HR) - 1e6, ALU.add)
        p.tt(d8b[:], d8b[:], ge8[:], ALU.mult)
        p.ts(d8b[:], d8b[:], 1e6, ALU.add)
        p.copy(dsti[1][:, i, :], d8b[:])
        for k in range(8):
            for h in range(2):
                oap, iap, src = XS[h].h[:, :], dsti[h].h[:, i, k:k + 1], ut.h[:, :]
                p.dma(XS[h][:], ut[:], eng="gpsimd", extra_reads=[dsti[h][:]],
                      fn=lambda e, oap=oap, iap=iap, src=src: e.indirect_dma_start(
                          out=oap, out_offset=bass.IndirectOffsetOnAxis(ap=iap, axis=0), in_=src, in_offset=None,
                          bounds_check=bcreg(e), oob_is_err=False))

    if dbg == 3:
        return p.finish([XS[0].dep, XS[1].dep])
    xs = [S("xs0", [128, D]), S("xs1", [128, D])]; xT = S("xT", [128, 8, 128], BF16)
    a1 = S("a1", [128, 256]); actT = S("actT", [128, 2, 128], BF16); actF = S("actF", [128, 2, 128]); yb = [S("yb0", [128, D]), S("yb1", [128, D])]
    for b in range(NB):
        w13b, w2b, xsb, ybb = w13t[(b % 2) * 2], w2t[(b % 2) * 2], xs[b % 2], yb[b % 2]
        w13r, w2r = w13t[1], w2t[1]
        bh, bl = b // HB, b % HB
        p.dma(xsb[:], XS[bh][bl * 128:(bl + 1) * 128, :])
        for (wb, Wsrc, width) in ((w13b, W13, 4096), (w2b, W2, 2048)):
            oap, iap, src = wb.h[:].rearrange("p a b -> p (a b)"), idxE.h[:, b:b + 1], Wsrc.h.rearrange("(r j) n -> r (j n)", r=256 * 128)
            p.dma(wb[:], Wsrc[:], eng="gpsimd", extra_reads=[idxE[:]],
                  fn=lambda e, oap=oap, iap=iap, src=src: e.indirect_dma_start(
                      out=oap, out_offset=None, in_=src, in_offset=bass.IndirectOffsetOnAxis(ap=iap, axis=0),
                      bounds_check=ereg(e), oob_is_err=False))
        xs3 = xsb.h[:].rearrange("t (q r) -> t r q", r=8)
        for g0 in (0, 4):
            for g in range(g0, g0 + 4):
                p.tr(pT[:, (g - g0) * 128:(g - g0 + 1) * 128], V(xs3[:, g, :], xsb.dep), ident[:])
            p.copy(V(xT.h[:, g0:g0 + 4, :].rearrange("p a b -> p (a b)"), xT.dep), pT[:], eng="scalar")
        p.copy(f32r(w13r[:]), w13b[:], eng="scalar")
        p.copy(f32r(w2r[:]), w2b[:], eng="vector")
        for m in range(4):
            for k in range(8):
                wsel = w13r.h[:, k, (m // 2) * 256:(m // 2 + 1) * 256].rearrange("p (c r) -> p r c", r=2)[:, m % 2, :]
                p.mm(pH[:, m * 128:(m + 1) * 128], V(wsel, w13r.dep), xT[:, k, :], start=(k == 0), stop=(k == 7))
        p.act(a1[:], pH[:, 0:256], AF.Silu)
        p.tt(f32r(V(actT.h[:].rearrange("p a b -> p (a b)"), actT.dep)), a1[:], pH[:, 256:512], ALU.mult)
        for n in range(2):
            for k in range(2):
                p.mm(pY[n][:], f32r(actT[:, k, :]), f32r(w2r[:, k, n * 512:(n + 1) * 512]), start=(k == 0), stop=(k == 1))
        p.copy(ybb[:, 0:512], pY[0][:], eng="scalar")
        p.copy(ybb[:, 512:1024], pY[1][:], eng="vector")
        p.dma(YS[bh][bl * 128:(bl + 1) * 128, :], ybb[:])

    if dbg == 4:
        return p.finish([YS[0].dep, YS[1].dep])
    acc = yb[1]; yg = xs; tmp = yb[0]
    sw13t = w13t[0]; p.dma(sw13t[:], V(sw13.h.rearrange("(k q) n -> q k n", q=128), None))
    sw2t = w2t[0]; p.dma(sw2t[:], V(sw2.h.rearrange("(k q) n -> q k n", q=128), None))
    for i in range(NT):
        mod = modc if i < 2 else modl
        p.dma(ut[:], V(U.h[i], u_deps[i]))
        p.dma(xt[:], xin[i * 128:(i + 1) * 128, :])
        transpose_tile(p, uT, ut[:], D, ident, pT)
        for m in range(4):
            for k in range(8):
                p.mm(pH[:, m * 128:(m + 1) * 128], sw13t[:, k, m * 128:(m + 1) * 128], uT[:, k, :], start=(k == 0), stop=(k == 7))
        p.act(a1[:], pH[:, 0:256], AF.Silu)
        p.tt(V(actF.h[:].rearrange("p a b -> p (a b)"), actF.dep), a1[:], pH[:, 256:512], ALU.mult)
        for n in range(2):
            for k in range(2):
                p.mm(pY[n][:], actF[:, k, :], sw2t[:, k, n * 512:(n + 1) * 512], start=(k == 0), stop=(k == 1))
        p.copy(acc[:, 0:512], pY[0][:], eng="scalar")
        p.copy(acc[:, 512:1024], pY[1][:], eng="scalar")
        for k in range(8):
            ygk = yg[k % 2]
            for h in range(2):
                oap, iap, src = ygk.h[:, :], dsti[h].h[:, i, k:k + 1], YS[h].h[:, :]
                p.dma(ygk[:], YS[h][:], eng="gpsimd", extra_reads=[dsti[h][:]],
                      fn=lambda e, oap=oap, iap=iap, src=src: e.indirect_dma_start(
                          out=oap, out_offset=None, in_=src, in_offset=bass.IndirectOffsetOnAxis(ap=iap, axis=0),
                          bounds_check=bcreg(e), oob_is_err=False))
            p.stt(acc[:], ygk[:], gk[:, i, k:k + 1], acc[:], ALU.mult, ALU.add)
        p.tt(acc[:], acc[:], mod[:, 2 * D:3 * D], ALU.mult)
        p.stt(acc[:], xt[:], ALPHA, acc[:], ALU.mult, ALU.add)
        layer_norm(p, ut[:], acc[:], lng[:], lnb[:], V(st.h[:, 8:12], st.dep), tmp[:])
        p.dma(V(xout.h[i * 128:(i + 1) * 128, :], out_deps[i]), ut[:])
    if standalone:
        return p.finish(out_deps)
    p.end_phase()


def moe_inputs(inp, l, b, xl, xc):
    d = dict(xin=np.concatenate([xc, xl], 0), cT=np.ascontiguousarray(inp["c"][b].reshape(8, 128).T),
             ccT=np.ascontiguousarray(inp["c_ctx"].reshape(8, 128).T), mod_w=inp["mod_w"][l], mod_b=inp["mod_b"][l][None],
             ln_g=inp["ln2_g"][l][None], ln_b=inp["ln2_b"][l][None], rw=inp["router_w"][l], rbias=inp["router_bias"][l][None],
             w13=inp["exp_w13"][l].reshape(256 * 1024, 512), w2=inp["exp_w2"][l].reshape(256 * 256, 1024),
             sw13=inp["sh_w13"][l], sw2=inp["sh_w2"][l])
    d.update(consts_moe())
    return d


RWW = 1856
DBGT = 0


def consts_rw():
    i = np.arange(128)
    ch = i // 64
    same = (ch[:, None] == ch[None, :])
    triF = (same & (i[:, None] <= i[None, :])).astype(np.float32)
    triB = (same & (i[:, None] >= i[None, :])).astype(np.float32)
    eye = np.eye(128, dtype=np.float32)
    last = same.astype(np.float32)
    out = dict(ident=eye, ci=np.stack([(ch == 0), (ch == 1)], 1).astype(np.float32))
    for nm, tri in (("F", triF), ("B", triB)):
        ms = tri - eye
        out["cm" + nm] = np.concatenate([tri, last - tri], 1)
        out["mk1" + nm] = np.concatenate([ms, tri], 1)
        out["mk2" + nm] = np.concatenate([-ms, -tri], 1)
        out["mk3" + nm] = np.ascontiguousarray(-ms.T)
    out["mlr"] = np.stack([(i % 64 != 0), (i % 64 != 63)], 1).astype(np.float32)
    return out


def build_l1_rw(dbg=99, p=None, io=None, pfx=""):
    standalone = p is None
    if standalone:
        p = Prog()
    io = io or {}
    p.begin_phase()
    IN = lambda n, s, dt=F32: io[n] if n in io else p.dram(pfx + n, s, dt, kind="ExternalInput")
    xin = IN("xin", [NT * 128, D])
    cT = IN("cT", [128, 8]); ccT = IN("ccT", [128, 8])
    mod_w = IN("mod_w", [D, 6 * D]); mod_b = IN("mod_b", [1, 6 * D])
    w_in = IN("w_in", [D, 3392])
    cw = IN("cw", [3, 1536]); cb = IN("cb", [1, 1536]); mu_d = IN("mu", [1, RWW])
    w0_d = IN("w0", [1, 1024]); w2_d = IN("w2", [128, 512]); a0_d = IN("a0", [1, 512]); a2_d = IN("a2", [64, 512]); g2_d = IN("g2", [128, 512])
    vec_d = IN("vecs", [5, 512])
    cst = {k: IN(k, list(v.shape)) for k, v in consts_rw().items()}
    yrw = io["yrw"] if "yrw" in io else p.dram("yrw", [4096, 512], kind="ExternalOutput")
    hv = io["hv"] if "hv" in io else p.dram("hv", [4096, 1536], kind="ExternalOutput")
    PH = p.dram(pfx + "PH", [4096 + 2, 1536]); PRc = p.dram(pfx + "PRc", [256 + 2, RWW]); PRl = p.dram(pfx + "PRl", [4096 + 128, RWW])
    SC = p.dram(pfx + "SC2", [NT, 128, 3584]); OF = p.dram(pfx + "OF2", [NT, 128, 512])
    sc_deps = [Dep() for _ in range(NT)]; of_deps = [Dep() for _ in range(NT)]
    out_deps = [Dep() for _ in range(64)]

    S = lambda n, s, dt=F32: p.sb(n, s, dt)
    C = {}
    for k in ("ident", "ci", "cmF", "cmB", "mk1F", "mk1B", "mk2F", "mk2B", "mk3F", "mk3B", "mlr"):
        C[k] = S(k, list(cst[k].h.shape)); p.dma(C[k][:], cst[k][:])
    ident = C["ident"]; ci = C["ci"]; cm = [C["cmF"], C["cmB"]]; mk1 = [C["mk1F"], C["mk1B"]]; mk2 = [C["mk2F"], C["mk2B"]]; mk3 = [C["mk3F"], C["mk3B"]]
    mlr = C["mlr"]
    modl = S("modl", [128, 2 * D]); modc = S("modc", [128, 2 * D])
    wk = [S("wk0", [128, 8, 512]), S("wk1", [128, 8, 512])]
    pA = [p.ps("pA0", [128, 512]), p.ps("pA1", [128, 512])]
    pT = p.ps("pT", [128, 512]); pG = p.ps("pG", [128, 512]); pP = p.ps("pP", [128, 512]); pY = p.ps("pY", [128, 512])
    pO = p.ps("pO", [128, 512]); pKV = p.ps("pKV", [128, 512])
    xt = S("xt", [128, D]); hT = S("hT", [128, 8, 128]); Z = S("Z", [128, 3392])

    def al(tt_, ap):
        t = T(ap); t.dep = tt_.dep; return t
    scr = dict(sc=S("sc", [128, 8]), lh=al(Z, Z.h[:, 0:1024].rearrange("p (a b) -> p a b", b=128)))
    mbb = pbc(mod_b)
    modulation(p, cT, mod_w, mbb, 0, 2 * D, modl, wk, pA, scr)
    modulation(p, ccT, mod_w, mbb, 0, 2 * D, modc, wk, pA, scr)
    for m in (modl, modc):
        p.ts(m[:, D:2 * D], m[:, D:2 * D], 1.0, ALU.add)

    def bct(name, src_v, n):
        t = S(name, [128, n]); p.dma(t[:], src_v); return t
    cwt = [bct("cw%d" % j, V(cw.h[j].partition_broadcast(128), None), 1536) for j in range(3)]
    cbt = bct("cbt", pbc(cb), 1536); mut = bct("mut", pbc(mu_d), RWW)
    w0t = bct("w0t", pbc(w0_d), 1024); a0t = bct("a0t", pbc(a0_d), 512)
    vecs = [bct("vec%d" % j, V(vec_d.h[j].partition_broadcast(128), None), 512) for j in range(5)]
    kkt, kat, rkt, lgt, lbt = vecs
    w2t = S("w2t", [128, 512]); p.dma(w2t[:], w2_d[:]); a2t = S("a2t", [64, 512]); p.dma(a2t[:], a2_d[:]); g2t = S("g2t", [128, 512]); p.dma(g2t[:], g2_d[:])

    zt = S("zt", [128, RWW]); p.memset(zt[:], 0.0)
    p.dma(PH[0:1, :], zt[0:1, 0:1536]); p.dma(PH[4097:4098, :], zt[0:1, 0:1536])
    p.dma(PRc[0:1, :], zt[0:1, :]); p.dma(PRc[257:258, :], zt[0:1, :])
    p.dma(PRl[0:64, :], zt[0:64, :]); p.dma(PRl[4096 + 64:4096 + 128, :], zt[0:64, :])

    for i in range(NT):
        mod = modc if i < 2 else modl
        p.dma(xt[:], xin[i * 128:(i + 1) * 128, :])
        p.tt(xt[:], xt[:], mod[:, D:2 * D], ALU.mult)
        p.tt(xt[:], xt[:], mod[:, 0:D], ALU.add)
        transpose_tile(p, hT, xt[:], D, ident, pT)
        for n in range(7):
            c0 = n * 512; c1 = min(3392, c0 + 512); w = wk[n % 2]
            p.dma(V(w.h[:, :, 0:c1 - c0], w.dep), V(w_in.h[:, c0:c1].rearrange("(k q) n -> q k n", q=128), None))
            pp = pA[n % 2]
            for k in range(8):
                p.mm(pp[:, 0:c1 - c0], hT[:, k, :], V(w.h[:, k, 0:c1 - c0], w.dep), start=(k == 0), stop=(k == 7))
            p.copy(Z[:, c0:c1], pp[:, 0:c1 - c0], eng="scalar")
        if i < 2:
            p.dma(PRc[1 + i * 128:1 + (i + 1) * 128, :], Z[:, 1536:3392])
        else:
            t0 = (i - 2) * 128
            p.dma(PRl[64 + t0:64 + t0 + 128, :], Z[:, 1536:3392])
            p.dma(PH[1 + t0:1 + t0 + 128, :], Z[:, 0:1536])
    if dbg == 1:
        return p.finish([PRl.dep, PH.dep, PRc.dep])

    Pc = S("Pc", [128, RWW]); Ps = [al(modl, modl.h[:, 0:RWW]), al(modc, modc.h[:, 0:RWW])]; sh = zt
    X1 = S("X1", [128, 128]); X3 = S("X3", [128, 128]); T1 = S("T1", [128, 128]); T2 = S("T2", [64, 128]); T3 = S("T3", [128, 128])
    NM = ["kk", "b", "km", "v", "r", "ldb", "g", "ldf", "a"]
    st_ = {n: S("s_" + n, [128, 512]) for n in NM}
    st8 = S("st8", [128, 16])
    E3 = S("E3", [128, 512]); E2 = S("E2", [128, 512]); ex = S("ex", [128, 512])
    At_ = S("Atl", [128, 512]); Rt_ = S("Rtl", [128, 512]); Kt_ = S("Ktl", [128, 512]); Bt_ = S("Btl", [128, 512])
    Kh = S("Kh", [128, 512]); nBh = S("nBh", [128, 512])
    ARt = S("ARt", [128, 4, 256]); KtT = S("KtT", [128, 4, 128]); BtT = S("BtT", [128, 4, 128])
    dec = S("dec", [128, 8])
    LM1 = S("LM1", [128, 256]); LM2 = S("LM2", [128, 256])
    Atp = [S("Atp%d" % j, [128, 128]) for j in range(6)]; Anp = [S("Anp%d" % j, [128, 128]) for j in range(5)]
    Y = S("Y", [128, 128]); ApT = S("ApT", [128, 128]); Usb = S("Usb", [128, 64])
    Tst = [[S("T%d_%d" % (d, g), [128, 64]) for g in range(4)] for d in range(2)]
    ot = al(xt, xt.h[:, 0:512]); of = S("of", [128, 512]); tmp5 = al(hT, hT.h[:].rearrange("p a b -> p (a b)")[:, 0:512])
    c3 = [al(Z, Z.h[:, 0:1536]), al(wk[0], wk[0].h[:].rearrange("p a b -> p (a b)")[:, 0:1536]), al(wk[1], wk[1].h[:].rearrange("p a b -> p (a b)")[:, 0:1536])]
    p.memset(Usb[:], 0.0)
    for d in range(2):
        for g in range(4):
            p.memset(Tst[d][g][:], 0.0)

    def v3(v, b):
        return V(v.ap.rearrange("p (a b) -> p a b", b=b), v.dep)

    def streams(i):
        if i < 2:
            r0 = 1 + i * 128
            p.dma(Pc[:], PRc[r0:r0 + 128, :])
            p.dma(Ps[0][:], PRc[r0 - 1:r0 + 127, :]); p.dma(Ps[1][:], PRc[r0 + 1:r0 + 129, :])
            for j in range(2):
                p.copy(v3(sh[:], 2)[:, :, j], v3(Ps[j][:], 2)[:, :, j], eng="gpsimd")
        else:
            r0 = 64 + (i - 2) * 128
            p.dma(Pc[:], PRl[r0:r0 + 128, :])
            for j, off in enumerate((-1, 1, -64, 64)):
                q = Ps[j % 2]
                p.dma(q[:], PRl[r0 + off:r0 + off + 128, :])
                if j < 2:
                    p.ts(v3(sh[:], 4)[:, :, j], v3(q[:], 4)[:, :, j], mlr[:, j:j + 1], ALU.mult)
                else:
                    p.copy(v3(sh[:], 4)[:, :, j], v3(q[:], 4)[:, :, j], eng="gpsimd")
        p.tt(sh[:], sh[:], Pc[:], ALU.subtract)
        p.tt(sh[:], sh[:], mut[:], ALU.mult)
        p.tt(Pc[:], Pc[:], sh[:], ALU.add)
        r, k, v = Pc[:, 0:512], Pc[:, 512:1024], Pc[:, 1024:1536]
        p.act(X1[:], Pc[:, 1536:1664], AF.Tanh)
        p.act(X3[:], Pc[:, 1728:1856], AF.Sigmoid)
        p.tr(pT[:, 0:128], X1[:], ident[:]); p.tr(pT[0:64, 128:256], Pc[:, 1664:1728], ident[:]); p.tr(pT[:, 256:384], X3[:], ident[:])
        p.copy(T1[:], pT[:, 0:128]); p.copy(T2[:], pT[0:64, 128:256]); p.copy(T3[:], pT[:, 256:384])
        for d, nm in ((0, "ldf"), (1, "ldb")):
            rows = slice(d * 64, d * 64 + 64)
            p.mm(pA[d][:], T1[rows, :], w2t[rows, :])
            p.tt(st_[nm][:], pA[d][:], w0t[:, d * 512:(d + 1) * 512], ALU.add)
            p.act(st_[nm][:], st_[nm][:], AF.Sigmoid)
            p.ts(st_[nm][:], st_[nm][:], -float(np.exp(-0.5)), ALU.mult)
        p.mm(pG[:], T2[:], a2t[:])
        p.tt(st_["a"][:], pG[:], a0t[:], ALU.add)
        p.act(st_["a"][:], st_["a"][:], AF.Sigmoid)
        p.mm(pP[:], T3[:], g2t[:])
        p.copy(st_["g"][:], pP[:], eng="scalar")
        p.copy(st_["v"][:], v, eng="gpsimd"); p.copy(st_["r"][:], r, eng="gpsimd")
        p.tt(st_["kk"][:], k, kkt[:], ALU.mult)
        p.tt(tmp5[:], st_["kk"][:], st_["kk"][:], ALU.mult)
        p.reduce(st8[:, 0:8], v3(tmp5[:], 64), ALU.add)
        p.act(st8[:, 0:8], st8[:, 0:8], AF.Sqrt)
        p.ts(st8[:, 0:8], st8[:, 0:8], 1e-12, ALU.max)
        p.recip(st8[:, 0:8], st8[:, 0:8])
        p.tt(v3(st_["kk"][:], 64), v3(st_["kk"][:], 64), V(st8.h[:, 0:8].to_broadcast([128, 8, 64]), st8.dep), ALU.mult)
        p.ts(tmp5[:], st_["a"][:], -1.0, ALU.add)
        p.tt(tmp5[:], tmp5[:], kat[:], ALU.mult)
        p.ts(tmp5[:], tmp5[:], 1.0, ALU.add)
        p.tt(st_["km"][:], k, tmp5[:], ALU.mult)
        p.tt(st_["b"][:], st_["kk"][:], st_["a"][:], ALU.mult)

    def recur(i, d, emit):
        ld = st_["ldf"] if d == 0 else st_["ldb"]
        for e, dst in ((0, E3), (1, E2)):
            p.mm(pA[e][:], cm[d][:, e * 128:(e + 1) * 128], ld[:])
            p.copy(dst[:], pA[e][:], eng="scalar")
        for g in range(4):
            p.mm(pKV[:, 384 + g * 2:384 + g * 2 + 2], ld[:, g * 128:(g + 1) * 128], ci[:])
        p.act(dec[:], pKV[:, 384:392], AF.Exp)
        p.act(ex[:], E3[:], AF.Exp); p.tt(Rt_[:], st_["r"][:], ex[:], ALU.mult)
        p.act(ex[:], E3[:], AF.Exp, scale=-1.0); p.tt(Kt_[:], st_["km"][:], ex[:], ALU.mult); p.tt(Bt_[:], st_["b"][:], ex[:], ALU.mult)
        p.tt(ex[:], E3[:], ld[:], ALU.subtract); p.act(ex[:], ex[:], AF.Exp); p.tt(At_[:], st_["kk"][:], ex[:], ALU.mult)
        p.act(ex[:], E2[:], AF.Exp); p.tt(Kh[:], st_["km"][:], ex[:], ALU.mult)
        p.stt(nBh[:], st_["b"][:], -1.0, ex[:], ALU.mult, ALU.mult)
        for src, dstv in ((At_, lambda g: ARt[:, g, 0:128]), (Rt_, lambda g: ARt[:, g, 128:256]), (Kt_, lambda g: KtT[:, g, :]), (Bt_, lambda g: BtT[:, g, :])):
            for g in range(4):
                p.tr(pT[:, g * 128:(g + 1) * 128], src[:, g * 128:(g + 1) * 128], ident[:])
            for g in range(4):
                p.copy(dstv(g), pT[:, g * 128:(g + 1) * 128], eng=("scalar" if g % 2 else "vector"))
        chunks = (0, 1) if d == 0 else (1, 0)
        for hh in range(8):
            g, base = hh // 2, (hh % 2) * 64
            rows = slice(base, base + 64)
            hc = slice(hh * 64, hh * 64 + 64)
            ya = slice(base, base + 64)
            yv = slice(64 - base, 128 - base)
            p.mm(pG[:, 0:256], KtT[rows, g, :], ARt[rows, g, :])
            p.tt(LM1[:], pG[:, 0:256], mk1[d][:], ALU.mult)
            p.mm(pG[:, 256:512], BtT[rows, g, :], ARt[rows, g, :])
            p.tt(LM2[:], pG[:, 256:512], mk2[d][:], ALU.mult)
            p.mm(pP[:, 0:128], ARt[rows, g, 0:128], BtT[rows, g, :])
            p.tt(Anp[0][:], pP[:, 0:128], mk3[d][:], ALU.mult)
            p.copy(Atp[0][:], LM2[:, 0:128], eng="gpsimd")
            p.mm(pY[:, 0:64], LM1[:, 0:128], st_["v"][:, hc])
            p.copy(Y[:, yv], pY[:, 0:64], eng="scalar")
            p.copy(Y[:, ya], At_[:, hc], eng="gpsimd")
            for j in range(1, 6):
                p.mm(pP[:, 128:256], Anp[j - 1][:], Atp[j - 1][:])
                p.copy(Atp[j][:], pP[:, 128:256], eng="scalar")
                if j < 5:
                    p.mm(pP[:, 256:384], Atp[j - 1][:], Anp[j - 1][:])
                    p.copy(Anp[j][:], pP[:, 256:384], eng="vector")
            for j in range(6):
                p.mm(pY[:, 128:256], Atp[j][:], Y[:])
                p.tt(Y[:], Y[:], pY[:, 128:256], ALU.add)
            p.tr(pY[:, 256:384], Y[:], ident[:])
            p.copy(ApT[:], pY[:, 256:384], eng="scalar")
            Tg = Tst[d][g]
            for n, c in enumerate(chunks):
                tk = slice(c * 64, (c + 1) * 64)
                us = slice(256 + n * 64, 256 + (n + 1) * 64)
                p.mm(pKV[tk, us], ApT[rows, tk], Tg[rows, :])
                p.tt(Usb[tk, :], pKV[tk, us], Y[tk, yv], ALU.add)
                if emit:
                    p.mm(pA[0][tk, hc], ARt[rows, g, 128 + c * 64:128 + (c + 1) * 64], Tg[rows, :], start=True, stop=True)
                    p.mm(pO[tk, hc], LM1[tk, 128 + c * 64:128 + (c + 1) * 64], st_["v"][tk, hc], start=True, stop=False)
                    p.mm(pO[tk, hc], LM2[tk, 128 + c * 64:128 + (c + 1) * 64], Usb[tk, :], start=False, stop=True)
                kvs = slice((hh % 2) * 128 + n * 64, (hh % 2) * 128 + (n + 1) * 64)
                p.mm(pKV[rows, kvs], Kh[tk, hc], st_["v"][tk, hc], start=True, stop=False)
                p.mm(pKV[rows, kvs], nBh[tk, hc], Usb[tk, :], start=False, stop=True)
                p.stt(Tg[rows, :], Tg[rows, :], dec[rows, g * 2 + c:g * 2 + c + 1], pKV[rows, kvs], ALU.mult, ALU.add)

    order_f = list(range(NT))
    order_b = [1, 0] + list(range(NT - 1, 1, -1))
    for i in order_f:
        streams(i)
        if dbg == 2 and i == DBGT:
            return p.finish([st_[n].dep for n in NM])
        scd = sc_deps[i]
        for j, nm in enumerate(("kk", "b", "km", "v", "r", "ldb", "g")):
            p.dma(V(SC.h[i, :, j * 512:(j + 1) * 512], scd), st_[nm][:])
        recur(i, 0, i >= 2)
        if dbg == 3 and i == DBGT:
            return p.finish([t.dep for t in Tst[0]] + [pO.dep])
        if i >= 2:
            p.copy(of[:], pO[:], eng="scalar")
            p.tt(of[:], of[:], pA[0][:], ALU.add)
            p.dma(V(OF.h[i], of_deps[i]), of[:])
            t0 = (i - 2) * 128
            for j in range(3):
                p.dma(c3[j][:], PH[t0 + j:t0 + j + 128, :])
            p.tt(c3[1][:], c3[1][:], cwt[1][:], ALU.mult)
            p.tt(c3[0][:], c3[0][:], cwt[0][:], ALU.mult)
            p.tt(c3[2][:], c3[2][:], cwt[2][:], ALU.mult)
            p.tt(c3[1][:], c3[1][:], c3[0][:], ALU.add)
            p.tt(c3[1][:], c3[1][:], c3[2][:], ALU.add)
            p.tt(c3[1][:], c3[1][:], cbt[:], ALU.add)
            p.dma(V(hv.h[t0:t0 + 128, :], out_deps[32 + i - 2]), c3[1][:])
        if (dbg == 4 and i == 2) or (dbg == 5 and i == NT - 1):
            return p.finish(out_deps + of_deps)
    for i in order_b:
        scd = sc_deps[i]
        for j, nm in enumerate(("kk", "b", "km", "v", "r", "ldb", "g")):
            p.dma(st_[nm][:], V(SC.h[i, :, j * 512:(j + 1) * 512], scd))
        recur(i, 1, i >= 2)
        if i < 2:
            continue
        p.dma(of[:], V(OF.h[i], of_deps[i]))
        p.tt(ot[:], pO[:], of[:], ALU.add)
        p.tt(ot[:], ot[:], pA[0][:], ALU.add)
        o3 = v3(ot[:], 64)
        p.reduce(st8[:, 0:8], o3, ALU.add)
        p.ts(st8[:, 0:8], st8[:, 0:8], -1.0 / 64, ALU.mult)
        p.tt(o3, o3, V(st8.h[:, 0:8].to_broadcast([128, 8, 64]), st8.dep), ALU.add)
        p.tt(tmp5[:], ot[:], ot[:], ALU.mult)
        p.reduce(st8[:, 8:16], v3(tmp5[:], 64), ALU.add)
        p.ts(st8[:, 8:16], st8[:, 8:16], 1.0 / 64, ALU.mult, 64e-5, ALU.add)
        p.act(st8[:, 8:16], st8[:, 8:16], AF.Sqrt)
        p.recip(st8[:, 8:16], st8[:, 8:16])
        p.tt(o3, o3, V(st8.h[:, 8:16].to_broadcast([128, 8, 64]), st8.dep), ALU.mult)
        p.tt(ot[:], ot[:], lgt[:], ALU.mult)
        p.tt(ot[:], ot[:], lbt[:], ALU.add)
        p.tt(tmp5[:], st_["r"][:], st_["km"][:], ALU.mult)
        p.tt(tmp5[:], tmp5[:], rkt[:], ALU.mult)
        p.reduce(st8[:, 0:8], v3(tmp5[:], 64), ALU.add)
        p.tt(v3(tmp5[:], 64), v3(st_["v"][:], 64), V(st8.h[:, 0:8].to_broadcast([128, 8, 64]), st8.dep), ALU.mult)
        p.tt(ot[:], ot[:], tmp5[:], ALU.add)
        p.tt(ot[:], ot[:], st_["g"][:], ALU.mult)
        t0 = (i - 2) * 128
        p.dma(V(yrw.h[t0:t0 + 128, :], out_deps[i - 2]), ot[:])
    if standalone:
        return p.finish(out_deps)
    p.end_phase()


def l1rw_inputs(inp, b, xl, xc):
    j = 0
    d = dict(xin=np.concatenate([xc, xl], 0), cT=np.ascontiguousarray(inp["c"][b].reshape(8, 128).T),
             ccT=np.ascontiguousarray(inp["c_ctx"].reshape(8, 128).T), mod_w=inp["mod_w"][1], mod_b=inp["mod_b"][1][None],
             w_in=inp["od_w_in"][j], cw=inp["hy_conv_w"][j], cb=inp["hy_conv_b"][j][None], mu=inp["rw_mu"][j][None],
             w0=inp["rw_w0"][j].reshape(1, 1024), w2=inp["rw_w2"][j].reshape(128, 512), a0=inp["rw_a0"][j][None], a2=inp["rw_a2"][j],
             g2=inp["rw_g2"][j],
             vecs=np.stack([inp["rw_k_k"][j], inp["rw_k_a"][j], inp["rw_r_k"][j].reshape(512), inp["rw_ln_g"][j].reshape(512), inp["rw_ln_b"][j].reshape(512)]))
    d.update(consts_rw())
    return d


NF = 33
_TAB = {}


def dft_tables():
    if not _TAB:
        n = np.arange(NF * 128, dtype=np.int64)
        prod = (n[:, None] * n[None, :]) % 8192
        ang = prod.astype(np.float64) * (2.0 * np.pi / 8192.0)
        valid = ((n[:, None] <= 4096) & (n[None, :] <= 4096))
        for nm, f in (("C", np.cos), ("S", np.sin)):
            t = (f(ang) * valid).astype(np.float32)
            t = t.reshape(NF, 128, NF, 128).transpose(2, 1, 0, 3)
            _TAB[nm] = np.ascontiguousarray(t)
        import ml_dtypes
        for nm in ("C", "S"):
            _TAB[nm] = _TAB[nm].astype(ml_dtypes.bfloat16)
        cf = np.full(NF * 128, 2.0 / 8192.0); cf[0] = 1.0 / 8192.0; cf[4096] = 1.0 / 8192.0; cf[4097:] = 0.0
        _TAB["cfn"] = np.ascontiguousarray(cf.reshape(NF, 128).T.astype(np.float32))
    return _TAB


def hyena_pos_consts():
    L = 4096
    t = np.linspace(0.0, 1.0, L, dtype=np.float32)[:, None]
    wpos = (2.0 * np.pi * np.arange(L, dtype=np.float32)[:, None] / L).astype(np.float32)
    fr = np.linspace(1e-4, 15, 16, dtype=np.float32)[None, :]
    z = np.concatenate([t, np.cos(fr * wpos), -np.sin(fr * wpos)], -1).astype(np.float32)
    deltas = np.linspace(np.log(1e-2) / 1.5, np.log(1e-2) / 0.3, 512, dtype=np.float32)
    tneg = np.ascontiguousarray((-t[:, 0]).reshape(32, 128).T)
    nz = np.ones((128, 32), np.float32); nz[0, 0] = 0.0
    return dict(zT=np.ascontiguousarray(z.T), absd=np.abs(deltas)[None].astype(np.float32), tneg=tneg, nz=nz)


def build_hy_filters():
    p = Prog()
    IN = lambda n, s, dt=F32: p.dram(n, s, dt, kind="ExternalInput")
    zT = IN("zT", [33, 4096]); w1 = IN("w1", [33, 64]); w2 = IN("w2", [64, 64]); w3s = IN("w3s", [64, 256])
    pv = IN("pv", [64, 4])
    absd = IN("absd", [1, 64]); tneg_d = IN("tneg", [128, 32]); nz_d = IN("nz", [128, 32]); cfn_d = IN("cfn", [128, NF])
    Cb = IN("Cb", [NF, 128, NF, 128]); Sb = IN("Sb", [NF, 128, NF, 128])
    FF = p.dram("FF", [2, 2, NF * 128, 64], kind="ExternalOutput")
    ff_deps = [Dep() for _ in range(NF)]
    S = lambda n, s, dt=F32: p.sb(n, s, dt)
    zTt = S("zTt", [33, 4096]); p.dma(zTt[:], zT[:])
    w1t = S("w1t", [33, 64]); p.dma(w1t[:], w1[:]); w2t = S("w2t", [64, 64]); p.dma(w2t[:], w2[:]); w3t = S("w3t", [64, 256]); p.dma(w3t[:], w3s[:])
    pvt = S("pvt", [64, 4]); p.dma(pvt[:], pv[:])
    adt = S("adt", [128, 64]); p.dma(adt[:], pbc(absd)); tneg = S("tneg", [128, 32]); p.dma(tneg[:], tneg_d[:])
    nz = S("nz", [128, 32]); p.dma(nz[:], nz_d[:]); cfn = S("cfn", [128, NF]); p.dma(cfn[:], cfn_d[:])
    H2 = S("H2", [64, 4096]); arg = S("arg", [64, 512]); nf = S("nf", [64, 512]); ni = S("ni", [64, 512], I32); h1s = S("h1s", [64, 512])
    pA = [p.ps("pA0", [128, 512]), p.ps("pA1", [128, 512])]; pB = [p.ps("pB0", [128, 512]), p.ps("pB1", [128, 512])]
    TWO_PI = float(2.0 * np.pi)

    def sin_of(dst, src_ps, bcol, fcol):
        p.ts(arg[:], src_ps, pvt[:, bcol:bcol + 1], ALU.add, pvt[:, fcol:fcol + 1], ALU.mult)
        p.ts(nf[:], arg[:], 1.0 / TWO_PI, ALU.mult)
        p.copy(ni[:], nf[:]); p.copy(nf[:], ni[:])
        p.stt(arg[:], nf[:], -TWO_PI, arg[:], ALU.mult, ALU.add)
        p.ts(arg[:], arg[:], 3.14159, ALU.min, -3.14159, ALU.max)
        p.act(dst, arg[:], AF.Sin)

    for c in range(8):
        cs = slice(c * 512, (c + 1) * 512)
        p.mm(pA[0][0:64, :], w1t[:], zTt[:, cs])
        sin_of(h1s[:], pA[0][0:64, :], 0, 1)
        p.mm(pA[1][0:64, :], w2t[:], h1s[:])
        sin_of(H2[:, cs], pA[1][0:64, :], 2, 3)
    HS = S("HS", [128, 32, 256]); wdw = S("wdw", [128, 64]); F = S("F", [128, 256]); fb = S("fb", [128, 128])
    for i in range(32):
        p.mm(pB[i % 2][:, 0:256], H2[:, i * 128:(i + 1) * 128], w3t[:])
        p.act(wdw[:], adt[:], AF.Exp, scale=tneg[:, i:i + 1])
        p.tt(V(F.h[:].rearrange("p (a c) -> p a c", c=64), F.dep), V(pB[i % 2].h[:, 0:256].rearrange("p (a c) -> p a c", c=64), pB[i % 2].dep),
             V(wdw.h[:, None, :].to_broadcast([128, 4, 64]), wdw.dep), ALU.mult)
        F4 = F.h[:].rearrange("p (o s c) -> p o s c", o=2, s=2)
        fb2 = V(fb.h[:].rearrange("p (o c) -> p o c", o=2), fb.dep)
        p.ts(fb2, V(F4[:, :, 1, :], F.dep), nz[:, i:i + 1], ALU.mult)
        p.tt(V(HS.h[:, i, 0:128].rearrange("p (o c) -> p o c", o=2), HS.dep), V(F4[:, :, 0, :], F.dep), fb2, ALU.add)
        p.tt(V(HS.h[:, i, 128:256].rearrange("p (o c) -> p o c", o=2), HS.dep), V(F4[:, :, 0, :], F.dep), fb2, ALU.subtract)
    tb = [[S("tb%d_%d" % (a, b), [128, 32, 128]) for b in range(2)] for a in range(2)]
    fo = [S("fo0", [128, 256]), S("fo1", [128, 256])]
    for m in range(NF):
        ct, stb = tb[0][m % 2], tb[1][m % 2]
        p.dma(ct[:], Cb[m, :, 0:32, :]); p.dma(stb[:], Sb[m, :, 0:32, :])
        pr, pi_ = pA[m % 2], pB[m % 2]
        for k in range(32):
            p.mm(pr[:, 0:128], ct[:, k, :], HS[:, k, 0:128], start=(k == 0), stop=(k == 31))
        for k in range(32):
            p.mm(pi_[:, 0:128], stb[:, k, :], HS[:, k, 128:256], start=(k == 0), stop=(k == 31))
        o = fo[m % 2]
        p.ts(o[:, 0:128], pr[:, 0:128], cfn[:, m:m + 1], ALU.mult)
        p.ts(o[:, 128:256], pi_[:, 0:128], cfn[:, m:m + 1], ALU.mult, -1.0, ALU.mult)
        for ri in range(2):
            for od in range(2):
                p.dma(V(FF.h[od, ri, m * 128:(m + 1) * 128, :], ff_deps[m]), o[:, ri * 128 + od * 64:ri * 128 + od * 64 + 64])
    return p.finish(ff_deps)


def hyf_inputs(inp, core):
    j = 0
    w3 = inp["hy_ffn_w3"][j].reshape(64, 2, 2, 512)[:, :, :, core * 64:(core + 1) * 64].reshape(64, 256)
    pc = hyena_pos_consts(); tb = dft_tables()
    return dict(zT=pc["zT"], w1=inp["hy_ffn_w1"][j], w2=inp["hy_ffn_w2"][j], w3s=np.ascontiguousarray(w3),
                pv=np.stack([inp["hy_ffn_b1"][j], inp["hy_sin_freq"][j][0], inp["hy_ffn_b2"][j], inp["hy_sin_freq"][j][1]], 1).astype(np.float32),
                absd=np.ascontiguousarray(pc["absd"][:, core * 64:(core + 1) * 64]), tneg=pc["tneg"], nz=pc["nz"], cfn=tb["cfn"],
                Cb=tb["C"], Sb=tb["S"])


def build_hy_conv(p=None, io=None, pfx=""):
    standalone = p is None
    if standalone:
        p = Prog()
    io = io or {}
    p.begin_phase()
    IN = lambda n, s, dt=F32: io[n] if n in io else p.dram(pfx + n, s, dt, kind="ExternalInput")
    hv = IN("hv", [4096, 1536]); FFd = IN("FFd", [2, 2, NF * 128, 512]); yrw = IN("yrw", [4096, 512]); hyb = IN("hyb", [2, 512])
    xin = IN("xin", [4096, D]); cT = IN("cT", [128, 8]); mod_w = IN("mod_w", [D, 6 * D]); mod_b = IN("mod_b", [1, 6 * D])
    w_out = IN("w_out", [D, D]); ln_g = IN("ln_g", [1, D]); ln_b = IN("ln_b", [1, D]); ident_d = IN("ident", [128, 128])
    Cb = IN("Cb", [NF, 128, NF, 128], BF16); Sb = IN("Sb", [NF, 128, NF, 128], BF16)
    xout = io["xout"] if "xout" in io else p.dram("xout", [4096, D], kind="ExternalOutput")
    ZZ = p.dram(pfx + "ZZ", [2, NF * 128, 512]); Z1 = p.dram(pfx + "Z1", [4096, 512]); HY = p.dram(pfx + "HY", [4096, 512])
    zz_deps = [Dep() for _ in range(NF)]; z1_deps = [Dep() for _ in range(32)]; hy_deps = [Dep() for _ in range(32)]
    out_deps = [Dep() for _ in range(32)]
    S = lambda n, s, dt=F32: p.sb(n, s, dt)

    def al(tt_, ap):
        t = T(ap); t.dep = tt_.dep; return t
    ident = S("ident", [128, 128]); p.dma(ident[:], ident_d[:])
    BIG = S("BIG", [128, NF, 1024], BF16)
    tb = [[S("tb%d_%d" % (a, b), [128, NF, 128], BF16) for b in range(2)] for a in range(2)]
    pA = [p.ps("pA0", [128, 512]), p.ps("pA1", [128, 512])]; pB = [p.ps("pB0", [128, 512]), p.ps("pB1", [128, 512])]
    pT = p.ps("pT", [128, 512])
    stg = [S("stg0", [128, 512]), S("stg1", [128, 512])]
    wk = [S("wk%d" % b, [128, 8, 512]) for b in range(2)]
    scr = dict(sc=S("sc", [128, 8]), lh=S("lh", [128, 8, 128]))
    modg = S("modg", [128, D])
    modulation(p, cT, mod_w, pbc(mod_b), 2 * D, D, modg, wk, pA, scr)
    lng = S("lng", [128, D]); p.dma(lng[:], pbc(ln_g)); lnb = S("lnb", [128, D]); p.dma(lnb[:], pbc(ln_b))
    hbt = [S("hbt%d" % n, [128, 512]) for n in range(2)]
    for n in range(2):
        p.dma(hbt[n][:], V(hyb.h[n].partition_broadcast(128), None))
    ffr = [S("ffr0", [128, 512])] * 2; ffi = [S("ffi0", [128, 512])] * 2
    za = [S("za0", [128, 512])] * 2; zb = [S("zb0", [128, 512])] * 2
    t1 = S("t1", [128, 512])
    yt = S("yt", [128, 512]); zc = S("zc", [128, 512]); gt = S("gt", [128, 512])
    for n in range(2):
        for k in range(32):
            sg = stg[k % 2]
            if n == 0:
                p.dma(sg[:], hv[k * 128:(k + 1) * 128, 0:512])
            else:
                p.dma(sg[:], V(Z1.h[k * 128:(k + 1) * 128, :], z1_deps[k]))
            p.copy(BIG[:, k, 0:512], sg[:], eng=("gpsimd" if k % 2 else "vector"))
        for m in range(NF):
            ct, stb = tb[0][m % 2], tb[1][m % 2]
            p.dma(ct[:, 0:32, :], Cb[m, :, 0:32, :]); p.dma(stb[:, 0:32, :], Sb[m, :, 0:32, :])
            fr, fi = ffr[m % 2], ffi[m % 2]
            p.dma(fr[:], FFd[n, 0, m * 128:(m + 1) * 128, :]); p.dma(fi[:], FFd[n, 1, m * 128:(m + 1) * 128, :])
            pr, pi_ = pA[m % 2], pB[m % 2]
            for k in range(32):
                p.mm(pr[:], ct[:, k, :], BIG[:, k, 0:512], start=(k == 0), stop=(k == 31))
            for k in range(32):
                p.mm(pi_[:], stb[:, k, :], BIG[:, k, 0:512], start=(k == 0), stop=(k == 31))
            a_, b_ = za[m % 2], zb[m % 2]
            p.tt(a_[:], pr[:], fr[:], ALU.mult); p.tt(t1[:], pi_[:], fi[:], ALU.mult); p.tt(a_[:], a_[:], t1[:], ALU.add)
            p.tt(b_[:], pi_[:], fr[:], ALU.mult); p.tt(t1[:], pr[:], fi[:], ALU.mult); p.tt(b_[:], b_[:], t1[:], ALU.subtract)
            p.dma(V(ZZ.h[0, m * 128:(m + 1) * 128, :], zz_deps[m]), a_[:])
            p.dma(V(ZZ.h[1, m * 128:(m + 1) * 128, :], zz_deps[m]), b_[:])
        for k in range(NF):
            p.dma(stg[0][:], V(ZZ.h[0, k * 128:(k + 1) * 128, :], zz_deps[k]))
            p.dma(stg[1][:], V(ZZ.h[1, k * 128:(k + 1) * 128, :], zz_deps[k]))
            p.copy(BIG[:, k, 0:512], stg[0][:], eng="vector")
            p.copy(BIG[:, k, 512:1024], stg[1][:], eng="gpsimd")
        for m in range(32):
            ct, stb = tb[0][m % 2], tb[1][m % 2]
            p.dma(ct[:], Cb[m]); p.dma(stb[:], Sb[m])
            py = pA[m % 2]
            for k in range(NF):
                p.mm(py[:], ct[:, k, :], BIG[:, k, 0:512], start=(k == 0), stop=False)
            for k in range(NF):
                p.mm(py[:], stb[:, k, :], BIG[:, k, 512:1024], start=False, stop=(k == NF - 1))
            rs = slice(m * 128, (m + 1) * 128)
            if n == 0:
                p.dma(zc[:], hv[rs, 0:512])
            else:
                p.dma(zc[:], V(Z1.h[rs, :], z1_deps[m]))
            p.dma(gt[:], hv[rs, 512 * (n + 1):512 * (n + 2)])
            p.tt(zc[:], zc[:], hbt[n][:], ALU.mult)
            p.tt(yt[:], py[:], zc[:], ALU.add)
            p.tt(yt[:], yt[:], gt[:], ALU.mult)
            if n == 0:
                p.dma(V(Z1.h[rs, :], z1_deps[m]), yt[:])
            else:
                p.dma(V(HY.h[rs, :], hy_deps[m]), yt[:])
    wo = wk
    xt = S("xt", [128, D]); yc = S("yc", [128, D]); tmp = S("tmp", [128, D]); pre = S("pre", [128, D]); ycT = S("ycT", [128, 8, 128])
    st = S("st", [128, 8])
    for nn in range(2):
        p.dma(wo[nn][:], V(w_out.h[:, nn * 512:(nn + 1) * 512].rearrange("(k q) n -> q k n", q=128), None))
    for i in range(32):
        rs = slice(i * 128, (i + 1) * 128)
        p.dma(yc[:, 0:512], V(HY.h[rs, :], hy_deps[i])); p.dma(yc[:, 512:1024], yrw[rs, :]); p.dma(xt[:], xin[rs, :])
        transpose_tile(p, ycT, yc[:], D, ident, pT)
        for nn in range(2):
            for k in range(8):
                p.mm(pB[nn][:], ycT[:, k, :], wo[nn][:, k, :], start=(k == 0), stop=(k == 7))
            p.tt(tmp[:, nn * 512:(nn + 1) * 512], pB[nn][:], modg[:, nn * 512:(nn + 1) * 512], ALU.mult)
        p.stt(pre[:], xt[:], ALPHA, tmp[:], ALU.mult, ALU.add)
        layer_norm(p, yc[:], pre[:], lng[:], lnb[:], st, tmp[:])
        p.dma(V(xout.h[rs, :], out_deps[i]), yc[:])
    if standalone:
        return p.finish(out_deps)
    p.end_phase()


def hyc_inputs(inp, b, xl, hv, yrw, FF):
    tb = dft_tables(); j = 0
    return dict(hv=hv, FFd=FF, yrw=yrw, hyb=inp["hy_bias"][j], xin=xl, cT=np.ascontiguousarray(inp["c"][b].reshape(8, 128).T),
                mod_w=inp["mod_w"][1], mod_b=inp["mod_b"][1][None], w_out=inp["od_w_out"][j], ln_g=inp["ln1_g"][1][None], ln_b=inp["ln1_b"][1][None],
                ident=np.eye(128, dtype=np.float32), Cb=tb["C"], Sb=tb["S"])


def emit_hy_filters_full(p, io, pfx):
    p.begin_phase()
    IN = lambda n, s, dt=F32: io[n] if n in io else p.dram(pfx + n, s, dt, kind="ExternalInput")
    zT = IN("zT", [33, 4096]); w1 = IN("w1", [33, 64]); w2 = IN("w2", [64, 64]); w3 = IN("w3", [64, 2048])
    pv = IN("pv", [64, 4]); absd = IN("absd", [1, 512]); tneg_d = IN("tneg", [128, 32]); nz_d = IN("nz", [128, 32]); cfn_d = IN("cfn", [128, NF])
    Cb = io["Cb"]; Sb = io["Sb"]; FF = io["FF"]
    ff_deps = [Dep() for _ in range(NF)]
    S = lambda n, s, dt=F32: p.sb(n, s, dt)
    zTt = S("zTt", [33, 4096]); p.dma(zTt[:], zT[:])
    w1t = S("w1t", [33, 64]); p.dma(w1t[:], w1[:]); w2t = S("w2t", [64, 64]); p.dma(w2t[:], w2[:])
    pvt = S("pvt", [64, 4]); p.dma(pvt[:], pv[:])
    adt = S("adt", [128, 512]); p.dma(adt[:], pbc(absd)); tneg = S("tneg", [128, 32]); p.dma(tneg[:], tneg_d[:])
    nz = S("nz", [128, 32]); p.dma(nz[:], nz_d[:]); cfn = S("cfn", [128, NF]); p.dma(cfn[:], cfn_d[:])
    H2 = S("H2", [64, 4096]); arg = S("arg", [64, 512]); nf = S("nf", [64, 512]); ni = S("ni", [64, 512], I32); h1s = S("h1s", [64, 512])
    pA = [p.ps("pA0", [128, 512]), p.ps("pA1", [128, 512])]; pB = [p.ps("pB0", [128, 512]), p.ps("pB1", [128, 512])]
    TWO_PI = float(2.0 * np.pi)

    def sin_of(dst, src_ps, bcol, fcol):
        p.ts(arg[:], src_ps, pvt[:, bcol:bcol + 1], ALU.add, pvt[:, fcol:fcol + 1], ALU.mult)
        p.ts(nf[:], arg[:], 1.0 / TWO_PI, ALU.mult)
        p.copy(ni[:], nf[:]); p.copy(nf[:], ni[:])
        p.stt(arg[:], nf[:], -TWO_PI, arg[:], ALU.mult, ALU.add)
        p.ts(arg[:], arg[:], 3.14159, ALU.min, -3.14159, ALU.max)
        p.act(dst, arg[:], AF.Sin)

    for c in range(8):
        cs = slice(c * 512, (c + 1) * 512)
        p.mm(pA[0][0:64, :], w1t[:], zTt[:, cs])
        sin_of(h1s[:], pA[0][0:64, :], 0, 1)
        p.mm(pA[1][0:64, :], w2t[:], h1s[:])
        sin_of(H2[:, cs], pA[1][0:64, :], 2, 3)
    HS = S("HS", [128, 32, 1024], BF16); wdw = S("wdw", [128, 512]); F = S("F", [128, 1024]); fb = S("fb", [128, 512]); w3p = S("w3p", [64, 1024])
    tb = [[S("tb%d_%d" % (a, b), [128, 32, 128], BF16) for b in range(2)] for a in range(2)]
    fo = [S("fo0", [128, 1024]), S("fo1", [128, 1024])]
    pC = [p.ps("pC0", [128, 512]), p.ps("pC1", [128, 512])]
    for od in range(2):
        p.dma(w3p[:], w3[:, od * 1024:(od + 1) * 1024])
        for i in range(32):
            for sd in range(2):
                p.mm(pC[sd][:], H2[:, i * 128:(i + 1) * 128], w3p[:, sd * 512:(sd + 1) * 512])
            p.act(wdw[:], adt[:], AF.Exp, scale=tneg[:, i:i + 1])
            p.tt(F[:, 0:512], pC[0][:], wdw[:], ALU.mult)
            p.tt(F[:, 512:1024], pC[1][:], wdw[:], ALU.mult)
            p.ts(fb[:], F[:, 512:1024], nz[:, i:i + 1], ALU.mult)
            p.tt(HS[:, i, 0:512], F[:, 0:512], fb[:], ALU.add)
            p.tt(HS[:, i, 512:1024], F[:, 0:512], fb[:], ALU.subtract)
        for m in range(NF):
            ct, stb = tb[0][m % 2], tb[1][m % 2]
            p.dma(ct[:], Cb[m, :, 0:32, :]); p.dma(stb[:], Sb[m, :, 0:32, :])
            pr, pi_ = pA[m % 2], pB[m % 2]
            for k in range(32):
                p.mm(pr[:], ct[:, k, :], HS[:, k, 0:512], start=(k == 0), stop=(k == 31))
            for k in range(32):
                p.mm(pi_[:], stb[:, k, :], HS[:, k, 512:1024], start=(k == 0), stop=(k == 31))
            o = fo[m % 2]
            p.ts(o[:, 0:512], pr[:], cfn[:, m:m + 1], ALU.mult)
            p.ts(o[:, 512:1024], pi_[:], cfn[:, m:m + 1], ALU.mult, -1.0, ALU.mult)
            for ri in range(2):
                p.dma(V(FF.h[od, ri, m * 128:(m + 1) * 128, :], ff_deps[m]), o[:, ri * 512:(ri + 1) * 512])
    p.end_phase()


def build_fused():
    p = Prog()
    p.use_arena()
    X0 = p.dram("xin0", [NT * 128, D], kind="ExternalInput")
    Cb = p.dram("Cb", [NF, 128, NF, 128], BF16, kind="ExternalInput"); Sb = p.dram("Sb", [NF, 128, NF, 128], BF16, kind="ExternalInput")
    OUT = p.dram("out", [NT * 128, D], kind="ExternalOutput")
    un = lambda n, s: p.dram(n, s)
    XA = un("XA", [NT * 128, D]); XB = un("XB", [NT * 128, D]); XD = un("XD", [NT * 128, D])
    U = un("U", [NT, 128, D]); RG = un("RG", [NT, 128, 512])
    HR = (NB // 2) * 128
    XS = [un("XSa", [HR, D]), un("XSb", [HR, D])]; YS = [un("YSa", [HR, D]), un("YSb", [HR, D])]
    FF = un("FF", [2, 2, NF * 128, 512]); HV = un("HV", [4096, 1536]); YRW = un("YRW", [4096, 512])
    for t in (XA, XB, XD, FF, HV, YRW):
        t.dep = None
    sub = lambda t, r0, r1: T(t.h[r0:r1, :], tracked=False)
    moe_io = dict(U=U, RG=RG, XS=XS, YS=YS)
    build_l0_mixer(p, dict(xin=X0, xout=XA), "a_")
    build_moe(99, p, dict(xin=XA, xout=XB, **moe_io), "b_")
    emit_hy_filters_full(p, dict(Cb=Cb, Sb=Sb, FF=FF), "f_")
    build_l1_rw(99, p, dict(xin=XB, yrw=YRW, hv=HV), "r_")
    build_hy_conv(p, dict(hv=HV, FFd=FF, yrw=YRW, xin=sub(XB, 256, NT * 128), xout=sub(XD, 256, NT * 128), Cb=Cb, Sb=Sb), "h_")
    p.begin_phase()
    p.dma(V(XD.h[0:256, :], None), V(XB.h[0:256, :], None))
    p.end_phase()
    for t in (U, RG, XS[0], XS[1], YS[0], YS[1]):
        t.dep = Dep()
    p.begin_phase()
    build_moe(99, p, dict(xin=XD, xout=OUT, **moe_io), "e_")
    return p.finish([OUT.dep])


def _pref(d, pfx, drop=()):
    return {pfx + k: v for k, v in d.items() if k not in drop}


def fused_inputs(inp, b):
    j = 0
    tb = dft_tables(); pc = hyena_pos_consts()
    xl, xc = inp["x"][b], inp["ctx"][b]
    z = np.zeros((1, 1), np.float32)
    d = {"xin0": np.concatenate([xc, xl], 0), "Cb": tb["C"], "Sb": tb["S"]}
    d.update(_pref(l0_inputs(inp, b, xl, xc), "a_", ("xin",)))
    d.update(_pref(moe_inputs(inp, 0, b, z, z), "b_", ("xin",)))
    d.update(_pref(dict(zT=pc["zT"], w1=inp["hy_ffn_w1"][j], w2=inp["hy_ffn_w2"][j], w3=inp["hy_ffn_w3"][j],
                        pv=np.stack([inp["hy_ffn_b1"][j], inp["hy_sin_freq"][j][0], inp["hy_ffn_b2"][j], inp["hy_sin_freq"][j][1]], 1).astype(np.float32),
                        absd=pc["absd"], tneg=pc["tneg"], nz=pc["nz"], cfn=tb["cfn"]), "f_"))
    d.update(_pref(l1rw_inputs(inp, b, z, z), "r_", ("xin",)))
    d.update(_pref(hyc_inputs(inp, b, z, z, z, z), "h_", ("xin", "hv", "yrw", "FFd", "Cb", "Sb")))
    d.update(_pref(moe_inputs(inp, 1, b, z, z), "e_", ("xin",)))
    return d


def l0_inputs(inp, b, xl, xc):
    gw2 = np.zeros((32, 512), np.float32)
    gw2[0:16, 0:256] = inp["gla_gate_w2"][0, 0]
    gw2[16:32, 256:512] = inp["gla_gate_w2"][0, 1]
    d = dict(xin=np.concatenate([xc, xl], 0), cT=np.ascontiguousarray(inp["c"][b].reshape(8, 128).T),
             ccT=np.ascontiguousarray(inp["c_ctx"].reshape(8, 128).T),
             mod_w=inp["mod_w"][0], mod_b=inp["mod_b"][0][None], ln_g=inp["ln1_g"][0][None], ln_b=inp["ln1_b"][0][None],
             w_in=inp["ev_w_in"][0], w_out=inp["ev_w_out"][0], gw2=gw2, gb=inp["gla_gate_b"][0].reshape(1, 512),
             gvec=np.concatenate([np.tile(inp["gla_norm_g"][0], 4), np.tile(inp["hg_norm_g"][0], 4)])[None],
             lbl=inp["hg_lb_logits"].reshape(1, 2048))
    d.update(consts_l0())
    return d


def _run(nc, maps):
    res = run_bass_kernel_spmd(nc, maps, core_ids=list(range(len(maps))))
    return res.results


def kernel_unfused(**inputs):
    inp = {k: np.ascontiguousarray(np.asarray(v, dtype=np.float32)) for k, v in inputs.items()}
    B = 8
    xl = [inp["x"][b] for b in range(B)]
    xc = [inp["ctx"][b] for b in range(B)]
    r = _run(build_l0_mixer(), [l0_inputs(inp, b, xl[b], xc[b]) for b in range(B)])
    xc = [r[b]["xout"][:256] for b in range(B)]
    xl = [r[b]["xout"][256:] for b in range(B)]
    moe_nc = build_moe()
    r = _run(moe_nc, [moe_inputs(inp, 0, b, xl[b], xc[b]) for b in range(B)])
    xc = [r[b]["xout"][:256] for b in range(B)]
    xl = [r[b]["xout"][256:] for b in range(B)]
    r = _run(build_hy_filters(), [hyf_inputs(inp, c) for c in range(B)])
    FF = np.ascontiguousarray(np.concatenate([r[c]["FF"] for c in range(B)], -1))
    r = _run(build_l1_rw(), [l1rw_inputs(inp, b, xl[b], xc[b]) for b in range(B)])
    yrw = [r[b]["yrw"] for b in range(B)]
    hv = [r[b]["hv"] for b in range(B)]
    r = _run(build_hy_conv(), [hyc_inputs(inp, b, xl[b], hv[b], yrw[b], FF) for b in range(B)])
    xl = [r[b]["xout"] for b in range(B)]
    r = _run(build_moe(), [moe_inputs(inp, 1, b, xl[b], xc[b]) for b in range(B)])
    out = np.stack([r[b]["xout"][256:] for b in range(B)], 0)
    return out.astype(np.float32)


def kernel(**inputs):
    inp = {k: np.ascontiguousarray(np.asarray(v, dtype=np.float32)) for k, v in inputs.items()}
    B = 8
    nc = build_fused()
    r = _run(nc, [fused_inputs(inp, b) for b in range(B)])
    out = np.stack([r[b]["out"][256:] for b in range(B)], 0)
    return out.astype(np.float32)
```

```python
import numpy as np
from contextlib import ExitStack
import concourse.bass as bass
import concourse.mybir as mybir
from concourse.bass_utils import run_bass_kernel_spmd

F32 = mybir.dt.float32
I32 = mybir.dt.int32
AF = mybir.ActivationFunctionType
ALU = mybir.AluOpType
AX = mybir.AxisListType

BF16 = mybir.dt.bfloat16
ENGS = ["tensor", "vector", "scalar", "gpsimd", "sync"]


def f32r(v):
    return v
NDS = 40


class Dep:
    __slots__ = ("w", "r")

    def __init__(self):
        self.w = None
        self.r = {}


class V:
    __slots__ = ("ap", "dep")

    def __init__(self, ap, dep):
        self.ap = ap
        self.dep = dep

    def __getitem__(self, idx):
        return V(self.ap[idx], self.dep)


class T:
    def __init__(self, h, tracked=True):
        self.h = h
        self.dep = Dep() if tracked else None

    def __getitem__(self, idx):
        return V(self.h[idx], self.dep)

    def v(self, ap):
        return V(ap, self.dep)


class Prog:
    def __init__(self):
        self.nc = bass.Bass("TRN2", target_bir_lowering=False)
        self.es = ExitStack()
        self.streams = {e: [] for e in ENGS}
        self.cnt = {e: 0 for e in ENGS}
        self.sem = {e: self.es.enter_context(self.nc.semaphore("s_" + e)) for e in ENGS}
        self.known = {e: {} for e in ENGS}
        self.dsem = [self.es.enter_context(self.nc.semaphore("d%d" % i)) for i in range(NDS)]
        self.dcnt = [0] * NDS
        self.dnext = 0
        self.nalloc = 0
        self.out_events = []
        self.arena = None
        self.banks = None
        self.aoff = 0
        self.nps = 0

    ARENA = 53200

    def use_arena(self):
        self.arena = self.es.enter_context(self.nc.sbuf_tensor("arena", [128, self.ARENA], F32))
        self.banks = [self.es.enter_context(self.nc.psum_tensor("bank%d" % i, [128, 512], F32)) for i in range(8)]

    def begin_phase(self):
        self.aoff = 0
        self.nps = 0

    def barrier(self):
        evs = [(e, self.cnt[e]) for e in ENGS if self.cnt[e] > 0]
        evs += [(sl, 16 * self.dcnt[sl]) for sl in range(NDS) if self.dcnt[sl] > 0]
        for e in ENGS:
            waits = []
            for k, v in evs:
                if self.known[e].get(k, 0) < v:
                    self.known[e][k] = v
                    waits.append((self._semof(k), v))
            self.streams[e].append((waits, None, None, 0))

    def end_phase(self):
        self.barrier()

    def _nm(self, name):
        self.nalloc += 1
        return "%s_%d" % (name, self.nalloc)

    def sb(self, name, shape, dt=F32):
        if self.arena is None:
            return T(self.es.enter_context(self.nc.sbuf_tensor(self._nm(name), list(shape), dt)))
        n = 1
        for d_ in shape[1:]:
            n *= d_
        words = (n + 1) // 2 if dt == BF16 else n
        n8 = (words + 7) // 8 * 8
        assert self.aoff + n8 <= self.ARENA, "arena overflow at %s: %d + %d" % (name, self.aoff, n8)
        ap = self.arena[0:shape[0], self.aoff:self.aoff + words]
        self.aoff += n8
        if dt != F32:
            ap = ap.bitcast(dt)
            if dt == BF16:
                ap = ap[:, 0:n]
        if len(shape) == 3:
            ap = ap.rearrange("p (a b) -> p a b", b=shape[2])
        return T(ap)

    def ps(self, name, shape, dt=F32):
        if self.banks is None:
            return T(self.es.enter_context(self.nc.psum_tensor(self._nm(name), list(shape), dt)))
        b = self.banks[self.nps]
        self.nps += 1
        return T(b[:, :])

    def dram(self, name, shape, dt=F32, kind="Internal"):
        t = self.nc.dram_tensor(name, list(shape), dt, kind=kind)
        return T(t.ap(), tracked=(kind != "ExternalInput"))

    def _semof(self, key):
        return self.sem[key] if isinstance(key, str) else self.dsem[key]

    def _waits(self, eng, reads, writes):
        need = {}
        for d in reads:
            if d is not None and d.w is not None:
                k, v = d.w
                need[k] = max(need.get(k, 0), v)
        for d in writes:
            if d is None:
                continue
            if d.w is not None:
                k, v = d.w
                need[k] = max(need.get(k, 0), v)
            for k, v in d.r.items():
                need[k] = max(need.get(k, 0), v)
        out = []
        kn = self.known[eng]
        for k, v in need.items():
            if k == eng and eng == "tensor":
                continue
            if kn.get(k, 0) >= v:
                continue
            kn[k] = v
            out.append((self._semof(k), v))
        return out

    def _record(self, ev, reads, writes):
        k, v = ev
        for d in reads:
            if d is not None:
                d.r[k] = max(d.r.get(k, 0), v)
        for d in writes:
            if d is not None:
                d.w = ev
                d.r = {}

    def op(self, eng, fn, outs, ins):
        reads = [x.dep for x in ins if isinstance(x, V)]
        writes = [x.dep for x in outs]
        waits = self._waits(eng, reads, writes)
        self.cnt[eng] += 1
        ev = (eng, self.cnt[eng])
        self.streams[eng].append((waits, fn, self.sem[eng], 1))
        self._record(ev, reads, writes)
        return ev

    store_eng = "sync"

    def dma(self, out, in_, eng=None, fn=None, extra_reads=()):
        if eng is None:
            eng = "sync"
            if fn is None and "DRam" in type(out.ap.tensor).__name__ and "DRam" not in type(in_.ap.tensor).__name__:
                eng = self.store_eng
        slot = self.dnext
        self.dnext = (self.dnext + 1) % NDS
        reads = [in_.dep] + [x.dep for x in extra_reads]
        writes = [out.dep]
        waits = self._waits(eng, reads, writes)
        if self.dcnt[slot] > 0:
            pv = 16 * self.dcnt[slot]
            if self.known[eng].get(slot, 0) < pv:
                self.known[eng][slot] = pv
                waits.append((self.dsem[slot], pv))
        self.dcnt[slot] += 1
        ev = (slot, 16 * self.dcnt[slot])
        if fn is None:
            o, i = out.ap, in_.ap
            fn = lambda e, o=o, i=i: e.dma_start(out=o, in_=i)
        self.streams[eng].append((waits, fn, self.dsem[slot], 16))
        self._record(ev, reads, writes)
        return ev

    def finish(self, final_deps):
        waits = self._waits("sync", [d for d in final_deps], [])
        self.streams["sync"].append((waits, None, None, 0))
        with self.nc.Block() as block:
            for e in ENGS:
                stream = self.streams[e]

                def body(engh, stream=stream):
                    for waits, fn, sem, inc in stream:
                        for (s, v) in waits:
                            engh.wait_ge(s, v)
                        if fn is not None:
                            ins = fn(engh)
                            ins.then_inc(sem, inc)

                getattr(block, e)(body)
        self.es.close()
        return self.nc

    def mm(self, out, lhsT, rhs, start=True, stop=True):
        o, a, b = out.ap, lhsT.ap, rhs.ap
        return self.op("tensor", lambda e: e.matmul(o, a, b, start=start, stop=stop), [out], [lhsT, rhs])

    def tr(self, out, in_, ident):
        o, a, b = out.ap, in_.ap, ident.ap
        return self.op("tensor", lambda e: e.transpose(o, a, b), [out], [in_, ident])

    def act(self, out, in_, func, bias=0.0, scale=1.0, eng="scalar", accum_out=None):
        o, a = out.ap, in_.ap
        bb = bias.ap if isinstance(bias, V) else bias
        ss = scale.ap if isinstance(scale, V) else scale
        outs = [out]
        kw = {}
        if accum_out is not None:
            kw["accum_out"] = accum_out.ap
            outs.append(accum_out)
        return self.op("scalar", lambda e: e.activation(o, a, func, bias=bb, scale=ss, **kw), outs, [in_, bias, scale])

    def tt(self, out, a, b, op, eng="vector"):
        o, x, y = out.ap, a.ap, b.ap
        return self.op(eng, lambda e: e.tensor_tensor(o, x, y, op), [out], [a, b])

    def ts(self, out, a, s1, op0, s2=None, op1=None, eng="vector", accum_out=None):
        o, x = out.ap, a.ap
        c1 = s1.ap if isinstance(s1, V) else s1
        c2 = s2.ap if isinstance(s2, V) else s2
        outs = [out]
        kw = {}
        if op1 is not None:
            kw["op1"] = op1
        if accum_out is not None:
            kw["accum_out"] = accum_out.ap
            outs.append(accum_out)
        return self.op(eng, lambda e: e.tensor_scalar(o, x, c1, c2, op0, **kw), outs, [a, s1, s2])

    def stt(self, out, a, s, b, op0, op1, eng="vector"):
        o, x, y = out.ap, a.ap, b.ap
        c = s.ap if isinstance(s, V) else s
        return self.op(eng, lambda e: e.scalar_tensor_tensor(o, x, c, y, op0, op1), [out], [a, s, b])

    def copy(self, out, in_, eng="vector"):
        o, a = out.ap, in_.ap
        if eng == "scalar":
            return self.op(eng, lambda e: e.copy(o, a), [out], [in_])
        return self.op(eng, lambda e: e.tensor_copy(o, a), [out], [in_])

    def memset(self, out, val, eng="vector"):
        o = out.ap
        return self.op(eng, lambda e: e.memset(o, val), [out], [])

    def reduce(self, out, in_, op, axis=None, eng="vector"):
        o, a = out.ap, in_.ap
        ax = AX.X if axis is None else axis
        return self.op(eng, lambda e: e.tensor_reduce(o, a, ax, op), [out], [in_])

    def recip(self, out, in_):
        o, a = out.ap, in_.ap
        return self.op("vector", lambda e: e.reciprocal(o, a), [out], [in_])


STORE_ENG = dict(l0="gpsimd", moe="scalar", rw="gpsimd", hyc="gpsimd", hyf="gpsimd")
D = 1024
ALPHA = 4.0 ** 0.25
NT = 34


def pbc(t, n=128):
    return V(t.h[0].partition_broadcast(n), t.dep)


def modulation(p, cT, mod_w, mod_b_bc, c0, ncols, out, wk, ps, scr):
    sc = scr["sc"]
    lh = scr["lh"]
    p.dma(sc[:], cT[:])
    p.act(sc[:], sc[:], AF.Silu)
    for k in range(8):
        p.copy(lh[:, k, :], V(sc.h[:, k:k + 1].to_broadcast([128, 128]), sc.dep))
    p.dma(out[:, 0:ncols], V(mod_b_bc.ap[:, c0:c0 + ncols], None))
    for n in range(ncols // 512):
        w = wk[n % 2]
        p.dma(w[:], V(mod_w.h[:, c0 + n * 512:c0 + (n + 1) * 512].rearrange("(k q) n -> q k n", q=128), None))
        pp = ps[n % 2]
        for k in range(8):
            p.mm(pp[:], lh[:, k, :], w[:, k, :], start=(k == 0), stop=(k == 7))
        p.tt(out[:, n * 512:(n + 1) * 512], pp[:], out[:, n * 512:(n + 1) * 512], ALU.add)


def layer_norm(p, out, in_, g_bc, b_bc, st, tmp, eps=1e-5, n=1024):
    p.reduce(st[:, 0:1], in_, ALU.add)
    p.ts(st[:, 1:2], st[:, 0:1], -1.0 / n, ALU.mult)
    p.act(tmp, in_, AF.Square, bias=st[:, 1:2], accum_out=st[:, 2:3])
    p.ts(st[:, 3:4], st[:, 2:3], 1.0 / n, ALU.mult, eps, ALU.add)
    p.act(st[:, 3:4], st[:, 3:4], AF.Sqrt)
    p.recip(st[:, 3:4], st[:, 3:4])
    p.ts(tmp, in_, st[:, 1:2], ALU.add, st[:, 3:4], ALU.mult)
    p.tt(tmp, tmp, g_bc, ALU.mult)
    p.tt(out, tmp, b_bc, ALU.add)


def transpose_tile(p, dstT, src, ncol, ident, pst, eng="vector", rnd=False):
    nb = ncol // 128
    for g0 in range(0, nb, 4):
        g1 = min(nb, g0 + 4)
        for g in range(g0, g1):
            p.tr(pst[:, (g - g0) * 128:(g - g0 + 1) * 128], V(src.ap[:, g * 128:(g + 1) * 128], src.dep), ident[:])
        dv = V(dstT.h[:, g0:g1, :].rearrange("p a b -> p (a b)"), dstT.dep)
        p.copy(f32r(dv) if rnd else dv, pst[:, 0:(g1 - g0) * 128], eng=eng)


def consts_l0():
    i = np.arange(128)
    ch = i // 64
    same = (ch[:, None] == ch[None, :])
    triF = (same & (i[:, None] <= i[None, :])).astype(np.float32)
    triB = (same & (i[:, None] >= i[None, :])).astype(np.float32)
    refF = triF[:, ch * 64 + 32]
    lastF = same.astype(np.float32)
    refB = triB[:, ch * 64 + 31]
    cmF = np.concatenate([triF - refF, lastF - triF, triF], 1)
    cmB = np.concatenate([triB - refB, lastF - triB, triB], 1)
    ci = np.stack([(ch == 0), (ch == 1)], 1).astype(np.float32)
    return dict(ident=np.eye(128, dtype=np.float32), cmF=cmF.astype(np.float32), cmB=cmB.astype(np.float32), ci=ci)


def build_l0_mixer(p=None, io=None, pfx=""):
    standalone = p is None
    if standalone:
        p = Prog()
    io = io or {}
    p.begin_phase()
    p.store_eng = STORE_ENG.get("l0", "sync")
    IN = lambda n, s, dt=F32: io[n] if n in io else p.dram(pfx + n, s, dt, kind="ExternalInput")
    xin = IN("xin", [NT * 128, D])
    cT = IN("cT", [128, 8]); ccT = IN("ccT", [128, 8])
    mod_w = IN("mod_w", [D, 6 * D]); mod_b = IN("mod_b", [1, 6 * D])
    ln_g = IN("ln_g", [1, D]); ln_b = IN("ln_b", [1, D])
    w_in = IN("w_in", [D, 4128]); w_out = IN("w_out", [D, D])
    gw2 = IN("gw2", [32, 512])
    gb = IN("gb", [1, 512])
    gvec = IN("gvec", [1, D])
    lbl = IN("lbl", [1, 2048])
    ident_d = IN("ident", [128, 128]); cmF_d = IN("cmF", [128, 384]); cmB_d = IN("cmB", [128, 384]); ci_d = IN("ci", [128, 2])
    xout = io["xout"] if "xout" in io else p.dram("xout", [NT * 128, D], kind="ExternalOutput")
    SC = p.dram(pfx + "SC", [NT, 128, 4352])
    OF = p.dram(pfx + "OF", [NT, 128, D])
    sc_deps = [Dep() for _ in range(NT)]
    of_deps = [Dep() for _ in range(NT)]
    out_deps = [Dep() for _ in range(NT)]

    S = lambda n, s: p.sb(n, s)
    ident = S("ident", [128, 128]); cm = [S("cmF", [128, 384]), S("cmB", [128, 384])]; ci = S("ci", [128, 2])
    p.dma(ident[:], ident_d[:]); p.dma(cm[0][:], cmF_d[:]); p.dma(cm[1][:], cmB_d[:]); p.dma(ci[:], ci_d[:])
    modl = S("modl", [128, 3 * D]); modc = S("modc", [128, 3 * D])
    wk = [S("wk0", [128, 8, 512]), S("wk1", [128, 8, 512])]
    pA = [p.ps("pA0", [128, 512]), p.ps("pA1", [128, 512])]
    pT = p.ps("pT", [128, 512]); pS = p.ps("pS", [128, 512]); pO = [p.ps("pO0", [128, 512]), p.ps("pO1", [128, 512])]
    pKV = p.ps("pKV", [128, 512]); pD = p.ps("pD", [128, 512])
    scr = dict(sc=S("sc", [128, 8]), lh=S("lh", [128, 8, 128]))
    mbb = pbc(mod_b)
    modulation(p, cT, mod_w, mbb, 0, 3 * D, modl, wk, pA, scr)
    modulation(p, ccT, mod_w, mbb, 0, 3 * D, modc, wk, pA, scr)
    for m in (modl, modc):
        p.ts(m[:, D:2 * D], m[:, D:2 * D], 1.0, ALU.add)
    gbt = S("gbt", [128, 512]); p.dma(gbt[:], pbc(gb))
    gw2t = S("gw2t", [32, 512]); p.dma(gw2t[:], gw2[:])
    gv = S("gv", [128, D]); p.dma(gv[:], pbc(gvec))
    lng = S("lng", [128, D]); p.dma(lng[:], pbc(ln_g)); lnb = S("lnb", [128, D]); p.dma(lnb[:], pbc(ln_b))
    Z = S("Z", [128, 4128])
    p.dma(Z[:, 0:2048], pbc(lbl))
    oml = S("oml", [128, 1024])
    p.tt(oml[:], Z[:, 1024:2048], Z[:, 0:1024], ALU.subtract)
    p.act(oml[:], oml[:], AF.Sigmoid)

    xt = S("xt", [128, D]); hT = S("hT", [128, 8, 128])
    aT = S("aT", [32, 128])
    Q = S("Q", [128, 768]); Kd = [S("K0", [128, 768]), S("K1", [128, 768])]; LAd = [S("LA0", [128, 768]), S("LA1", [128, 768])]
    Vv = S("Vv", [128, D]); G = S("G", [128, D])
    E = [S("E%d" % i, [128, 768]) for i in range(3)]
    qp = S("qp", [128, 768]); kp = S("kp", [128, 768]); qi = S("qi", [128, 768]); ko = S("ko", [128, 768])
    qpT = S("qpT", [128, 6, 128]); kpT = S("kpT", [128, 6, 128]); qiT = S("qiT", [128, 6, 128])
    dec = S("dec", [128, 12]); PT2 = [S("PT", [128, 128]), S("PT1", [128, 128])]; ot = S("ot", [128, D]); of = S("of", [128, D])
    Sst = [[S("S%d_%d" % (d, g), [128, 128]) for g in range(6)] for d in range(2)]
    st = S("st", [128, 16]); tmp = S("tmp", [128, D]); ht = tmp; yn = S("yn", [128, D]); ynT = S("ynT", [128, 8, 128])
    for d in range(2):
        for g in range(6):
            p.memset(Sst[d][g][:], 0.0)

    def gates(i):
        p.tr(pT[0:32, 0:128], Z[:, 1536:1568], ident[:])
        p.copy(aT[:], pT[0:32, 0:128])
        p.mm(pA[0][:], aT[:], gw2t[:])
        p.tt(E[0][:, 0:512], pA[0][:], gbt[:], ALU.add)
        p.act(E[0][:, 0:512], E[0][:, 0:512], AF.Exp, scale=-1.0)
        p.act(E[0][:, 0:512], E[0][:, 0:512], AF.Ln, bias=1.0)
        for d in range(2):
            p.ts(LAd[d][:, 0:256], E[0][:, d * 256:(d + 1) * 256], -1.0 / 16.0, ALU.mult)
            p.copy(Kd[d][:, 0:256], Z[:, 256:512], eng="gpsimd")
            p.act(Kd[d][:, 256:768], Z[:, 2080 + 512 * d:2592 + 512 * d], AF.Sigmoid, scale=-1.0)
            p.tt(Kd[d][:, 256:768], Kd[d][:, 256:768], oml[:, 512 * d:512 * (d + 1)], ALU.mult)
            p.act(LAd[d][:, 256:768], Kd[d][:, 256:768], AF.Ln, scale=-1.0, bias=1.0)
        p.ts(Q[:, 0:256], Z[:, 0:256], 0.125, ALU.mult)
        p.act(Q[:, 256:768], Z[:, 1568:2080], AF.Silu)
        p.copy(Vv[:, 0:512], Z[:, 512:1024], eng="gpsimd")
        p.copy(Vv[:, 512:1024], Z[:, 3104:3616], eng="gpsimd")
        p.act(G[:, 0:512], Z[:, 1024:1536], AF.Silu)
        p.act(G[:, 512:1024], Z[:, 3616:4128], AF.Silu)

    def recur(i, d):
        la, kk = LAd[d], Kd[d]
        for e in range(3):
            for (c0, c1) in ((0, 512), (512, 768)):
                pp = pA[(e + (c0 > 0)) % 2]
                p.mm(pp[:, 0:c1 - c0], cm[d][:, e * 128:(e + 1) * 128], la[:, c0:c1])
                p.copy(E[e][:, c0:c1], pp[:, 0:c1 - c0], eng="scalar")
        for g in range(6):
            p.mm(pD[:, g * 2:g * 2 + 2], la[:, g * 128:(g + 1) * 128], ci[:], start=True, stop=True)
        p.act(dec[:], pD[:, 0:12], AF.Exp)
        p.act(qp[:], E[0][:], AF.Exp); p.tt(qp[:], qp[:], Q[:], ALU.mult)
        p.act(kp[:], E[0][:], AF.Exp, scale=-1.0); p.tt(kp[:], kp[:], kk[:], ALU.mult)
        p.act(ko[:], E[1][:], AF.Exp); p.tt(ko[:], ko[:], kk[:], ALU.mult)
        p.act(qi[:], E[2][:], AF.Exp); p.tt(qi[:], qi[:], Q[:], ALU.mult)
        transpose_tile(p, qpT, qp[:], 768, ident, pT)
        transpose_tile(p, kpT, kp[:], 768, ident, pT)
        transpose_tile(p, qiT, qi[:], 768, ident, pT)
        chunks = (0, 1) if d == 0 else (1, 0)
        for hh in range(8):
            if hh < 4:
                g, base, dk = hh // 2, (hh % 2) * 64, 64
            else:
                g, base, dk = hh - 2, 0, 128
            vc = slice(hh * 128, (hh + 1) * 128)
            ob = pO[hh // 4]
            oc = slice((hh % 4) * 128, (hh % 4 + 1) * 128)
            rows = slice(base, base + dk)
            PT = PT2[hh % 2]
            sc_ = slice((hh % 4) * 128, (hh % 4 + 1) * 128)
            p.mm(pS[:, sc_], kpT[rows, g, :], qpT[rows, g, :])
            p.tt(PT[:], pS[:, sc_], cm[d][:, 256:384], ALU.mult)
            p.mm(ob[:, oc], PT[:], Vv[:, vc], start=True, stop=False)
            Sg = Sst[d][g]
            for n, c in enumerate(chunks):
                tk = slice(c * 64, (c + 1) * 64)
                p.mm(ob[tk, oc], qiT[rows, g, tk], Sg[rows, :], start=False, stop=(n == 1))
                kvs = slice((hh % 2) * 256 + n * 128, (hh % 2) * 256 + (n + 1) * 128)
                p.mm(pKV[rows, kvs], ko[tk, g * 128 + base:g * 128 + base + dk], Vv[tk, vc])
                p.stt(Sg[rows, :], Sg[rows, :], dec[rows, g * 2 + c:g * 2 + c + 1], pKV[rows, kvs], ALU.mult, ALU.add)

    order_f = list(range(NT))
    order_b = [1, 0] + list(range(NT - 1, 1, -1))
    for i in order_f:
        mod = modc if i < 2 else modl
        p.dma(xt[:], xin[i * 128:(i + 1) * 128, :])
        p.tt(ht[:], xt[:], mod[:, D:2 * D], ALU.mult)
        p.tt(ht[:], ht[:], mod[:, 0:D], ALU.add)
        transpose_tile(p, hT, ht[:], D, ident, pT)
        for n in range(9):
            c0 = n * 512; c1 = min(4128, c0 + 512); w = wk[n % 2]
            p.dma(V(w.h[:, :, 0:c1 - c0], w.dep), V(w_in.h[:, c0:c1].rearrange("(k q) n -> q k n", q=128), None))
            pp = pA[n % 2]
            for k in range(8):
                p.mm(pp[:, 0:c1 - c0], hT[:, k, :], V(w.h[:, k, 0:c1 - c0], w.dep), start=(k == 0), stop=(k == 7))
            p.copy(Z[:, c0:c1], pp[:, 0:c1 - c0], eng="scalar")
        gates(i)
        scd = sc_deps[i]
        p.dma(V(SC.h[i, :, 0:768], scd), Q[:]); p.dma(V(SC.h[i, :, 768:1536], scd), Kd[1][:])
        p.dma(V(SC.h[i, :, 1536:2304], scd), LAd[1][:]); p.dma(V(SC.h[i, :, 2304:3328], scd), Vv[:])
        p.dma(V(SC.h[i, :, 3328:4352], scd), G[:])
        recur(i, 0)
        p.copy(of[:, 0:512], pO[0][:], eng="scalar"); p.copy(of[:, 512:1024], pO[1][:], eng="scalar")
        p.dma(V(OF.h[i], of_deps[i]), of[:])
    for i in order_b:
        mod = modc if i < 2 else modl
        scd = sc_deps[i]
        p.dma(Q[:], V(SC.h[i, :, 0:768], scd)); p.dma(Kd[1][:], V(SC.h[i, :, 768:1536], scd))
        p.dma(LAd[1][:], V(SC.h[i, :, 1536:2304], scd)); p.dma(Vv[:], V(SC.h[i, :, 2304:3328], scd))
        p.dma(G[:], V(SC.h[i, :, 3328:4352], scd))
        p.dma(of[:], V(OF.h[i], of_deps[i]))
        p.dma(xt[:], xin[i * 128:(i + 1) * 128, :])
        recur(i, 1)
        p.tt(ot[:, 0:512], pO[0][:], of[:, 0:512], ALU.add); p.tt(ot[:, 512:1024], pO[1][:], of[:, 512:1024], ALU.add)
        p.tt(tmp[:], ot[:], ot[:], ALU.mult)
        p.reduce(st[:, 0:8], V(tmp.h[:].rearrange("p (a b) -> p a b", b=128), tmp.dep), ALU.add)
        p.ts(st[:, 0:8], st[:, 0:8], 1.0 / 128, ALU.mult, 1e-6, ALU.add)
        p.act(st[:, 0:8], st[:, 0:8], AF.Sqrt)
        p.recip(st[:, 0:8], st[:, 0:8])
        p.tt(V(yn.h[:].rearrange("p (a b) -> p a b", b=128), yn.dep), V(ot.h[:].rearrange("p (a b) -> p a b", b=128), ot.dep),
             V(st.h[:, 0:8].to_broadcast([128, 8, 128]), st.dep), ALU.mult)
        p.tt(yn[:], yn[:], gv[:], ALU.mult)
        p.tt(yn[:], yn[:], G[:], ALU.mult)
        transpose_tile(p, ynT, yn[:], D, ident, pT)
        for n in range(2):
            p.dma(wk[n][:], V(w_out.h[:, n * 512:(n + 1) * 512].rearrange("(k q) n -> q k n", q=128), None))
            for k in range(8):
                p.mm(pA[n][:], ynT[:, k, :], wk[n][:, k, :], start=(k == 0), stop=(k == 7))
            p.tt(tmp[:, n * 512:(n + 1) * 512], pA[n][:], mod[:, 2 * D + n * 512:2 * D + (n + 1) * 512], ALU.mult)
        p.stt(ot[:], xt[:], ALPHA, tmp[:], ALU.mult, ALU.add)
        layer_norm(p, yn[:], ot[:], lng[:], lnb[:], V(st.h[:, 8:12], st.dep), tmp[:])
        p.dma(V(xout.h[i * 128:(i + 1) * 128, :], out_deps[i]), yn[:])
    if standalone:
        return p.finish(out_deps)
    p.end_phase()


SKIPW = False
NB = NT * 8 + 256


def consts_moe():
    i = np.arange(128)
    triU = (i[:, None] <= i[None, :]).astype(np.float32)
    io13 = (np.arange(8)[None, :] * 128 + i[:, None]).astype(np.float32)
    io2 = (np.arange(2)[None, :] * 128 + i[:, None]).astype(np.float32)
    return dict(ident=np.eye(128, dtype=np.float32), triU=triU, ones=np.ones((128, 128), np.float32),
                bvals=(np.arange(NB, dtype=np.float32) * 128)[None], io13=io13, io2=io2)


def build_moe(dbg=99, p=None, io=None, pfx=""):
    standalone = p is None
    if standalone:
        p = Prog()
    io = io or {}
    p.begin_phase()
    p.store_eng = STORE_ENG.get("moe", "sync")
    IN = lambda n, s, dt=F32: io[n] if n in io else p.dram(pfx + n, s, dt, kind="ExternalInput")
    xin = IN("xin", [NT * 128, D])
    cT = IN("cT", [128, 8]); ccT = IN("ccT", [128, 8])
    mod_w = IN("mod_w", [D, 6 * D]); mod_b = IN("mod_b", [1, 6 * D])
    ln_g = IN("ln_g", [1, D]); ln_b = IN("ln_b", [1, D])
    rw = IN("rw", [D, 256]); rbias = IN("rbias", [1, 256])
    W13 = IN("w13", [256 * 1024, 512]); W2 = IN("w2", [256 * 256, 1024])
    sw13 = IN("sw13", [D, 512]); sw2 = IN("sw2", [256, D])
    ident_d = IN("ident", [128, 128]); triU_d = IN("triU", [128, 128]); ones_d = IN("ones", [128, 128])
    bvals_d = IN("bvals", [1, NB]); io13_d = IN("io13", [128, 8]); io2_d = IN("io2", [128, 2])
    xout = io["xout"] if "xout" in io else p.dram("xout", [NT * 128, D], kind="ExternalOutput")
    U = io["U"] if "U" in io else p.dram("U", [NT, 128, D]); RG = io["RG"] if "RG" in io else p.dram("RG", [NT, 128, 512])
    HB = NB // 2; HR = HB * 128
    XS = io["XS"] if "XS" in io else [p.dram("XSa", [HR, D]), p.dram("XSb", [HR, D])]; YS = io["YS"] if "YS" in io else [p.dram("YSa", [HR, D]), p.dram("YSb", [HR, D])]
    u_deps = [Dep() for _ in range(NT)]; rg_deps = [Dep() for _ in range(NT)]; out_deps = [Dep() for _ in range(NT)]

    if dbg == 0:
        return p.finish([])
    S = lambda n, s, dt=F32: p.sb(n, s, dt)
    ident = S("ident", [128, 128]); triU = S("triU", [128, 128]); ones = S("ones", [128, 128])
    p.dma(ident[:], ident_d[:]); p.dma(triU[:], triU_d[:]); p.dma(ones[:], ones_d[:])
    io13 = S("io13", [128, 8]); io2 = S("io2", [128, 2]); p.dma(io13[:], io13_d[:]); p.dma(io2[:], io2_d[:])
    bv = S("bv", [128, NB]); p.dma(bv[:], pbc(bvals_d))
    modl = S("modl", [128, 3 * D]); modc = S("modc", [128, 3 * D])
    w13t = [S("w13t0", [128, 8, 512]), S("w13r", [128, 8, 512], BF16), S("w13t1", [128, 8, 512])]
    w2t = [S("w2t0", [128, 2, 1024]), S("w2r", [128, 2, 1024], BF16), S("w2t1", [128, 2, 1024])]
    pA = [p.ps("pA0", [128, 512]), p.ps("pA1", [128, 512])]
    pT = p.ps("pT", [128, 512]); pH = p.ps("pH", [128, 512]); pY = [p.ps("pY0", [128, 512]), p.ps("pY1", [128, 512])]
    scr = dict(sc=S("sc", [128, 8]), lh=S("lh", [128, 8, 128]))
    mbb = pbc(mod_b)
    modulation(p, cT, mod_w, mbb, 3 * D, 3 * D, modl, [w13t[0], w13t[0]], pA, scr)
    modulation(p, ccT, mod_w, mbb, 3 * D, 3 * D, modc, [w13t[0], w13t[0]], pA, scr)
    for m in (modl, modc):
        p.ts(m[:, D:2 * D], m[:, D:2 * D], 1.0, ALU.add)
    lng = S("lng", [128, D]); p.dma(lng[:], pbc(ln_g)); lnb = S("lnb", [128, D]); p.dma(lnb[:], pbc(ln_b))
    rwt = S("rwt", [128, 8, 256]); p.dma(rwt[:], V(rw.h.rearrange("(k q) n -> q k n", q=128), None))
    rbt = S("rbt", [128, 256]); p.dma(rbt[:], pbc(rbias))

    xt = S("xt", [128, D]); ut = S("ut", [128, D]); uT = S("uT", [128, 8, 128])
    sc = S("scs", [128, 256]); sel = S("sel", [128, 256]); selm = S("selm", [128, 256]); M = S("M", [128, 256])
    Macc = S("Macc", [128, 256]); RGt = S("RGt", [128, 512]); t256 = S("t256", [128, 256])
    m8g = S("m8g", [128, 8, 8]); m8 = S("m8", [128, 8]); grp = S("grp", [128, 8]); gm = S("gm", [128, 8]); pen = S("pen", [128, 8])
    st = S("st", [128, 16])
    gk = S("gk", [128, NT, 8]); d8 = S("d8", [128, 8]); dsti = [S("dstia", [128, NT, 8], I32), S("dstib", [128, NT, 8], I32)]
    d8b = S("d8b", [128, 8]); ge8 = S("ge8", [128, 8])
    p.memset(Macc[:], 0.0)

    def vmax(out, in_):
        o, a = out.ap, in_.ap
        p.op("vector", lambda e: e.max(o, a), [out], [in_])

    _rc = {}

    def ereg(e):
        if "e" not in _rc:
            r = e.alloc_register(pfx + "ereg")
            e.reg_mov(r, 256 * 128 - 1)
            _rc["e"] = r
        return _rc["e"]

    def bcreg(e):
        if "r" not in _rc:
            r = e.alloc_register(pfx + "bcreg")
            e.reg_mov(r, HR - 1)
            _rc["r"] = r
        return _rc["r"]

    def bc3(v, shape):
        return V(v.ap.to_broadcast(shape), v.dep)

    zt = V(w13t[0].h[:].rearrange("p a b -> p (a b)"), w13t[0].dep)
    p.memset(zt, 0.0)
    for h in range(2):
        XSv = XS[h].h.rearrange("(n q f) d -> n q (f d)", q=128, f=4)
        for n in range(HR // 512):
            p.dma(V(XSv[n], None), zt)

    for i in range(NT):
        mod = modc if i < 2 else modl
        p.dma(xt[:], xin[i * 128:(i + 1) * 128, :])
        p.tt(ut[:], xt[:], mod[:, D:2 * D], ALU.mult)
        p.tt(ut[:], ut[:], mod[:, 0:D], ALU.add)
        p.dma(V(U.h[i], u_deps[i]), ut[:])
        transpose_tile(p, uT, ut[:], D, ident, pT)
        for k in range(8):
            p.mm(pA[0][:, 0:256], uT[:, k, :], rwt[:, k, :], start=(k == 0), stop=(k == 7))
        p.act(sc[:], pA[0][:, 0:256], AF.Sigmoid)
        p.tt(sel[:], sc[:], rbt[:], ALU.add)
        for g in range(8):
            vmax(m8g[:, g, :], sel[:, g * 32:(g + 1) * 32])
        p.tt(grp[:], m8g[:, :, 0], m8g[:, :, 1], ALU.add)
        vmax(m8[:], grp[:])
        p.ts(gm[:], grp[:], m8[:, 3:4], ALU.is_ge)
        p.ts(pen[:], gm[:], -1.0, ALU.add, 1e30, ALU.mult)
        s3 = V(sel.h[:].rearrange("p (a b) -> p a b", b=32), sel.dep)
        sm3 = V(selm.h[:].rearrange("p (a b) -> p a b", b=32), selm.dep)
        p.tt(sm3, s3, V(gm.h[:].to_broadcast([128, 8, 32]), gm.dep), ALU.mult)
        p.tt(sm3, sm3, V(pen.h[:].to_broadcast([128, 8, 32]), pen.dep), ALU.add)
        vmax(m8[:], selm[:])
        p.ts(M[:], selm[:], m8[:, 7:8], ALU.is_ge)
        p.tt(t256[:], M[:], sc[:], ALU.mult)
        p.reduce(st[:, 0:1], t256[:], ALU.add)
        p.recip(st[:, 0:1], st[:, 0:1])
        p.ts(RGt[:, 256:512], t256[:], st[:, 0:1], ALU.mult, 2.5, ALU.mult)
        p.mm(pA[1][:, 0:256], ones[:], Macc[:], start=True, stop=False)
        p.mm(pA[1][:, 0:256], triU[:], M[:], start=False, stop=True)
        p.tt(RGt[:, 0:256], M[:], pA[1][:, 0:256], ALU.mult)
        p.tt(Macc[:], Macc[:], M[:], ALU.add)
        p.dma(V(RG.h[i], rg_deps[i]), RGt[:])

    if dbg == 1:
        return p.finish(rg_deps)
    cnt = S("cnt", [128, 256]); cnti = S("cnti", [128, 256], I32); pe = [S("pe0", [128, 256]), S("pe1", [128, 256])]
    pstart = S("pstart", [128, 256])
    p.mm(pA[0][:, 0:256], ones[:], Macc[:])
    p.ts(cnt[:], pA[0][:, 0:256], 127.0, ALU.add)
    p.copy(cnti[:], cnt[:])
    p.ts(cnti[:], cnti[:], 7, ALU.arith_shift_right, 7, ALU.logical_shift_left)
    p.copy(cnt[:], cnti[:])
    p.copy(pe[0][:], cnt[:])
    cur = 0
    for sft in (1, 2, 4, 8, 16, 32, 64, 128):
        nxt = 1 - cur
        p.copy(pe[nxt][:, 0:sft], pe[cur][:, 0:sft])
        p.tt(pe[nxt][:, sft:256], pe[cur][:, sft:256], pe[cur][:, 0:256 - sft], ALU.add)
        cur = nxt
    pend = pe[cur]
    p.tt(pstart[:], pend[:], cnt[:], ALU.subtract)
    be = S("be", [128, NB])
    cmp_ = V(w13t[0].h[:].rearrange("p a b -> p (a b)"), w13t[0].dep)
    for c0 in range(0, NB, 16):
        cv = V(cmp_.ap.rearrange("p (a b) -> p a b", b=256), cmp_.dep)
        p.tt(cv, V(pend.h[:, None, :].to_broadcast([128, 16, 256]), pend.dep),
             V(bv.h[:, c0:c0 + 16, None].to_broadcast([128, 16, 256]), bv.dep), ALU.is_le)
        p.reduce(be[:, c0:c0 + 16], cv, ALU.add)
    idxE = S("idxE", [128, NB], I32)
    p.ts(be[:], be[:], 128.0, ALU.mult, io13[:, 0:1], ALU.add)
    p.copy(idxE[:], be[:])

    if dbg == 2:
        return p.finish([idxE.dep])
    p.barrier()
    for i in range(NT):
        p.dma(RGt[:], V(RG.h[i], rg_deps[i]))
        p.dma(ut[:], V(U.h[i], u_deps[i]))
        p.stt(t256[:], RGt[:, 0:256], 0.0, pstart[:], ALU.is_gt, ALU.mult)
        p.tt(sel[:], t256[:], RGt[:, 0:256], ALU.add)
        vmax(d8[:], sel[:])
        for k in range(8):
            p.stt(t256[:], sel[:], d8[:, k:k + 1], RGt[:, 256:512], ALU.is_equal, ALU.mult)
            p.reduce(gk[:, i, k:k + 1], t256[:], ALU.add)
        p.ts(d8[:], d8[:], -1.0, ALU.add)
        p.copy(dsti[0][:, i, :], d8[:])
        p.ts(ge8[:], d8[:], float(HR), ALU.is_ge)
        p.ts(d8b[:], d8[:], -float(HR) - 1e6, ALU.add)
        p.tt(d8b[:], d8b[:], ge8[:], ALU.mult)
        p.ts(d8b[:], d8b[:], 1e6, ALU.add)
        p.copy(dsti[1][:, i, :], d8b[:])
        for k in range(8):
            for h in range(2):
                oap, iap, src = XS[h].h[:, :], dsti[h].h[:, i, k:k + 1], ut.h[:, :]
                p.dma(V(XS[h].h[:, :], None), ut[:], eng="gpsimd", extra_reads=[dsti[h][:]],
                      fn=lambda e, oap=oap, iap=iap, src=src: e.indirect_dma_start(
                          out=oap, out_offset=bass.IndirectOffsetOnAxis(ap=iap, axis=0), in_=src, in_offset=None,
                          bounds_check=bcreg(e), oob_is_err=False))

    if dbg == 3:
        return p.finish([XS[0].dep, XS[1].dep])
    p.barrier()
    xs = [S("xs0", [128, D]), S("xs1", [128, D])]; xT = S("xT", [128, 8, 128], BF16)
    a1 = S("a1", [128, 256]); actT = S("actT", [128, 2, 128], BF16); actF = S("actF", [128, 2, 128]); yb = [S("yb0", [128, D]), S("yb1", [128, D])]
    for b in range(NB):
        w13b, w2b, xsb, ybb = w13t[(b % 2) * 2], w2t[(b % 2) * 2], xs[b % 2], yb[b % 2]
        w13r, w2r = w13t[1], w2t[1]
        bh, bl = b // HB, b % HB
        p.dma(xsb[:], XS[bh][bl * 128:(bl + 1) * 128, :])
        for (wb, Wsrc, width) in ((w13b, W13, 4096), (w2b, W2, 2048)):
            oap, iap, src = wb.h[:].rearrange("p a b -> p (a b)"), idxE.h[:, b:b + 1], Wsrc.h.rearrange("(r j) n -> r (j n)", r=256 * 128)
            p.dma(wb[:], Wsrc[:], eng="gpsimd", extra_reads=[idxE[:]],
                  fn=lambda e, oap=oap, iap=iap, src=src: e.indirect_dma_start(
                      out=oap, out_offset=None, in_=src, in_offset=bass.IndirectOffsetOnAxis(ap=iap, axis=0),
                      bounds_check=ereg(e), oob_is_err=False))
        xs3 = xsb.h[:].rearrange("t (q r) -> t r q", r=8)
        for g0 in (0, 4):
            for g in range(g0, g0 + 4):
                p.tr(pT[:, (g - g0) * 128:(g - g0 + 1) * 128], V(xs3[:, g, :], xsb.dep), ident[:])
            p.copy(V(xT.h[:, g0:g0 + 4, :].rearrange("p a b -> p (a b)"), xT.dep), pT[:], eng="scalar")
        p.copy(f32r(w13r[:]), w13b[:], eng="scalar")
        p.copy(f32r(w2r[:]), w2b[:], eng="vector")
        for m in range(4):
            for k in range(8):
                wsel = w13r.h[:, k, (m // 2) * 256:(m // 2 + 1) * 256].rearrange("p (c r) -> p r c", r=2)[:, m % 2, :]
                p.mm(pH[:, m * 128:(m + 1) * 128], V(wsel, w13r.dep), xT[:, k, :], start=(k == 0), stop=(k == 7))
        p.act(a1[:], pH[:, 0:256], AF.Silu)
        p.tt(f32r(V(actT.h[:].rearrange("p a b -> p (a b)"), actT.dep)), a1[:], pH[:, 256:512], ALU.mult)
        for n in range(2):
            for k in range(2):
                p.mm(pY[n][:], f32r(actT[:, k, :]), f32r(w2r[:, k, n * 512:(n + 1) * 512]), start=(k == 0), stop=(k == 1))
        p.copy(ybb[:, 0:512], pY[0][:], eng="scalar")
        p.copy(ybb[:, 512:1024], pY[1][:], eng="vector")
        p.dma(V(YS[bh].h[bl * 128:(bl + 1) * 128, :], None), ybb[:])

    if dbg == 4:
        return p.finish([YS[0].dep, YS[1].dep])
    p.barrier()
    acc = yb[1]; yg = xs; tmp = yb[0]
    sw13t = w13t[0]; p.dma(sw13t[:], V(sw13.h.rearrange("(k q) n -> q k n", q=128), None))
    sw2t = w2t[0]; p.dma(sw2t[:], V(sw2.h.rearrange("(k q) n -> q k n", q=128), None))
    for i in range(NT):
        mod = modc if i < 2 else modl
        p.dma(ut[:], V(U.h[i], u_deps[i]))
        p.dma(xt[:], xin[i * 128:(i + 1) * 128, :])
        transpose_tile(p, uT, ut[:], D, ident, pT)
        for m in range(4):
            for k in range(8):
                p.mm(pH[:, m * 128:(m + 1) * 128], sw13t[:, k, m * 128:(m + 1) * 128], uT[:, k, :], start=(k == 0), stop=(k == 7))
        p.act(a1[:], pH[:, 0:256], AF.Silu)
        p.tt(V(actF.h[:].rearrange("p a b -> p (a b)"), actF.dep), a1[:], pH[:, 256:512], ALU.mult)
        for n in range(2):
            for k in range(2):
                p.mm(pY[n][:], actF[:, k, :], sw2t[:, k, n * 512:(n + 1) * 512], start=(k == 0), stop=(k == 1))
        p.copy(acc[:, 0:512], pY[0][:], eng="scalar")
        p.copy(acc[:, 512:1024], pY[1][:], eng="scalar")
        for k in range(8):
            ygk = yg[k % 2]
            for h in range(2):
                oap, iap, src = ygk.h[:, :], dsti[h].h[:, i, k:k + 1], YS[h].h[:, :]
                p.dma(ygk[:], YS[h][:], eng="gpsimd", extra_reads=[dsti[h][:]],
                      fn=lambda e, oap=oap, iap=iap, src=src: e.indirect_dma_start(
                          out=oap, out_offset=None, in_=src, in_offset=bass.IndirectOffsetOnAxis(ap=iap, axis=0),
                          bounds_check=bcreg(e), oob_is_err=False))
            p.stt(acc[:], ygk[:], gk[:, i, k:k + 1], acc[:], ALU.mult, ALU.add)
        p.tt(acc[:], acc[:], mod[:, 2 * D:3 * D], ALU.mult)
        p.stt(acc[:], xt[:], ALPHA, acc[:], ALU.mult, ALU.add)
        layer_norm(p, ut[:], acc[:], lng[:], lnb[:], V(st.h[:, 8:12], st.dep), tmp[:])
        p.dma(V(xout.h[i * 128:(i + 1) * 128, :], out_deps[i]), ut[:])
    if standalone:
        return p.finish(out_deps)
    p.end_phase()


def moe_inputs(inp, l, b, xl, xc):
    d = dict(xin=np.concatenate([xc, xl], 0), cT=np.ascontiguousarray(inp["c"][b].reshape(8, 128).T),
             ccT=np.ascontiguousarray(inp["c_ctx"].reshape(8, 128).T), mod_w=inp["mod_w"][l], mod_b=inp["mod_b"][l][None],
             ln_g=inp["ln2_g"][l][None], ln_b=inp["ln2_b"][l][None], rw=inp["router_w"][l], rbias=inp["router_bias"][l][None],
             w13=inp["exp_w13"][l].reshape(256 * 1024, 512), w2=inp["exp_w2"][l].reshape(256 * 256, 1024),
             sw13=inp["sh_w13"][l], sw2=inp["sh_w2"][l])
    d.update(consts_moe())
    return d


RWW = 1856
DBGT = 0


def consts_rw():
    i = np.arange(128)
    ch = i // 64
    same = (ch[:, None] == ch[None, :])
    triF = (same & (i[:, None] <= i[None, :])).astype(np.float32)
    triB = (same & (i[:, None] >= i[None, :])).astype(np.float32)
    eye = np.eye(128, dtype=np.float32)
    last = same.astype(np.float32)
    out = dict(ident=eye, ci=np.stack([(ch == 0), (ch == 1)], 1).astype(np.float32))
    for nm, tri in (("F", triF), ("B", triB)):
        ms = tri - eye
        out["cm" + nm] = np.concatenate([tri, last - tri], 1)
        out["mk1" + nm] = np.concatenate([ms, tri], 1)
        out["mk2" + nm] = np.concatenate([-ms, -tri], 1)
        out["mk3" + nm] = np.ascontiguousarray(-ms.T)
    out["mlr"] = np.stack([(i % 64 != 0), (i % 64 != 63)], 1).astype(np.float32)
    return out


def build_l1_rw(dbg=99, p=None, io=None, pfx=""):
    standalone = p is None
    if standalone:
        p = Prog()
    io = io or {}
    p.begin_phase()
    p.store_eng = STORE_ENG.get("rw", "sync")
    IN = lambda n, s, dt=F32: io[n] if n in io else p.dram(pfx + n, s, dt, kind="ExternalInput")
    xin = IN("xin", [NT * 128, D])
    cT = IN("cT", [128, 8]); ccT = IN("ccT", [128, 8])
    mod_w = IN("mod_w", [D, 6 * D]); mod_b = IN("mod_b", [1, 6 * D])
    w_in = IN("w_in", [D, 3392])
    cw = IN("cw", [3, 1536]); cb = IN("cb", [1, 1536]); mu_d = IN("mu", [1, RWW])
    w0_d = IN("w0", [1, 1024]); w2_d = IN("w2", [128, 512]); a0_d = IN("a0", [1, 512]); a2_d = IN("a2", [64, 512]); g2_d = IN("g2", [128, 512])
    vec_d = IN("vecs", [5, 512])
    cst = {k: IN(k, list(v.shape)) for k, v in consts_rw().items()}
    yrw = io["yrw"] if "yrw" in io else p.dram("yrw", [4096, 512], kind="ExternalOutput")
    hv = io["hv"] if "hv" in io else p.dram("hv", [4096, 1536], kind="ExternalOutput")
    PH = p.dram(pfx + "PH", [4096 + 2, 1536]); PRc = p.dram(pfx + "PRc", [256 + 2, RWW]); PRl = p.dram(pfx + "PRl", [4096 + 128, RWW])
    SC = p.dram(pfx + "SC2", [NT, 128, 3584]); OF = p.dram(pfx + "OF2", [NT, 128, 512])
    sc_deps = [Dep() for _ in range(NT)]; of_deps = [Dep() for _ in range(NT)]
    out_deps = [Dep() for _ in range(64)]

    S = lambda n, s, dt=F32: p.sb(n, s, dt)
    C = {}
    for k in ("ident", "ci", "cmF", "cmB", "mk1F", "mk1B", "mk2F", "mk2B", "mk3F", "mk3B", "mlr"):
        C[k] = S(k, list(cst[k].h.shape)); p.dma(C[k][:], cst[k][:])
    ident = C["ident"]; ci = C["ci"]; cm = [C["cmF"], C["cmB"]]; mk1 = [C["mk1F"], C["mk1B"]]; mk2 = [C["mk2F"], C["mk2B"]]; mk3 = [C["mk3F"], C["mk3B"]]
    mlr = C["mlr"]
    modl = S("modl", [128, 2 * D]); modc = S("modc", [128, 2 * D])
    wk = [S("wk0", [128, 8, 512]), S("wk1", [128, 8, 512])]
    pA = [p.ps("pA0", [128, 512]), p.ps("pA1", [128, 512])]
    pT = p.ps("pT", [128, 512]); pG = p.ps("pG", [128, 512]); pP = p.ps("pP", [128, 512]); pY = p.ps("pY", [128, 512])
    pO = p.ps("pO", [128, 512]); pKV = p.ps("pKV", [128, 512])
    xt = S("xt", [128, D]); hT = S("hT", [128, 8, 128]); Z = S("Z", [128, 3392])

    def al(tt_, ap):
        t = T(ap); t.dep = tt_.dep; return t
    scr = dict(sc=S("sc", [128, 8]), lh=al(Z, Z.h[:, 0:1024].rearrange("p (a b) -> p a b", b=128)))
    mbb = pbc(mod_b)
    modulation(p, cT, mod_w, mbb, 0, 2 * D, modl, wk, pA, scr)
    modulation(p, ccT, mod_w, mbb, 0, 2 * D, modc, wk, pA, scr)
    for m in (modl, modc):
        p.ts(m[:, D:2 * D], m[:, D:2 * D], 1.0, ALU.add)

    def bct(name, src_v, n):
        t = S(name, [128, n]); p.dma(t[:], src_v); return t
    cwt = [bct("cw%d" % j, V(cw.h[j].partition_broadcast(128), None), 1536) for j in range(3)]
    cbt = bct("cbt", pbc(cb), 1536); mut = bct("mut", pbc(mu_d), RWW)
    w0t = bct("w0t", pbc(w0_d), 1024); a0t = bct("a0t", pbc(a0_d), 512)
    vecs = [bct("vec%d" % j, V(vec_d.h[j].partition_broadcast(128), None), 512) for j in range(5)]
    kkt, kat, rkt, lgt, lbt = vecs
    w2t = S("w2t", [128, 512]); p.dma(w2t[:], w2_d[:]); a2t = S("a2t", [64, 512]); p.dma(a2t[:], a2_d[:]); g2t = S("g2t", [128, 512]); p.dma(g2t[:], g2_d[:])

    zt = S("zt", [128, RWW]); p.memset(zt[:], 0.0)
    p.dma(PH[0:1, :], zt[0:1, 0:1536]); p.dma(PH[4097:4098, :], zt[0:1, 0:1536])
    p.dma(PRc[0:1, :], zt[0:1, :]); p.dma(PRc[257:258, :], zt[0:1, :])
    p.dma(PRl[0:64, :], zt[0:64, :]); p.dma(PRl[4096 + 64:4096 + 128, :], zt[0:64, :])

    for i in range(NT):
        mod = modc if i < 2 else modl
        p.dma(xt[:], xin[i * 128:(i + 1) * 128, :])
        p.tt(xt[:], xt[:], mod[:, D:2 * D], ALU.mult)
        p.tt(xt[:], xt[:], mod[:, 0:D], ALU.add)
        transpose_tile(p, hT, xt[:], D, ident, pT)
        for n in range(7):
            c0 = n * 512; c1 = min(3392, c0 + 512); w = wk[n % 2]
            p.dma(V(w.h[:, :, 0:c1 - c0], w.dep), V(w_in.h[:, c0:c1].rearrange("(k q) n -> q k n", q=128), None))
            pp = pA[n % 2]
            for k in range(8):
                p.mm(pp[:, 0:c1 - c0], hT[:, k, :], V(w.h[:, k, 0:c1 - c0], w.dep), start=(k == 0), stop=(k == 7))
            p.copy(Z[:, c0:c1], pp[:, 0:c1 - c0], eng="scalar")
        if i < 2:
            p.dma(PRc[1 + i * 128:1 + (i + 1) * 128, :], Z[:, 1536:3392])
        else:
            t0 = (i - 2) * 128
            p.dma(PRl[64 + t0:64 + t0 + 128, :], Z[:, 1536:3392])
            p.dma(PH[1 + t0:1 + t0 + 128, :], Z[:, 0:1536])
    if dbg == 1:
        return p.finish([PRl.dep, PH.dep, PRc.dep])

    Pc = S("Pc", [128, RWW]); Ps = [al(modl, modl.h[:, 0:RWW]), al(modc, modc.h[:, 0:RWW])]; sh = zt
    X1 = S("X1", [128, 128]); X3 = S("X3", [128, 128]); T1 = S("T1", [128, 128]); T2 = S("T2", [64, 128]); T3 = S("T3", [128, 128])
    NM = ["kk", "b", "km", "v", "r", "ldb", "g", "ldf", "a"]
    st_ = {n: S("s_" + n, [128, 512]) for n in NM}
    st8 = S("st8", [128, 16])
    E3 = S("E3", [128, 512]); E2 = S("E2", [128, 512]); ex = S("ex", [128, 512])
    At_ = S("Atl", [128, 512]); Rt_ = S("Rtl", [128, 512]); Kt_ = S("Ktl", [128, 512]); Bt_ = S("Btl", [128, 512])
    Kh = S("Kh", [128, 512]); nBh = S("nBh", [128, 512])
    ARt = S("ARt", [128, 4, 256]); KtT = S("KtT", [128, 4, 128]); BtT = S("BtT", [128, 4, 128])
    dec = S("dec", [128, 8])
    zfree = [1536]

    def Zs(n_):
        t = al(Z, Z.h[:, zfree[0]:zfree[0] + n_]); zfree[0] += n_
        assert zfree[0] <= 3392
        return t
    LM1 = [S("LM1", [128, 256]), Zs(256)]; LM2 = [S("LM2", [128, 256]), Zs(256)]
    Atp = [[S("Atp%d" % j, [128, 128]) for j in range(6)], [Zs(128) for j in range(6)]]
    Anp = [[S("Anp%d" % j, [128, 128]) for j in range(5)], [Zs(128) for j in range(4)] + [S("Anp1_4", [128, 128])]]
    Y = [S("Y", [128, 128]), S("Y1", [128, 128])]; ApT = [S("ApT", [128, 128]), S("ApT1", [128, 128])]
    Usb = [S("Usb", [128, 64]), S("Usb1", [128, 64])]
    Tst = [[S("T%d_%d" % (d, g), [128, 64]) for g in range(4)] for d in range(2)]
    ot = al(xt, xt.h[:, 0:512]); of = al(hT, hT.h[:].rearrange("p a b -> p (a b)")[:, 512:1024]); tmp5 = al(hT, hT.h[:].rearrange("p a b -> p (a b)")[:, 0:512])
    c3 = [al(Z, Z.h[:, 0:1536]), al(wk[0], wk[0].h[:].rearrange("p a b -> p (a b)")[:, 0:1536]), al(wk[1], wk[1].h[:].rearrange("p a b -> p (a b)")[:, 0:1536])]
    p.memset(Usb[0][:], 0.0); p.memset(Usb[1][:], 0.0)
    for d in range(2):
        for g in range(4):
            p.memset(Tst[d][g][:], 0.0)

    def v3(v, b):
        return V(v.ap.rearrange("p (a b) -> p a b", b=b), v.dep)

    def streams(i):
        if i < 2:
            r0 = 1 + i * 128
            p.dma(Pc[:], PRc[r0:r0 + 128, :])
            p.dma(Ps[0][:], PRc[r0 - 1:r0 + 127, :]); p.dma(Ps[1][:], PRc[r0 + 1:r0 + 129, :])
            for j in range(2):
                p.copy(v3(sh[:], 2)[:, :, j], v3(Ps[j][:], 2)[:, :, j], eng="gpsimd")
        else:
            r0 = 64 + (i - 2) * 128
            p.dma(Pc[:], PRl[r0:r0 + 128, :])
            for j, off in enumerate((-1, 1, -64, 64)):
                q = Ps[j % 2]
                p.dma(q[:], PRl[r0 + off:r0 + off + 128, :])
                if j < 2:
                    p.ts(v3(sh[:], 4)[:, :, j], v3(q[:], 4)[:, :, j], mlr[:, j:j + 1], ALU.mult)
                else:
                    p.copy(v3(sh[:], 4)[:, :, j], v3(q[:], 4)[:, :, j], eng="gpsimd")
        p.tt(sh[:], sh[:], Pc[:], ALU.subtract)
        p.tt(sh[:], sh[:], mut[:], ALU.mult)
        p.tt(Pc[:], Pc[:], sh[:], ALU.add)
        r, k, v = Pc[:, 0:512], Pc[:, 512:1024], Pc[:, 1024:1536]
        p.act(X1[:], Pc[:, 1536:1664], AF.Tanh)
        p.act(X3[:], Pc[:, 1728:1856], AF.Sigmoid)
        p.tr(pT[:, 0:128], X1[:], ident[:]); p.tr(pT[0:64, 128:256], Pc[:, 1664:1728], ident[:]); p.tr(pT[:, 256:384], X3[:], ident[:])
        p.copy(T1[:], pT[:, 0:128]); p.copy(T2[:], pT[0:64, 128:256]); p.copy(T3[:], pT[:, 256:384])
        for d, nm in ((0, "ldf"), (1, "ldb")):
            rows = slice(d * 64, d * 64 + 64)
            p.mm(pA[d][:], T1[rows, :], w2t[rows, :])
            p.tt(st_[nm][:], pA[d][:], w0t[:, d * 512:(d + 1) * 512], ALU.add)
            p.act(st_[nm][:], st_[nm][:], AF.Sigmoid)
            p.ts(st_[nm][:], st_[nm][:], -float(np.exp(-0.5)), ALU.mult)
        p.mm(pG[:], T2[:], a2t[:])
        p.tt(st_["a"][:], pG[:], a0t[:], ALU.add)
        p.act(st_["a"][:], st_["a"][:], AF.Sigmoid)
        p.mm(pP[:], T3[:], g2t[:])
        p.copy(st_["g"][:], pP[:], eng="scalar")
        p.copy(st_["v"][:], v, eng="gpsimd"); p.copy(st_["r"][:], r, eng="gpsimd")
        p.tt(st_["kk"][:], k, kkt[:], ALU.mult)
        p.tt(tmp5[:], st_["kk"][:], st_["kk"][:], ALU.mult)
        p.reduce(st8[:, 0:8], v3(tmp5[:], 64), ALU.add)
        p.act(st8[:, 0:8], st8[:, 0:8], AF.Sqrt)
        p.ts(st8[:, 0:8], st8[:, 0:8], 1e-12, ALU.max)
        p.recip(st8[:, 0:8], st8[:, 0:8])
        p.tt(v3(st_["kk"][:], 64), v3(st_["kk"][:], 64), V(st8.h[:, 0:8].to_broadcast([128, 8, 64]), st8.dep), ALU.mult)
        p.ts(tmp5[:], st_["a"][:], -1.0, ALU.add)
        p.tt(tmp5[:], tmp5[:], kat[:], ALU.mult)
        p.ts(tmp5[:], tmp5[:], 1.0, ALU.add)
        p.tt(st_["km"][:], k, tmp5[:], ALU.mult)
        p.tt(st_["b"][:], st_["kk"][:], st_["a"][:], ALU.mult)

    def recur(i, d, emit):
        ld = st_["ldf"] if d == 0 else st_["ldb"]
        for e, dst in ((0, E3), (1, E2)):
            p.mm(pA[e][:], cm[d][:, e * 128:(e + 1) * 128], ld[:])
            p.copy(dst[:], pA[e][:], eng="scalar")
        for g in range(4):
            p.mm(pKV[:, 384 + g * 2:384 + g * 2 + 2], ld[:, g * 128:(g + 1) * 128], ci[:])
        p.act(dec[:], pKV[:, 384:392], AF.Exp)
        p.act(ex[:], E3[:], AF.Exp); p.tt(Rt_[:], st_["r"][:], ex[:], ALU.mult)
        p.act(ex[:], E3[:], AF.Exp, scale=-1.0); p.tt(Kt_[:], st_["km"][:], ex[:], ALU.mult); p.tt(Bt_[:], st_["b"][:], ex[:], ALU.mult)
        p.tt(ex[:], E3[:], ld[:], ALU.subtract); p.act(ex[:], ex[:], AF.Exp); p.tt(At_[:], st_["kk"][:], ex[:], ALU.mult)
        p.act(ex[:], E2[:], AF.Exp); p.tt(Kh[:], st_["km"][:], ex[:], ALU.mult)
        p.stt(nBh[:], st_["b"][:], -1.0, ex[:], ALU.mult, ALU.mult)
        for src, dstv in ((At_, lambda g: ARt[:, g, 0:128]), (Rt_, lambda g: ARt[:, g, 128:256]), (Kt_, lambda g: KtT[:, g, :]), (Bt_, lambda g: BtT[:, g, :])):
            for g in range(4):
                p.tr(pT[:, g * 128:(g + 1) * 128], src[:, g * 128:(g + 1) * 128], ident[:])
            for g in range(4):
                p.copy(dstv(g), pT[:, g * 128:(g + 1) * 128], eng=("scalar" if g % 2 else "vector"))
        chunks = (0, 1) if d == 0 else (1, 0)
        R = [dict(g1=pG[:, 0:256], g2=pG[:, 256:512], g3=pP[:, 0:128], at=pP[:, 128:256], an=pP[:, 256:384],
                  lkv=pY[:, 0:64], ap=[pY[:, 128:256], pY[:, 256:384]], tr=pY[:, 384:512]),
             dict(g1=pA[1][:, 0:256], g2=pA[1][:, 256:512], g3=pT[:, 0:128], at=pT[:, 128:256], an=pT[:, 256:384],
                  lkv=pO[:, 0:0 + 64] if False else pA[1][:, 0:64], ap=[pT[:, 384:512], pT[:, 384:512]], tr=pA[1][:, 256:384])]

        def chain(hh, sl):
            g, base = hh // 2, (hh % 2) * 64
            rows = slice(base, base + 64)
            hc = slice(hh * 64, hh * 64 + 64)
            ya = slice(base, base + 64)
            yv = slice(64 - base, 128 - base)
            r_ = R[sl]
            lm1, lm2, atp, anp, y_, apt, usb = LM1[sl], LM2[sl], Atp[sl], Anp[sl], Y[sl], ApT[sl], Usb[sl]
            p.mm(r_["g1"], KtT[rows, g, :], ARt[rows, g, :])
            p.tt(lm1[:], r_["g1"], mk1[d][:], ALU.mult)
            yield
            p.mm(r_["g2"], BtT[rows, g, :], ARt[rows, g, :])
            p.tt(lm2[:], r_["g2"], mk2[d][:], ALU.mult)
            p.mm(r_["g3"], ARt[rows, g, 0:128], BtT[rows, g, :])
            p.tt(anp[0][:], r_["g3"], mk3[d][:], ALU.mult)
            p.copy(atp[0][:], lm2[:, 0:128], eng="gpsimd")
            yield
            p.mm(r_["lkv"], lm1[:, 0:128], st_["v"][:, hc])
            p.copy(y_[:, yv], r_["lkv"], eng="scalar")
            p.copy(y_[:, ya], At_[:, hc], eng="gpsimd")
            yield
            for j in range(6):
                if j > 0:
                    p.mm(r_["at"], anp[j - 1][:], atp[j - 1][:])
                    p.copy(atp[j][:], r_["at"], eng="scalar")
                    if j < 5:
                        p.mm(r_["an"], atp[j - 1][:], anp[j - 1][:])
                        p.copy(anp[j][:], r_["an"], eng="vector")
                p.mm(r_["ap"][j % 2], atp[j][:], y_[:])
                p.tt(y_[:], y_[:], r_["ap"][j % 2], ALU.add)
                yield
            p.tr(r_["tr"], y_[:], ident[:])
            p.copy(apt[:], r_["tr"], eng="scalar")
            yield
            Tg = Tst[d][g]
            for n, c in enumerate(chunks):
                tk = slice(c * 64, (c + 1) * 64)
                us = slice(256 + sl * 128 + n * 64, 256 + sl * 128 + (n + 1) * 64)
                p.mm(pKV[tk, us], apt[rows, tk], Tg[rows, :])
                p.tt(usb[tk, :], pKV[tk, us], y_[tk, yv], ALU.add)
                if emit:
                    p.mm(pA[0][tk, hc], ARt[rows, g, 128 + c * 64:128 + (c + 1) * 64], Tg[rows, :], start=True, stop=True)
                    p.mm(pO[tk, hc], lm1[tk, 128 + c * 64:128 + (c + 1) * 64], st_["v"][tk, hc], start=True, stop=False)
                    p.mm(pO[tk, hc], lm2[tk, 128 + c * 64:128 + (c + 1) * 64], usb[tk, :], start=False, stop=True)
                kvs = slice(sl * 128 + n * 64, sl * 128 + (n + 1) * 64)
                p.mm(pKV[rows, kvs], Kh[tk, hc], st_["v"][tk, hc], start=True, stop=False)
                p.mm(pKV[rows, kvs], nBh[tk, hc], usb[tk, :], start=False, stop=True)
                p.stt(Tg[rows, :], Tg[rows, :], dec[rows, g * 2 + c:g * 2 + c + 1], pKV[rows, kvs], ALU.mult, ALU.add)
                yield

        for (ha, hb) in ((0, 2), (1, 3), (4, 6), (5, 7)):
            ga, gb = chain(ha, 0), chain(hb, 1)
            da = db = False
            while not (da and db):
                if not da:
                    try:
                        next(ga)
                    except StopIteration:
                        da = True
                if not db:
                    try:
                        next(gb)
                    except StopIteration:
                        db = True

    order_f = list(range(NT))
    order_b = [1, 0] + list(range(NT - 1, 1, -1))
    for i in order_f:
        streams(i)
        if dbg == 2 and i == DBGT:
            return p.finish([st_[n].dep for n in NM])
        scd = sc_deps[i]
        for j, nm in enumerate(("kk", "b", "km", "v", "r", "ldb", "g")):
            p.dma(V(SC.h[i, :, j * 512:(j + 1) * 512], scd), st_[nm][:])
        recur(i, 0, i >= 2)
        if dbg == 3 and i == DBGT:
            return p.finish([t.dep for t in Tst[0]] + [pO.dep])
        if i >= 2:
            p.copy(of[:], pO[:], eng="scalar")
            p.tt(of[:], of[:], pA[0][:], ALU.add)
            p.dma(V(OF.h[i], of_deps[i]), of[:])
            t0 = (i - 2) * 128
            for j in range(3):
                p.dma(c3[j][:], PH[t0 + j:t0 + j + 128, :])
            p.tt(c3[1][:], c3[1][:], cwt[1][:], ALU.mult)
            p.tt(c3[0][:], c3[0][:], cwt[0][:], ALU.mult)
            p.tt(c3[2][:], c3[2][:], cwt[2][:], ALU.mult)
            p.tt(c3[1][:], c3[1][:], c3[0][:], ALU.add)
            p.tt(c3[1][:], c3[1][:], c3[2][:], ALU.add)
            p.tt(c3[1][:], c3[1][:], cbt[:], ALU.add)
            p.dma(V(hv.h[t0:t0 + 128, :], out_deps[32 + i - 2]), c3[1][:])
        if (dbg == 4 and i == 2) or (dbg == 5 and i == NT - 1):
            return p.finish(out_deps + of_deps)
    for i in order_b:
        scd = sc_deps[i]
        for j, nm in enumerate(("kk", "b", "km", "v", "r", "ldb", "g")):
            p.dma(st_[nm][:], V(SC.h[i, :, j * 512:(j + 1) * 512], scd))
        recur(i, 1, i >= 2)
        if i < 2:
            continue
        p.dma(of[:], V(OF.h[i], of_deps[i]))
        p.tt(ot[:], pO[:], of[:], ALU.add)
        p.tt(ot[:], ot[:], pA[0][:], ALU.add)
        o3 = v3(ot[:], 64)
        p.reduce(st8[:, 0:8], o3, ALU.add)
        p.ts(st8[:, 0:8], st8[:, 0:8], -1.0 / 64, ALU.mult)
        p.tt(o3, o3, V(st8.h[:, 0:8].to_broadcast([128, 8, 64]), st8.dep), ALU.add)
        p.tt(tmp5[:], ot[:], ot[:], ALU.mult)
        p.reduce(st8[:, 8:16], v3(tmp5[:], 64), ALU.add)
        p.ts(st8[:, 8:16], st8[:, 8:16], 1.0 / 64, ALU.mult, 64e-5, ALU.add)
        p.act(st8[:, 8:16], st8[:, 8:16], AF.Sqrt)
        p.recip(st8[:, 8:16], st8[:, 8:16])
        p.tt(o3, o3, V(st8.h[:, 8:16].to_broadcast([128, 8, 64]), st8.dep), ALU.mult)
        p.tt(ot[:], ot[:], lgt[:], ALU.mult)
        p.tt(ot[:], ot[:], lbt[:], ALU.add)
        p.tt(tmp5[:], st_["r"][:], st_["km"][:], ALU.mult)
        p.tt(tmp5[:], tmp5[:], rkt[:], ALU.mult)
        p.reduce(st8[:, 0:8], v3(tmp5[:], 64), ALU.add)
        p.tt(v3(tmp5[:], 64), v3(st_["v"][:], 64), V(st8.h[:, 0:8].to_broadcast([128, 8, 64]), st8.dep), ALU.mult)
        p.tt(ot[:], ot[:], tmp5[:], ALU.add)
        p.tt(ot[:], ot[:], st_["g"][:], ALU.mult)
        t0 = (i - 2) * 128
        p.dma(V(yrw.h[t0:t0 + 128, :], out_deps[i - 2]), ot[:])
    if standalone:
        return p.finish(out_deps)
    p.end_phase()


def l1rw_inputs(inp, b, xl, xc):
    j = 0
    d = dict(xin=np.concatenate([xc, xl], 0), cT=np.ascontiguousarray(inp["c"][b].reshape(8, 128).T),
             ccT=np.ascontiguousarray(inp["c_ctx"].reshape(8, 128).T), mod_w=inp["mod_w"][1], mod_b=inp["mod_b"][1][None],
             w_in=inp["od_w_in"][j], cw=inp["hy_conv_w"][j], cb=inp["hy_conv_b"][j][None], mu=inp["rw_mu"][j][None],
             w0=inp["rw_w0"][j].reshape(1, 1024), w2=inp["rw_w2"][j].reshape(128, 512), a0=inp["rw_a0"][j][None], a2=inp["rw_a2"][j],
             g2=inp["rw_g2"][j],
             vecs=np.stack([inp["rw_k_k"][j], inp["rw_k_a"][j], inp["rw_r_k"][j].reshape(512), inp["rw_ln_g"][j].reshape(512), inp["rw_ln_b"][j].reshape(512)]))
    d.update(consts_rw())
    return d


NF = 33
_TAB = {}


def dft_tables():
    if not _TAB:
        n = np.arange(NF * 128, dtype=np.int64)
        prod = (n[:, None] * n[None, :]) % 8192
        ang = prod.astype(np.float64) * (2.0 * np.pi / 8192.0)
        valid = ((n[:, None] <= 4096) & (n[None, :] <= 4096))
        for nm, f in (("C", np.cos), ("S", np.sin)):
            t = (f(ang) * valid).astype(np.float32)
            t = t.reshape(NF, 128, NF, 128).transpose(2, 1, 0, 3)
            _TAB[nm] = np.ascontiguousarray(t)
        import ml_dtypes
        for nm in ("C", "S"):
            _TAB[nm] = _TAB[nm].astype(ml_dtypes.bfloat16)
        cf = np.full(NF * 128, 2.0 / 8192.0); cf[0] = 1.0 / 8192.0; cf[4096] = 1.0 / 8192.0; cf[4097:] = 0.0
        _TAB["cfn"] = np.ascontiguousarray(cf.reshape(NF, 128).T.astype(np.float32))
    return _TAB


def hyena_pos_consts():
    L = 4096
    t = np.linspace(0.0, 1.0, L, dtype=np.float32)[:, None]
    wpos = (2.0 * np.pi * np.arange(L, dtype=np.float32)[:, None] / L).astype(np.float32)
    fr = np.linspace(1e-4, 15, 16, dtype=np.float32)[None, :]
    z = np.concatenate([t, np.cos(fr * wpos), -np.sin(fr * wpos)], -1).astype(np.float32)
    deltas = np.linspace(np.log(1e-2) / 1.5, np.log(1e-2) / 0.3, 512, dtype=np.float32)
    tneg = np.ascontiguousarray((-t[:, 0]).reshape(32, 128).T)
    nz = np.ones((128, 32), np.float32); nz[0, 0] = 0.0
    return dict(zT=np.ascontiguousarray(z.T), absd=np.abs(deltas)[None].astype(np.float32), tneg=tneg, nz=nz)


def build_hy_filters():
    p = Prog()
    IN = lambda n, s, dt=F32: p.dram(n, s, dt, kind="ExternalInput")
    zT = IN("zT", [33, 4096]); w1 = IN("w1", [33, 64]); w2 = IN("w2", [64, 64]); w3s = IN("w3s", [64, 256])
    pv = IN("pv", [64, 4])
    absd = IN("absd", [1, 64]); tneg_d = IN("tneg", [128, 32]); nz_d = IN("nz", [128, 32]); cfn_d = IN("cfn", [128, NF])
    Cb = IN("Cb", [NF, 128, NF, 128]); Sb = IN("Sb", [NF, 128, NF, 128])
    FF = p.dram("FF", [2, 2, NF * 128, 64], kind="ExternalOutput")
    ff_deps = [Dep() for _ in range(NF)]
    S = lambda n, s, dt=F32: p.sb(n, s, dt)
    zTt = S("zTt", [33, 4096]); p.dma(zTt[:], zT[:])
    w1t = S("w1t", [33, 64]); p.dma(w1t[:], w1[:]); w2t = S("w2t", [64, 64]); p.dma(w2t[:], w2[:]); w3t = S("w3t", [64, 256]); p.dma(w3t[:], w3s[:])
    pvt = S("pvt", [64, 4]); p.dma(pvt[:], pv[:])
    adt = S("adt", [128, 64]); p.dma(adt[:], pbc(absd)); tneg = S("tneg", [128, 32]); p.dma(tneg[:], tneg_d[:])
    nz = S("nz", [128, 32]); p.dma(nz[:], nz_d[:]); cfn = S("cfn", [128, NF]); p.dma(cfn[:], cfn_d[:])
    H2 = S("H2", [64, 4096]); arg = S("arg", [64, 512]); nf = S("nf", [64, 512]); ni = S("ni", [64, 512], I32); h1s = S("h1s", [64, 512])
    pA = [p.ps("pA0", [128, 512]), p.ps("pA1", [128, 512])]; pB = [p.ps("pB0", [128, 512]), p.ps("pB1", [128, 512])]
    TWO_PI = float(2.0 * np.pi)

    def sin_of(dst, src_ps, bcol, fcol):
        p.ts(arg[:], src_ps, pvt[:, bcol:bcol + 1], ALU.add, pvt[:, fcol:fcol + 1], ALU.mult)
        p.ts(nf[:], arg[:], 1.0 / TWO_PI, ALU.mult)
        p.copy(ni[:], nf[:]); p.copy(nf[:], ni[:])
        p.stt(arg[:], nf[:], -TWO_PI, arg[:], ALU.mult, ALU.add)
        p.ts(arg[:], arg[:], 3.14159, ALU.min, -3.14159, ALU.max)
        p.act(dst, arg[:], AF.Sin)

    for c in range(8):
        cs = slice(c * 512, (c + 1) * 512)
        p.mm(pA[0][0:64, :], w1t[:], zTt[:, cs])
        sin_of(h1s[:], pA[0][0:64, :], 0, 1)
        p.mm(pA[1][0:64, :], w2t[:], h1s[:])
        sin_of(H2[:, cs], pA[1][0:64, :], 2, 3)
    HS = S("HS", [128, 32, 256]); wdw = S("wdw", [128, 64]); F = S("F", [128, 256]); fb = S("fb", [128, 128])
    for i in range(32):
        p.mm(pB[i % 2][:, 0:256], H2[:, i * 128:(i + 1) * 128], w3t[:])
        p.act(wdw[:], adt[:], AF.Exp, scale=tneg[:, i:i + 1])
        p.tt(V(F.h[:].rearrange("p (a c) -> p a c", c=64), F.dep), V(pB[i % 2].h[:, 0:256].rearrange("p (a c) -> p a c", c=64), pB[i % 2].dep),
             V(wdw.h[:, None, :].to_broadcast([128, 4, 64]), wdw.dep), ALU.mult)
        F4 = F.h[:].rearrange("p (o s c) -> p o s c", o=2, s=2)
        fb2 = V(fb.h[:].rearrange("p (o c) -> p o c", o=2), fb.dep)
        p.ts(fb2, V(F4[:, :, 1, :], F.dep), nz[:, i:i + 1], ALU.mult)
        p.tt(V(HS.h[:, i, 0:128].rearrange("p (o c) -> p o c", o=2), HS.dep), V(F4[:, :, 0, :], F.dep), fb2, ALU.add)
        p.tt(V(HS.h[:, i, 128:256].rearrange("p (o c) -> p o c", o=2), HS.dep), V(F4[:, :, 0, :], F.dep), fb2, ALU.subtract)
    tb = [[S("tb%d_%d" % (a, b), [128, 32, 128]) for b in range(2)] for a in range(2)]
    fo = [S("fo0", [128, 256]), S("fo1", [128, 256])]
    for m in range(NF):
        ct, stb = tb[0][m % 2], tb[1][m % 2]
        p.dma(ct[:], Cb[m, :, 0:32, :]); p.dma(stb[:], Sb[m, :, 0:32, :])
        pr, pi_ = pA[m % 2], pB[m % 2]
        for k in range(32):
            p.mm(pr[:, 0:128], ct[:, k, :], HS[:, k, 0:128], start=(k == 0), stop=(k == 31))
        for k in range(32):
            p.mm(pi_[:, 0:128], stb[:, k, :], HS[:, k, 128:256], start=(k == 0), stop=(k == 31))
        o = fo[m % 2]
        p.ts(o[:, 0:128], pr[:, 0:128], cfn[:, m:m + 1], ALU.mult)
        p.ts(o[:, 128:256], pi_[:, 0:128], cfn[:, m:m + 1], ALU.mult, -1.0, ALU.mult)
        for ri in range(2):
            for od in range(2):
                p.dma(V(FF.h[od, ri, m * 128:(m + 1) * 128, :], ff_deps[m]), o[:, ri * 128 + od * 64:ri * 128 + od * 64 + 64])
    return p.finish(ff_deps)


def hyf_inputs(inp, core):
    j = 0
    w3 = inp["hy_ffn_w3"][j].reshape(64, 2, 2, 512)[:, :, :, core * 64:(core + 1) * 64].reshape(64, 256)
    pc = hyena_pos_consts(); tb = dft_tables()
    return dict(zT=pc["zT"], w1=inp["hy_ffn_w1"][j], w2=inp["hy_ffn_w2"][j], w3s=np.ascontiguousarray(w3),
                pv=np.stack([inp["hy_ffn_b1"][j], inp["hy_sin_freq"][j][0], inp["hy_ffn_b2"][j], inp["hy_sin_freq"][j][1]], 1).astype(np.float32),
                absd=np.ascontiguousarray(pc["absd"][:, core * 64:(core + 1) * 64]), tneg=pc["tneg"], nz=pc["nz"], cfn=tb["cfn"],
                Cb=tb["C"], Sb=tb["S"])


def build_hy_conv(p=None, io=None, pfx=""):
    standalone = p is None
    if standalone:
        p = Prog()
    io = io or {}
    p.begin_phase()
    p.store_eng = STORE_ENG.get("hyc", "sync")
    IN = lambda n, s, dt=F32: io[n] if n in io else p.dram(pfx + n, s, dt, kind="ExternalInput")
    hv = IN("hv", [4096, 1536]); FFd = IN("FFd", [2, 2, NF * 128, 512]); yrw = IN("yrw", [4096, 512]); hyb = IN("hyb", [2, 512])
    xin = IN("xin", [4096, D]); cT = IN("cT", [128, 8]); mod_w = IN("mod_w", [D, 6 * D]); mod_b = IN("mod_b", [1, 6 * D])
    w_out = IN("w_out", [D, D]); ln_g = IN("ln_g", [1, D]); ln_b = IN("ln_b", [1, D]); ident_d = IN("ident", [128, 128])
    Cb = IN("Cb", [NF, 128, NF, 128], BF16); Sb = IN("Sb", [NF, 128, NF, 128], BF16)
    xout = io["xout"] if "xout" in io else p.dram("xout", [4096, D], kind="ExternalOutput")
    ZZ = p.dram(pfx + "ZZ", [2, NF * 128, 512]); Z1 = p.dram(pfx + "Z1", [4096, 512]); HY = p.dram(pfx + "HY", [4096, 512])
    zz_deps = [Dep() for _ in range(NF)]; z1_deps = [Dep() for _ in range(32)]; hy_deps = [Dep() for _ in range(32)]
    out_deps = [Dep() for _ in range(32)]
    S = lambda n, s, dt=F32: p.sb(n, s, dt)

    def al(tt_, ap):
        t = T(ap); t.dep = tt_.dep; return t
    ident = S("ident", [128, 128]); p.dma(ident[:], ident_d[:])
    BIG = S("BIG", [128, NF, 1024], BF16)
    tb = [[S("tb%d_%d" % (a, b), [128, NF, 128], BF16) for b in range(2)] for a in range(2)]
    pA = [p.ps("pA0", [128, 512]), p.ps("pA1", [128, 512])]; pB = [p.ps("pB0", [128, 512]), p.ps("pB1", [128, 512])]
    pT = p.ps("pT", [128, 512])
    stg = [S("stg0", [128, 512]), S("stg1", [128, 512])]
    wk = [S("wk%d" % b, [128, 8, 512]) for b in range(2)]
    scr = dict(sc=S("sc", [128, 8]), lh=S("lh", [128, 8, 128]))
    modg = S("modg", [128, D])
    modulation(p, cT, mod_w, pbc(mod_b), 2 * D, D, modg, wk, pA, scr)
    lng = S("lng", [128, D]); p.dma(lng[:], pbc(ln_g)); lnb = S("lnb", [128, D]); p.dma(lnb[:], pbc(ln_b))
    hbt = [S("hbt%d" % n, [128, 512]) for n in range(2)]
    for n in range(2):
        p.dma(hbt[n][:], V(hyb.h[n].partition_broadcast(128), None))
    ffr = [S("ffr0", [128, 512])] * 2; ffi = [S("ffi0", [128, 512])] * 2
    za = [S("za0", [128, 512])] * 2; zb = [S("zb0", [128, 512])] * 2
    t1 = S("t1", [128, 512])
    yt = S("yt", [128, 512]); zc = S("zc", [128, 512]); gt = S("gt", [128, 512])
    for n in range(2):
        for k in range(32):
            sg = stg[k % 2]
            if n == 0:
                p.dma(sg[:], hv[k * 128:(k + 1) * 128, 0:512])
            else:
                p.dma(sg[:], V(Z1.h[k * 128:(k + 1) * 128, :], z1_deps[k]))
            p.copy(BIG[:, k, 0:512], sg[:], eng=("gpsimd" if k % 2 else "vector"))
        for m in range(NF):
            ct, stb = tb[0][m % 2], tb[1][m % 2]
            p.dma(ct[:, 0:32, :], Cb[m, :, 0:32, :]); p.dma(stb[:, 0:32, :], Sb[m, :, 0:32, :])
            fr, fi = ffr[m % 2], ffi[m % 2]
            p.dma(fr[:], FFd[n, 0, m * 128:(m + 1) * 128, :]); p.dma(fi[:], FFd[n, 1, m * 128:(m + 1) * 128, :])
            pr, pi_ = pA[m % 2], pB[m % 2]
            for k in range(32):
                p.mm(pr[:], ct[:, k, :], BIG[:, k, 0:512], start=(k == 0), stop=(k == 31))
            for k in range(32):
                p.mm(pi_[:], stb[:, k, :], BIG[:, k, 0:512], start=(k == 0), stop=(k == 31))
            a_, b_ = za[m % 2], zb[m % 2]
            p.tt(a_[:], pr[:], fr[:], ALU.mult); p.tt(t1[:], pi_[:], fi[:], ALU.mult); p.tt(a_[:], a_[:], t1[:], ALU.add)
            p.tt(b_[:], pi_[:], fr[:], ALU.mult); p.tt(t1[:], pr[:], fi[:], ALU.mult); p.tt(b_[:], b_[:], t1[:], ALU.subtract)
            p.dma(V(ZZ.h[0, m * 128:(m + 1) * 128, :], zz_deps[m]), a_[:])
            p.dma(V(ZZ.h[1, m * 128:(m + 1) * 128, :], zz_deps[m]), b_[:])
        for k in range(NF):
            p.dma(stg[0][:], V(ZZ.h[0, k * 128:(k + 1) * 128, :], zz_deps[k]))
            p.dma(stg[1][:], V(ZZ.h[1, k * 128:(k + 1) * 128, :], zz_deps[k]))
            p.copy(BIG[:, k, 0:512], stg[0][:], eng="vector")
            p.copy(BIG[:, k, 512:1024], stg[1][:], eng="gpsimd")
        for m in range(32):
            ct, stb = tb[0][m % 2], tb[1][m % 2]
            p.dma(ct[:], Cb[m]); p.dma(stb[:], Sb[m])
            py = pA[m % 2]
            for k in range(NF):
                p.mm(py[:], ct[:, k, :], BIG[:, k, 0:512], start=(k == 0), stop=False)
            for k in range(NF):
                p.mm(py[:], stb[:, k, :], BIG[:, k, 512:1024], start=False, stop=(k == NF - 1))
            rs = slice(m * 128, (m + 1) * 128)
            if n == 0:
                p.dma(zc[:], hv[rs, 0:512])
            else:
                p.dma(zc[:], V(Z1.h[rs, :], z1_deps[m]))
            p.dma(gt[:], hv[rs, 512 * (n + 1):512 * (n + 2)])
            p.tt(zc[:], zc[:], hbt[n][:], ALU.mult)
            p.tt(yt[:], py[:], zc[:], ALU.add)
            p.tt(yt[:], yt[:], gt[:], ALU.mult)
            if n == 0:
                p.dma(V(Z1.h[rs, :], z1_deps[m]), yt[:])
            else:
                p.dma(V(HY.h[rs, :], hy_deps[m]), yt[:])
    wo = wk
    xt = S("xt", [128, D]); yc = S("yc", [128, D]); tmp = S("tmp", [128, D]); pre = S("pre", [128, D]); ycT = S("ycT", [128, 8, 128])
    st = S("st", [128, 8])
    for nn in range(2):
        p.dma(wo[nn][:], V(w_out.h[:, nn * 512:(nn + 1) * 512].rearrange("(k q) n -> q k n", q=128), None))
    for i in range(32):
        rs = slice(i * 128, (i + 1) * 128)
        p.dma(yc[:, 0:512], V(HY.h[rs, :], hy_deps[i])); p.dma(yc[:, 512:1024], yrw[rs, :]); p.dma(xt[:], xin[rs, :])
        transpose_tile(p, ycT, yc[:], D, ident, pT)
        for nn in range(2):
            for k in range(8):
                p.mm(pB[nn][:], ycT[:, k, :], wo[nn][:, k, :], start=(k == 0), stop=(k == 7))
            p.tt(tmp[:, nn * 512:(nn + 1) * 512], pB[nn][:], modg[:, nn * 512:(nn + 1) * 512], ALU.mult)
        p.stt(pre[:], xt[:], ALPHA, tmp[:], ALU.mult, ALU.add)
        layer_norm(p, yc[:], pre[:], lng[:], lnb[:], st, tmp[:])
        p.dma(V(xout.h[rs, :], out_deps[i]), yc[:])
    if standalone:
        return p.finish(out_deps)
    p.end_phase()


def hyc_inputs(inp, b, xl, hv, yrw, FF):
    tb = dft_tables(); j = 0
    return dict(hv=hv, FFd=FF, yrw=yrw, hyb=inp["hy_bias"][j], xin=xl, cT=np.ascontiguousarray(inp["c"][b].reshape(8, 128).T),
                mod_w=inp["mod_w"][1], mod_b=inp["mod_b"][1][None], w_out=inp["od_w_out"][j], ln_g=inp["ln1_g"][1][None], ln_b=inp["ln1_b"][1][None],
                ident=np.eye(128, dtype=np.float32), Cb=tb["C"], Sb=tb["S"])


def emit_hy_filters_full(p, io, pfx):
    p.begin_phase()
    p.store_eng = STORE_ENG.get("hyf", "sync")
    IN = lambda n, s, dt=F32: io[n] if n in io else p.dram(pfx + n, s, dt, kind="ExternalInput")
    zT = IN("zT", [33, 4096]); w1 = IN("w1", [33, 64]); w2 = IN("w2", [64, 64]); w3 = IN("w3", [64, 2048])
    pv = IN("pv", [64, 4]); absd = IN("absd", [1, 512]); tneg_d = IN("tneg", [128, 32]); nz_d = IN("nz", [128, 32]); cfn_d = IN("cfn", [128, NF])
    Cb = io["Cb"]; Sb = io["Sb"]; FF = io["FF"]
    ff_deps = [Dep() for _ in range(NF)]
    S = lambda n, s, dt=F32: p.sb(n, s, dt)
    zTt = S("zTt", [33, 4096]); p.dma(zTt[:], zT[:])
    w1t = S("w1t", [33, 64]); p.dma(w1t[:], w1[:]); w2t = S("w2t", [64, 64]); p.dma(w2t[:], w2[:])
    pvt = S("pvt", [64, 4]); p.dma(pvt[:], pv[:])
    adt = S("adt", [128, 512]); p.dma(adt[:], pbc(absd)); tneg = S("tneg", [128, 32]); p.dma(tneg[:], tneg_d[:])
    nz = S("nz", [128, 32]); p.dma(nz[:], nz_d[:]); cfn = S("cfn", [128, NF]); p.dma(cfn[:], cfn_d[:])
    H2 = S("H2", [64, 4096]); arg = S("arg", [64, 512]); nf = S("nf", [64, 512]); ni = S("ni", [64, 512], I32); h1s = S("h1s", [64, 512])
    pA = [p.ps("pA0", [128, 512]), p.ps("pA1", [128, 512])]; pB = [p.ps("pB0", [128, 512]), p.ps("pB1", [128, 512])]
    TWO_PI = float(2.0 * np.pi)

    def sin_of(dst, src_ps, bcol, fcol):
        p.ts(arg[:], src_ps, pvt[:, bcol:bcol + 1], ALU.add, pvt[:, fcol:fcol + 1], ALU.mult)
        p.ts(nf[:], arg[:], 1.0 / TWO_PI, ALU.mult)
        p.copy(ni[:], nf[:]); p.copy(nf[:], ni[:])
        p.stt(arg[:], nf[:], -TWO_PI, arg[:], ALU.mult, ALU.add)
        p.ts(arg[:], arg[:], 3.14159, ALU.min, -3.14159, ALU.max)
        p.act(dst, arg[:], AF.Sin)

    for c in range(8):
        cs = slice(c * 512, (c + 1) * 512)
        p.mm(pA[0][0:64, :], w1t[:], zTt[:, cs])
        sin_of(h1s[:], pA[0][0:64, :], 0, 1)
        p.mm(pA[1][0:64, :], w2t[:], h1s[:])
        sin_of(H2[:, cs], pA[1][0:64, :], 2, 3)
    HS = S("HS", [128, 32, 1024], BF16); wdw = S("wdw", [128, 512]); F = S("F", [128, 1024]); fb = S("fb", [128, 512]); w3p = S("w3p", [64, 1024])
    tb = [[S("tb%d_%d" % (a, b), [128, 32, 128], BF16) for b in range(2)] for a in range(2)]
    fo = [S("fo0", [128, 1024]), S("fo1", [128, 1024])]
    pC = [p.ps("pC0", [128, 512]), p.ps("pC1", [128, 512])]
    for od in range(2):
        p.dma(w3p[:], w3[:, od * 1024:(od + 1) * 1024])
        for i in range(32):
            for sd in range(2):
                p.mm(pC[sd][:], H2[:, i * 128:(i + 1) * 128], w3p[:, sd * 512:(sd + 1) * 512])
            p.act(wdw[:], adt[:], AF.Exp, scale=tneg[:, i:i + 1])
            p.tt(F[:, 0:512], pC[0][:], wdw[:], ALU.mult)
            p.tt(F[:, 512:1024], pC[1][:], wdw[:], ALU.mult)
            p.ts(fb[:], F[:, 512:1024], nz[:, i:i + 1], ALU.mult)
            p.tt(HS[:, i, 0:512], F[:, 0:512], fb[:], ALU.add)
            p.tt(HS[:, i, 512:1024], F[:, 0:512], fb[:], ALU.subtract)
        for m in range(NF):
            ct, stb = tb[0][m % 2], tb[1][m % 2]
            p.dma(ct[:], Cb[m, :, 0:32, :]); p.dma(stb[:], Sb[m, :, 0:32, :])
            pr, pi_ = pA[m % 2], pB[m % 2]
            for k in range(32):
                p.mm(pr[:], ct[:, k, :], HS[:, k, 0:512], start=(k == 0), stop=(k == 31))
            for k in range(32):
                p.mm(pi_[:], stb[:, k, :], HS[:, k, 512:1024], start=(k == 0), stop=(k == 31))
            o = fo[m % 2]
            p.ts(o[:, 0:512], pr[:], cfn[:, m:m + 1], ALU.mult)
            p.ts(o[:, 512:1024], pi_[:], cfn[:, m:m + 1], ALU.mult, -1.0, ALU.mult)
            for ri in range(2):
                p.dma(V(FF.h[od, ri, m * 128:(m + 1) * 128, :], ff_deps[m]), o[:, ri * 512:(ri + 1) * 512])
    p.end_phase()


def build_fused():
    p = Prog()
    p.use_arena()
    X0 = p.dram("xin0", [NT * 128, D], kind="ExternalInput")
    Cb = p.dram("Cb", [NF, 128, NF, 128], BF16, kind="ExternalInput"); Sb = p.dram("Sb", [NF, 128, NF, 128], BF16, kind="ExternalInput")
    OUT = p.dram("out", [NT * 128, D], kind="ExternalOutput")
    un = lambda n, s: p.dram(n, s)
    XA = un("XA", [NT * 128, D]); XB = un("XB", [NT * 128, D]); XD = un("XD", [NT * 128, D])
    U = un("U", [NT, 128, D]); RG = un("RG", [NT, 128, 512])
    HR = (NB // 2) * 128
    XS = [un("XSa", [HR, D]), un("XSb", [HR, D])]; YS = [un("YSa", [HR, D]), un("YSb", [HR, D])]
    FF = un("FF", [2, 2, NF * 128, 512]); HV = un("HV", [4096, 1536]); YRW = un("YRW", [4096, 512])
    for t in (XA, XB, XD, FF, HV, YRW):
        t.dep = None
    sub = lambda t, r0, r1: T(t.h[r0:r1, :], tracked=False)
    moe_io = dict(U=U, RG=RG, XS=XS, YS=YS)
    build_l0_mixer(p, dict(xin=X0, xout=XA), "a_")
    build_moe(99, p, dict(xin=XA, xout=XB, **moe_io), "b_")
    emit_hy_filters_full(p, dict(Cb=Cb, Sb=Sb, FF=FF), "f_")
    build_l1_rw(99, p, dict(xin=XB, yrw=YRW, hv=HV), "r_")
    build_hy_conv(p, dict(hv=HV, FFd=FF, yrw=YRW, xin=sub(XB, 256, NT * 128), xout=sub(XD, 256, NT * 128), Cb=Cb, Sb=Sb), "h_")
    p.begin_phase()
    p.dma(V(XD.h[0:256, :], None), V(XB.h[0:256, :], None))
    p.end_phase()
    for t in (U, RG, XS[0], XS[1], YS[0], YS[1]):
        t.dep = Dep()
    p.begin_phase()
    build_moe(99, p, dict(xin=XD, xout=OUT, **moe_io), "e_")
    return p.finish([OUT.dep])


def _pref(d, pfx, drop=()):
    return {pfx + k: v for k, v in d.items() if k not in drop}


def fused_inputs(inp, b):
    j = 0
    tb = dft_tables(); pc = hyena_pos_consts()
    xl, xc = inp["x"][b], inp["ctx"][b]
    z = np.zeros((1, 1), np.float32)
    d = {"xin0": np.concatenate([xc, xl], 0), "Cb": tb["C"], "Sb": tb["S"]}
    d.update(_pref(l0_inputs(inp, b, xl, xc), "a_", ("xin",)))
    d.update(_pref(moe_inputs(inp, 0, b, z, z), "b_", ("xin",)))
    d.update(_pref(dict(zT=pc["zT"], w1=inp["hy_ffn_w1"][j], w2=inp["hy_ffn_w2"][j], w3=inp["hy_ffn_w3"][j],
                        pv=np.stack([inp["hy_ffn_b1"][j], inp["hy_sin_freq"][j][0], inp["hy_ffn_b2"][j], inp["hy_sin_freq"][j][1]], 1).astype(np.float32),
                        absd=pc["absd"], tneg=pc["tneg"], nz=pc["nz"], cfn=tb["cfn"]), "f_"))
    d.update(_pref(l1rw_inputs(inp, b, z, z), "r_", ("xin",)))
    d.update(_pref(hyc_inputs(inp, b, z, z, z, z), "h_", ("xin", "hv", "yrw", "FFd", "Cb", "Sb")))
    d.update(_pref(moe_inputs(inp, 1, b, z, z), "e_", ("xin",)))
    return d


def l0_inputs(inp, b, xl, xc):
    gw2 = np.zeros((32, 512), np.float32)
    gw2[0:16, 0:256] = inp["gla_gate_w2"][0, 0]
    gw2[16:32, 256:512] = inp["gla_gate_w2"][0, 1]
    d = dict(xin=np.concatenate([xc, xl], 0), cT=np.ascontiguousarray(inp["c"][b].reshape(8, 128).T),
             ccT=np.ascontiguousarray(inp["c_ctx"].reshape(8, 128).T),
             mod_w=inp["mod_w"][0], mod_b=inp["mod_b"][0][None], ln_g=inp["ln1_g"][0][None], ln_b=inp["ln1_b"][0][None],
             w_in=inp["ev_w_in"][0], w_out=inp["ev_w_out"][0], gw2=gw2, gb=inp["gla_gate_b"][0].reshape(1, 512),
             gvec=np.concatenate([np.tile(inp["gla_norm_g"][0], 4), np.tile(inp["hg_norm_g"][0], 4)])[None],
             lbl=inp["hg_lb_logits"].reshape(1, 2048))
    d.update(consts_l0())
    return d


def _run(nc, maps):
    res = run_bass_kernel_spmd(nc, maps, core_ids=list(range(len(maps))))
    return res.results


def kernel_unfused(**inputs):
    inp = {k: np.ascontiguousarray(np.asarray(v, dtype=np.float32)) for k, v in inputs.items()}
    B = 8
    xl = [inp["x"][b] for b in range(B)]
    xc = [inp["ctx"][b] for b in range(B)]
    r = _run(build_l0_mixer(), [l0_inputs(inp, b, xl[b], xc[b]) for b in range(B)])
    xc = [r[b]["xout"][:256] for b in range(B)]
    xl = [r[b]["xout"][256:] for b in range(B)]
    moe_nc = build_moe()
    r = _run(moe_nc, [moe_inputs(inp, 0, b, xl[b], xc[b]) for b in range(B)])
    xc = [r[b]["xout"][:256] for b in range(B)]
    xl = [r[b]["xout"][256:] for b in range(B)]
    r = _run(build_hy_filters(), [hyf_inputs(inp, c) for c in range(B)])
    FF = np.ascontiguousarray(np.concatenate([r[c]["FF"] for c in range(B)], -1))
    r = _run(build_l1_rw(), [l1rw_inputs(inp, b, xl[b], xc[b]) for b in range(B)])
    yrw = [r[b]["yrw"] for b in range(B)]
    hv = [r[b]["hv"] for b in range(B)]
    r = _run(build_hy_conv(), [hyc_inputs(inp, b, xl[b], hv[b], yrw[b], FF) for b in range(B)])
    xl = [r[b]["xout"] for b in range(B)]
    r = _run(build_moe(), [moe_inputs(inp, 1, b, xl[b], xc[b]) for b in range(B)])
    out = np.stack([r[b]["xout"][256:] for b in range(B)], 0)
    return out.astype(np.float32)


def kernel(**inputs):
    inp = {k: np.ascontiguousarray(np.asarray(v, dtype=np.float32)) for k, v in inputs.items()}
    B = 8
    nc = build_fused()
    r = _run(nc, [fused_inputs(inp, b) for b in range(B)])
    out = np.stack([r[b]["out"][256:] for b in range(B)], 0)
    return out.astype(np.float32)
```
